# Optimizing a Trainium2 kernel written in Bass

```python
import jax, jax.numpy as jnp
from jax import lax
import numpy as np

D_MODEL = 2048
BATCH = 1
SEQ = 8192
DEPTH = 4

N_A_LAYERS = DEPTH // 2
N_B_LAYERS = DEPTH - N_A_LAYERS
DEEPNORM_ALPHA = (2 * DEPTH) ** 0.25
DEEPNORM_BETA = (8 * DEPTH) ** -0.25
LN_EPS = 1e-5
RMS_EPS = 1e-6
D_RNN = D_MODEL
RG_HEADS = 8
RG_BLOCK = D_RNN // RG_HEADS
CONV_WIDTH = 4
RG_C = 8.0
MLA_HEADS = 16
Q_LORA_RANK = 768
KV_LORA_RANK = 512
QK_NOPE_DIM = 128
QK_ROPE_DIM = 64
V_HEAD_DIM = 128
ROPE_THETA = 10000.0
Q_BLOCK = 128
ATTN_SCALE = (QK_NOPE_DIM + QK_ROPE_DIM) ** -0.5
N_EXPERTS = 32
TOP_K = 4
D_EXPERT = 1024
SWIGLU_LIMIT = 7.0
SWIGLU_ALPHA = 1.702
ROW_BLOCK = 128
PLE_DIM = 256

kernel_name = 'hawk_yoco_mla_moe_deepnorm_ple'


def layer_norm(x, g, b):
    xf = x.astype(jnp.float32)
    mu = jnp.mean(xf, axis=-1, keepdims=True)
    var = jnp.mean(jnp.square(xf - mu), axis=-1, keepdims=True)
    return ((xf - mu) * lax.rsqrt(var + LN_EPS) * g + b).astype(x.dtype)


def rms_norm(x, g):
    xf = x.astype(jnp.float32)
    y = xf * lax.rsqrt(jnp.mean(jnp.square(xf), axis=-1, keepdims=True) + RMS_EPS)
    return (y * g).astype(x.dtype)


def rope_tables(positions):
    inv = 1.0 / (ROPE_THETA ** (jnp.arange(0, QK_ROPE_DIM, 2, dtype=jnp.float32) / QK_ROPE_DIM))
    ang = positions.astype(jnp.float32)[..., None] * inv
    return jnp.cos(ang), jnp.sin(ang)


def apply_rope(t, cos, sin):
    half = QK_ROPE_DIM // 2
    t1, t2 = t[..., :half], t[..., half:]
    c, s = cos.astype(t.dtype), sin.astype(t.dtype)
    return jnp.concatenate([t1 * c - t2 * s, t2 * c + t1 * s], axis=-1)


def causal_conv(x, w, b):
    s_len = x.shape[1]
    xp = jnp.pad(x, ((0, 0), (CONV_WIDTH - 1, 0), (0, 0)))
    out = b
    for k in range(CONV_WIDTH):
        out = out + w[k] * xp[:, k:k + s_len]
    return out


def _linear_recurrence_combine(c1, c2):
    a1, b1 = c1
    a2, b2 = c2
    return a1 * a2, a2 * b1 + b2


def rg_lru(x, gate_a_w, gate_a_b, gate_x_w, gate_x_b, lam):
    bsz, s_len, ch = x.shape
    xb = x.reshape(bsz, s_len, RG_HEADS, RG_BLOCK)
    r = jax.nn.sigmoid((jnp.einsum('bshi,hij->bshj', xb, gate_a_w) + gate_a_b).astype(jnp.float32)).reshape(bsz, s_len, ch)
    ig = jax.nn.sigmoid((jnp.einsum('bshi,hij->bshj', xb, gate_x_w) + gate_x_b).astype(jnp.float32)).reshape(bsz, s_len, ch)
    log_a = RG_C * r * jax.nn.log_sigmoid(lam.astype(jnp.float32))
    a = jnp.exp(log_a)
    b = jnp.sqrt(-jnp.expm1(2.0 * log_a)) * (ig * x.astype(jnp.float32))
    _, h = lax.associative_scan(_linear_recurrence_combine, (a, b), axis=1)
    return h.astype(x.dtype)


def recurrent_block(h, w_in, conv_w, conv_b, gate_a_w, gate_a_b, gate_x_w, gate_x_b, lam, w_out):
    u = h @ w_in
    gate_branch = jax.nn.gelu(u[..., :D_RNN], approximate=True)
    rec = causal_conv(u[..., D_RNN:], conv_w, conv_b)
    rec = rg_lru(rec, gate_a_w, gate_a_b, gate_x_w, gate_x_b, lam)
    return (gate_branch * rec) @ w_out


def mla_shared_kv(h, w_dkv, kv_norm, w_ukv, cos, sin):
    bsz, s_len, _ = h.shape
    ckv = h @ w_dkv
    c = rms_norm(ckv[..., :KV_LORA_RANK], kv_norm)
    k_pe = apply_rope(ckv[..., KV_LORA_RANK:], cos, sin)
    kv = (c @ w_ukv).reshape(bsz, s_len, MLA_HEADS, QK_NOPE_DIM + V_HEAD_DIM)
    return kv[..., :QK_NOPE_DIM], k_pe, kv[..., QK_NOPE_DIM:]


def mla_block(h, w_dq, q_norm, w_uq, w_o, k_nope, k_pe, v, cos, sin):
    bsz, s_len, _ = h.shape
    cq = rms_norm(h @ w_dq, q_norm)
    q = (cq @ w_uq).reshape(bsz, s_len, MLA_HEADS, QK_NOPE_DIM + QK_ROPE_DIM)
    q_nope = q[..., :QK_NOPE_DIM]
    q_pe = apply_rope(q[..., QK_NOPE_DIM:], cos[:, :, None, :], sin[:, :, None, :])
    nqb = s_len // Q_BLOCK
    qn = q_nope.reshape(bsz, nqb, Q_BLOCK, MLA_HEADS, QK_NOPE_DIM).transpose(1, 0, 2, 3, 4)
    qp = q_pe.reshape(bsz, nqb, Q_BLOCK, MLA_HEADS, QK_ROPE_DIM).transpose(1, 0, 2, 3, 4)
    k_idx = jnp.arange(s_len)

    def attend_block(args):
        qn_b, qp_b, blk = args
        s = (jnp.einsum('bqhd,bkhd->bhqk', qn_b, k_nope) + jnp.einsum('bqhd,bkd->bhqk', qp_b, k_pe)).astype(jnp.float32) * ATTN_SCALE
        q_idx = blk * Q_BLOCK + jnp.arange(Q_BLOCK)
        s = jnp.where(k_idx[None, :] <= q_idx[:, None], s, jnp.float32(-1e30))
        pr = jax.nn.softmax(s, axis=-1).astype(v.dtype)
        return jnp.einsum('bhqk,bkhd->bqhd', pr, v)

    o = lax.map(attend_block, (qn, qp, jnp.arange(nqb)))
    o = o.transpose(1, 0, 2, 3, 4).reshape(bsz, s_len, MLA_HEADS * V_HEAD_DIM)
    return o @ w_o


def moe(h, router_w, router_b, w1, b1, w2, b2):
    bsz, s_len, d = h.shape
    n_tok = bsz * s_len
    xf = h.reshape(n_tok, d)
    logits = (xf @ router_w + router_b).astype(jnp.float32)
    top_vals, top_idx = lax.top_k(logits, TOP_K)
    gates = jax.nn.softmax(top_vals, axis=-1)
    n_asg = n_tok * TOP_K
    flat_e = top_idx.reshape(n_asg)
    flat_tok = jnp.repeat(jnp.arange(n_tok, dtype=jnp.int32), TOP_K)
    flat_g = gates.reshape(n_asg)
    order = jnp.argsort(flat_e)
    sorted_e = flat_e[order]
    counts = jnp.bincount(flat_e, length=N_EXPERTS)
    padded = (counts + ROW_BLOCK - 1) // ROW_BLOCK * ROW_BLOCK
    pad_end = jnp.cumsum(padded)
    pad_start = pad_end - padded
    start = jnp.cumsum(counts) - counts
    dest = pad_start[sorted_e] + (jnp.arange(n_asg, dtype=jnp.int32) - start[sorted_e])
    n_blocks = (n_asg + ROW_BLOCK - 1) // ROW_BLOCK + N_EXPERTS
    n_rows = n_blocks * ROW_BLOCK
    buf_tok = jnp.full((n_rows,), n_tok, jnp.int32).at[dest].set(flat_tok[order])
    buf_g = jnp.zeros((n_rows,), jnp.float32).at[dest].set(flat_g[order])
    block_e = jnp.minimum(jnp.searchsorted(pad_end, jnp.arange(n_blocks) * ROW_BLOCK, side='right'), N_EXPERTS - 1)
    x_pad = jnp.concatenate([xf, jnp.zeros((1, d), xf.dtype)], axis=0)
    xb = x_pad[buf_tok].reshape(n_blocks, ROW_BLOCK, d)

    def expert_rows(args):
        x_blk, e = args
        hh = x_blk @ w1[e] + b1[e]
        glu = jnp.minimum(hh[..., :D_EXPERT], SWIGLU_LIMIT)
        lin = jnp.clip(hh[..., D_EXPERT:], -SWIGLU_LIMIT, SWIGLU_LIMIT)
        act = glu * jax.nn.sigmoid(SWIGLU_ALPHA * glu) * (lin + 1.0)
        return act @ w2[e] + b2[e]

    yb = lax.map(expert_rows, (xb, block_e)).reshape(n_rows, d)
    y = jax.ops.segment_sum((yb * buf_g[:, None]).astype(yb.dtype), buf_tok, num_segments=n_tok + 1)[:n_tok]
    return y.reshape(bsz, s_len, d)


def setup_inputs(seed: int = 0) -> dict:
    key = jax.random.key(seed)
    ks = iter(jax.random.split(key, 40))

    def nrm(shape, scale):
        return jax.random.normal(next(ks), shape, jnp.float32) * scale

    def gain(shape):
        return 1.0 + nrm(shape, 0.02)

    x = nrm((BATCH, SEQ, D_MODEL), 1.0)
    p = nrm((DEPTH, BATCH, SEQ, PLE_DIM), 1.0)
    offset = jax.random.randint(next(ks), (BATCH, 1), 0, 1024, jnp.int32)
    positions = (offset + jnp.arange(SEQ, dtype=jnp.int32)[None, :]).astype(jnp.int32)
    u = jax.random.uniform(next(ks), (N_A_LAYERS, D_RNN), jnp.float32, 0.9, 0.999)
    a_base = u ** (1.0 / RG_C)
    rg_lambda = jnp.log(a_base) - jnp.log1p(-a_base)
    return {
        'x': x,
        'p': p,
        'positions': positions,
        'ln_mix_g': gain((DEPTH, D_MODEL)),
        'ln_mix_b': nrm((DEPTH, D_MODEL), 0.01),
        'ln_ffn_g': gain((DEPTH, D_MODEL)),
        'ln_ffn_b': nrm((DEPTH, D_MODEL), 0.01),
        'rg_w_in': nrm((N_A_LAYERS, D_MODEL, 2 * D_RNN), D_MODEL ** -0.5),
        'rg_conv_w': nrm((N_A_LAYERS, CONV_WIDTH, D_RNN), CONV_WIDTH ** -0.5),
        'rg_conv_b': nrm((N_A_LAYERS, D_RNN), 0.01),
        'rg_gate_a_w': nrm((N_A_LAYERS, RG_HEADS, RG_BLOCK, RG_BLOCK), RG_BLOCK ** -0.5),
        'rg_gate_a_b': nrm((N_A_LAYERS, RG_HEADS, RG_BLOCK), 0.01),
        'rg_gate_x_w': nrm((N_A_LAYERS, RG_HEADS, RG_BLOCK, RG_BLOCK), RG_BLOCK ** -0.5),
        'rg_gate_x_b': nrm((N_A_LAYERS, RG_HEADS, RG_BLOCK), 0.01),
        'rg_lambda': rg_lambda,
        'rg_w_out': nrm((N_A_LAYERS, D_RNN, D_MODEL), DEEPNORM_BETA * D_RNN ** -0.5),
        'mla_w_dq': nrm((N_B_LAYERS, D_MODEL, Q_LORA_RANK), D_MODEL ** -0.5),
        'mla_q_norm': gain((N_B_LAYERS, Q_LORA_RANK)),
        'mla_w_uq': nrm((N_B_LAYERS, Q_LORA_RANK, MLA_HEADS * (QK_NOPE_DIM + QK_ROPE_DIM)), Q_LORA_RANK ** -0.5),
        'mla_w_o': nrm((N_B_LAYERS, MLA_HEADS * V_HEAD_DIM, D_MODEL), DEEPNORM_BETA * (MLA_HEADS * V_HEAD_DIM) ** -0.5),
        'kv_w_dkv': nrm((D_MODEL, KV_LORA_RANK + QK_ROPE_DIM), D_MODEL ** -0.5),
        'kv_norm': gain((KV_LORA_RANK,)),
        'kv_w_ukv': nrm((KV_LORA_RANK, MLA_HEADS * (QK_NOPE_DIM + V_HEAD_DIM)), KV_LORA_RANK ** -0.5),
        'moe_router_w': nrm((DEPTH, D_MODEL, N_EXPERTS), D_MODEL ** -0.5),
        'moe_router_b': nrm((DEPTH, N_EXPERTS), 0.01),
        'moe_w1': nrm((DEPTH, N_EXPERTS, D_MODEL, 2 * D_EXPERT), D_MODEL ** -0.5),
        'moe_b1': nrm((DEPTH, N_EXPERTS, 2 * D_EXPERT), 0.01),
        'moe_w2': nrm((DEPTH, N_EXPERTS, D_EXPERT, D_MODEL), DEEPNORM_BETA * D_EXPERT ** -0.5),
        'moe_b2': nrm((DEPTH, N_EXPERTS, D_MODEL), 0.01),
        'ple_w_proj': nrm((DEPTH, PLE_DIM, D_MODEL), PLE_DIM ** -0.5),
        'ple_w_gate': nrm((DEPTH, D_MODEL, D_MODEL), D_MODEL ** -0.5),
    }


def reference(x, p, positions, ln_mix_g, ln_mix_b, ln_ffn_g, ln_ffn_b,
              rg_w_in, rg_conv_w, rg_conv_b, rg_gate_a_w, rg_gate_a_b, rg_gate_x_w, rg_gate_x_b, rg_lambda, rg_w_out,
              mla_w_dq, mla_q_norm, mla_w_uq, mla_w_o, kv_w_dkv, kv_norm, kv_w_ukv,
              moe_router_w, moe_router_b, moe_w1, moe_b1, moe_w2, moe_b2, ple_w_proj, ple_w_gate):
    cos, sin = rope_tables(positions)
    h = x
    shared_kv = None
    for i in range(DEPTH):
        if i < N_A_LAYERS:
            mix = recurrent_block(h, rg_w_in[i], rg_conv_w[i], rg_conv_b[i], rg_gate_a_w[i], rg_gate_a_b[i],
                                  rg_gate_x_w[i], rg_gate_x_b[i], rg_lambda[i], rg_w_out[i])
        else:
            if shared_kv is None:
                shared_kv = mla_shared_kv(h, kv_w_dkv, kv_norm, kv_w_ukv, cos, sin)
            j = i - N_A_LAYERS
            k_nope, k_pe, v = shared_kv
            mix = mla_block(h, mla_w_dq[j], mla_q_norm[j], mla_w_uq[j], mla_w_o[j], k_nope, k_pe, v, cos, sin)
        h = layer_norm(DEEPNORM_ALPHA * h + mix, ln_mix_g[i], ln_mix_b[i])
        ffn = moe(h, moe_router_w[i], moe_router_b[i], moe_w1[i], moe_b1[i], moe_w2[i], moe_b2[i])
        h = layer_norm(DEEPNORM_ALPHA * h + ffn, ln_ffn_g[i], ln_ffn_b[i])
        h = h + (p[i] @ ple_w_proj[i]) * jax.nn.sigmoid(h @ ple_w_gate[i])
    return h
```

```python
import contextlib
import math
import numpy as np
import concourse.bass as bass
import concourse.mybir as mybir
from concourse.bass_utils import run_bass_kernel_spmd

F32 = mybir.dt.float32
BF16 = mybir.dt.bfloat16
I32 = mybir.dt.int32
AF = mybir.ActivationFunctionType
ALU = mybir.AluOpType
AX = mybir.AxisListType

NCORE = 8
D = 2048
KC = D // 128
SEQ = 8192
TC = SEQ // NCORE
DEPTH = 4
N_A = 2
ALPHA = (2 * DEPTH) ** 0.25
LN_EPS = 1e-5
RMS_EPS = 1e-6
NEXP = 32
EPC = NEXP // NCORE
CAP = 256
NSLOT = NCORE * CAP
DEXP = 1024
QLORA = 768
KVLORA = 512
ROPE = 64
ATTN_SCALE = (128 + 64) ** -0.5
G4 = [[0, 1, 2, 3], [4, 5, 6, 7]]
G2 = [[0, 4], [1, 5], [2, 6], [3, 7]]


class Buf:
    def __init__(self, t, name):
        self.t = t
        self.name = name
        self.w = {}
        self.r = {}
        self.dsem = None

    def __getitem__(self, idx):
        return self.t[idx]


class Ker:
    NDSEM = 48

    def __init__(self, nc):
        self.nc = nc
        self.es = contextlib.ExitStack()
        self.eng = dict(pe=nc.tensor, dve=nc.vector, act=nc.scalar, pool=nc.gpsimd, sp=nc.sync)
        self.semh = {}
        self.semcur = {}
        self.waited = {}
        for e in ("pe", "dve", "act", "pool", "cc"):
            self._mksem("s_" + e)
        self.free_dsem = []
        for i in range(self.NDSEM):
            self._mksem("d%d" % i)
            self.free_dsem.append("d%d" % i)
        self.phase_dsem = []
        self.phase_bufs = []
        self.freed = {}
        self.ninst = 0

    def _mksem(self, name):
        self.semh[name] = self.es.enter_context(self.nc.semaphore(name))
        self.semcur[name] = 0

    def sb(self, st, name, shape, dt):
        self.nsb = getattr(self, "nsb", 0) + 1
        b = Buf(st.enter_context(self.nc.sbuf_tensor("sb%d_%s" % (self.nsb, name), list(shape), dt)), name)
        b.w = dict(self.freed)
        if hasattr(st, "bufs"):
            st.bufs.append(b)
        self.phase_bufs.append(b)
        return b

    @contextlib.contextmanager
    def scope(self):
        with contextlib.ExitStack() as st:
            st.bufs = []
            yield st
            for b in st.bufs:
                self._merge(self.freed, b.w)
                self._merge(self.freed, b.r)

    def dram(self, name, shape, dt):
        return Buf(self.nc.dram_tensor(name, list(shape), dt), name)

    def _dsem(self, b):
        if b.dsem is None:
            b.dsem = self.free_dsem.pop()
            self.phase_dsem.append(b)
        return b.dsem

    def _wait(self, e, deps):
        for s, v in deps.items():
            if e == "pe" and s == "s_pe":
                continue
            if self.waited.get((e, s), 0) < v:
                self.eng[e].wait_ge(self.semh[s], v)
                self.waited[(e, s)] = v
                self.ninst += 1

    @staticmethod
    def _merge(d, o):
        for s, v in o.items():
            if d.get(s, 0) < v:
                d[s] = v

    def _deps(self, reads, writes):
        deps = {}
        for b in reads:
            self._merge(deps, b.w)
        for b in writes:
            self._merge(deps, b.w)
            self._merge(deps, b.r)
        return deps

    def _commit(self, ev, reads, writes):
        s, v = ev
        for b in reads:
            if b.r.get(s, 0) < v:
                b.r[s] = v
        for b in writes:
            if b.w.get(s, 0) < v:
                b.w[s] = v
            b.r = {}

    def op(self, e, fn, reads=(), writes=()):
        self._wait(e, self._deps(reads, writes))
        ins = fn(self.eng[e])
        s = "s_" + e
        self.semcur[s] += 1
        ins.then_inc(self.semh[s], 1)
        self._commit((s, self.semcur[s]), reads, writes)
        self.ninst += 1

    def dma(self, q, out, in_, reads=(), writes=(), sem=None, **kw):
        self._wait(q, self._deps(reads, writes))
        s = self._dsem(sem)
        ins = self.eng[q].dma_start(out=out, in_=in_, **kw)
        self.semcur[s] += 16
        ins.then_inc(self.semh[s], 16)
        self._commit((s, self.semcur[s]), reads, writes)
        self.ninst += 1

    def gather(self, out, src_rows, idx_ap, reads=(), writes=(), sem=None):
        self._wait("pool", self._deps(reads, writes))
        s = self._dsem(sem)
        ins = self.nc.gpsimd.indirect_dma_start(
            out=out, out_offset=None, in_=src_rows,
            in_offset=bass.IndirectOffsetOnAxis(ap=idx_ap, axis=0))
        self.semcur[s] += 16
        ins.then_inc(self.semh[s], 16)
        self._commit((s, self.semcur[s]), reads, writes)
        self.ninst += 1

    def scatter_add(self, dst_rows, idx_ap, src, reads=(), writes=(), sem=None, extra_deps=None):
        deps = self._deps(reads, ())
        if extra_deps:
            self._merge(deps, extra_deps)
        self._wait("pool", deps)
        s = self._dsem(sem)
        ins = self.nc.gpsimd.indirect_dma_start(
            out=dst_rows, out_offset=bass.IndirectOffsetOnAxis(ap=idx_ap, axis=0), in_=src, in_offset=None,
            compute_op=ALU.add)
        self.semcur[s] += 16
        ins.then_inc(self.semh[s], 16)
        ev = (s, self.semcur[s])
        self._commit(ev, reads, ())
        self.ninst += 1
        return ev

    def collective(self, kind, groups, src, dst, src_ap, dst_ap):
        self._wait("pool", self._deps([src], [dst]))
        op = ALU.add if kind in ("AllReduce", "ReduceScatter") else ALU.bypass
        ins = self.nc.gpsimd.collective_compute(kind, op, replica_groups=groups, ins=[src_ap], outs=[dst_ap])
        self.semcur["s_cc"] += 1
        ins.then_inc(self.semh["s_cc"], 1)
        self._commit(("s_cc", self.semcur["s_cc"]), [src], [dst])
        self.ninst += 1

    def barrier(self):
        for e in ("pe", "dve", "act", "pool", "sp"):
            for s, v in self.semcur.items():
                if v > 0 and self.waited.get((e, s), 0) < v:
                    if e == "pe" and s == "s_pe":
                        continue
                    self.eng[e].wait_ge(self.semh[s], v)
                    self.waited[(e, s)] = v
                    self.ninst += 1
        for b in self.phase_dsem:
            self.free_dsem.append(b.dsem)
            b.dsem = None
        self.phase_dsem = []
        self.phase_bufs = []
        self.freed = {}


class _LazyInputs(dict):
    def __init__(self, prog):
        super().__init__()
        self.prog = prog

    def __missing__(self, name):
        shape, dt = self.prog.shapes[name]
        t = self.prog.nc.dram_tensor(name, list(shape), dt, kind="ExternalInput")
        self[name] = t
        return t


def _fm(ap, p=128):
    return ap.rearrange("(k p) n -> p k n", p=p)


CC_MAX = 1 << 20


def _ag_layout(R, C, es):
    ch = R
    while ch * C * es > CC_MAX:
        assert ch % 2 == 0
        ch //= 2
    pq = 4
    while pq * ch * C * es > 2 * CC_MAX:
        pq //= 2
    return ch, pq


def _ysp_row(c, tk, m):
    g, q = c // 4, c % 4
    return (2 * tk + q // 2) * 512 + g * 256 + (q % 2) * 128 + m


def _ag_rowoff(ch, pq, r, rho):
    i, rp = rho // ch, rho % ch
    g, q = r // 4, r % 4
    hq, ql = q // pq, q % pq
    return ((((i * (4 // pq) + hq) * 2 + g) * pq + ql) * ch + rp)


class AG:
    def __init__(self, k, name, R, C, dt, es):
        self.k = k
        self.R, self.C = R, C
        self.ch, self.pq = _ag_layout(R, C, es)
        self.src = k.dram(name + "_in", [R, C], dt)
        self.mid = k.dram(name + "_mid", [4 * R, C], dt)
        self.out = k.dram(name + "_out", [8 * R, C], dt)

    def rowoff(self, r, rho):
        return _ag_rowoff(self.ch, self.pq, r, rho)

    def rows(self, r, rho, n):
        assert rho // self.ch == (rho + n - 1) // self.ch
        o = self.rowoff(r, rho)
        return self.out.t[o:o + n, :]

    @property
    def nchunk(self):
        return self.R // self.ch

    def run_chunk(self, i):
        k = self.k
        ch, pq = self.ch, self.pq
        k.collective("AllGather", G4, self.src, self.mid, self.src.t[i * ch:(i + 1) * ch, :],
                     self.mid.t[i * 4 * ch:(i + 1) * 4 * ch, :])
        for hq in range(4 // pq):
            a = (i * 4 + hq * pq) * ch
            b = ((i * (4 // pq) + hq) * 2) * pq * ch
            k.collective("AllGather", G2, self.mid, self.out, self.mid.t[a:a + pq * ch, :],
                         self.out.t[b:b + 2 * pq * ch, :])

    def run(self):
        for i in range(self.nchunk):
            self.run_chunk(i)


class Prog:
    def __init__(self, nc, stop_after=None, dumps=(), debug=False):
        self.nc = nc
        self.debug = debug
        self.k = Ker(nc)
        self.stop_after = stop_after
        self.dumps = list(dumps)
        self.stopped = False
        self.inp = _LazyInputs(self)
        self.out_dumps = {}

    def din(self, name, shape, dt=F32):
        t = self.nc.dram_tensor(name, list(shape), dt, kind="ExternalInput")
        self.inp[name] = t
        return t

    def declare(self):
        self.shapes = {}

        def d(name, shape, dt=F32):
            self.shapes[name] = (shape, dt)
        d("xT", [D, TC]); d("pT", [DEPTH, 256, TC]); d("pos", [1, TC], I32)
        d("ln_mix_g", [DEPTH, 128, KC]); d("ln_mix_b", [DEPTH, 128, KC])
        d("ln_ffn_g", [DEPTH, 128, KC]); d("ln_ffn_b", [DEPTH, 128, KC])
        d("w_in_g", [N_A, D, D]); d("w_in_r", [N_A, D, 256]); d("w_out", [N_A, D, D])
        d("conv_w", [N_A, 128, 2, 4]); d("conv_b", [N_A, 128, 2])
        d("ga_w", [N_A, 256, 256]); d("ga_b", [N_A, 128, 2]); d("gx_w", [N_A, 256, 256]); d("gx_b", [N_A, 128, 2])
        d("lam", [N_A, 128, 2])
        d("router_w", [DEPTH, D, NEXP]); d("router_b", [DEPTH, 128, NEXP])
        d("w1", [DEPTH, EPC, D, 2 * DEXP]); d("b1", [DEPTH, EPC, 128, 16])
        d("w2", [DEPTH, EPC, DEXP, D]); d("b2", [DEPTH, EPC, 128, 16])
        d("ple_proj", [DEPTH, 256, D]); d("ple_gate", [DEPTH, D, D])
        d("w_dq", [2, D, QLORA]); d("q_norm", [2, 128, 6]); d("w_o", [2, D, D])
        d("w_uq_n", [2, QLORA, 256]); d("w_uq_pe", [2, QLORA, 128]); d("w_uq_pesw", [2, QLORA, 128])
        d("w_dkv_c", [D, KVLORA]); d("w_dkv_pe", [D, ROPE]); d("w_dkv_pesw", [D, ROPE]); d("kv_norm", [128, 4])
        d("w_ukv_k", [KVLORA, 256]); d("w_ukv_v", [KVLORA, 256])
        d("ropec", [64, 2]); d("masks", [128, 4, 512]); d("ident", [128, 128])
        d("idx_t1", [128, KC], I32); d("idx_tF", [128, KC], I32)
        d("ltri", [128, 128]); d("iota", [128, CAP]); d("vals3", [128, 8, 3]); d("dumprow", [128, 2])
        d("idx_L", [128, EPC * NCORE * 2], I32); d("idx_y", [128, 8], I32); d("b2row", [DEPTH, EPC, D]); d("idx_g", [128, EPC * 16], I32)
        self.outT = self.nc.dram_tensor("outT", [D, TC], F32, kind="ExternalOutput")

    def scratch(self):
        k = self.k
        self.hT_d = k.dram("hT_d", [D, TC], F32)
        self.h1T_d = k.dram("h1T_d", [D, TC], F32)
        self.gb_d = k.dram("gb_d", [D, TC], BF16)
        self.agH = AG(k, "agH", D, TC, BF16, 2)
        self.agF = AG(k, "agF", 8 * 256, TC, BF16, 2)
        self.agG = AG(k, "agG", NEXP, TC, F32, 4)
        self.agKc = AG(k, "agKc", KVLORA, TC, BF16, 2)
        self.agKp = AG(k, "agKp", ROPE, TC, BF16, 2)
        self.agQ = AG(k, "agQ", QLORA, TC, BF16, 2)
        self.agT = AG(k, "agT", 128, TC, F32, 4)
        self.agHt = AG(k, "agHt", TC, D, BF16, 2)
        self.agL = AG(k, "agL", 2 * 128 * NEXP, 4, F32, 4)
        self.ysp = [k.dram("ysp%d" % i, [SEQ + CAP, D], F32) for i in range(3)]
        self.dbg_d = k.dram("dbg_d", [D, TC], F32)
        self.dbg2_d = k.dram("dbg2_d", [D, TC], BF16)
        self.ym = [k.dram("ym0", [D, SEQ], F32), k.dram("ym1", [D, SEQ], F32), k.dram("ym2", [D, SEQ], F32)]

    def end_phase(self, name):
        self.k.barrier()
        if self.stop_after == name:
            self.stopped = True
        return self.stopped

    def consts(self, st):
        k = self.k
        self.ps = []
        for i in range(8):
            self.ps.append(Buf(st.enter_context(self.nc.psum_tensor("ps%d" % i, [128, 512], F32)), "ps%d" % i))
        self.ones32 = k.sb(st, "ones32", [128, 128], F32)
        self.onesb = k.sb(st, "onesb", [128, 128], BF16)
        self.ident = k.sb(st, "ident", [128, 128], F32)
        self.t1 = k.sb(st, "t1", [128, KC], I32)
        k.op("dve", lambda e: e.memset(self.ones32[:], 1.0), writes=[self.ones32])
        k.op("dve", lambda e: e.memset(self.onesb[:], 1.0), writes=[self.onesb])
        k.dma("sp", self.ident[:], self.inp["ident"][:, :], writes=[self.ident], sem=self.ident)
        k.dma("sp", self.t1[:], self.inp["idx_t1"][:, :], writes=[self.t1], sem=self.t1)
        self.identb = k.sb(st, "identb", [128, 128], BF16)
        k.op("dve", lambda e: e.tensor_copy(out=self.identb[:], in_=self.ident[:]), reads=[self.ident], writes=[self.identb])
        self.tF = k.sb(st, "tF", [128, KC], I32)
        k.dma("sp", self.tF[:], self.inp["idx_tF"][:, :], writes=[self.tF], sem=self.tF)
        self.psi = 0

    def load_fm_tile(self, x, ag, r, half, nrows):
        k = self.k
        ch = ag.ch
        for r0 in range(0, nrows, ch):
            n = min(ch, nrows - r0)
            k.dma("sp", x[:, r0 // 128:(r0 + n) // 128, :], _fm(ag.rows(r, r0, n)[:, half * 512:(half + 1) * 512]),
                  reads=[ag.out], writes=[x], sem=x)

    def nps(self):
        p = self.ps[self.psi % 8]
        self.psi += 1
        return p

    def linear_fm(self, st, xT, kc, ntok, wsrc, M, epi, mblk=512, tile=512, tag="w"):
        k = self.k
        mblk = min(mblk, M)
        nblk = (M + mblk - 1) // mblk
        ws = [k.sb(st, "%s_s%d" % (tag, i), [128, kc, mblk], BF16) for i in range(min(2, nblk))]

        def load(bi):
            w = ws[bi % len(ws)]
            m0 = bi * mblk
            mw = min(mblk, M - m0)
            k.dma("pool", w[:, :, :mw], _fm(wsrc[:, m0:m0 + mw]), writes=[w], sem=w)

        load(0)
        for bi in range(nblk):
            if bi + 1 < nblk:
                load(bi + 1)
            w = ws[bi % len(ws)]
            m0 = bi * mblk
            mw = min(mblk, M - m0)
            for mi in range((mw + 127) // 128):
                mr = min(128, mw - mi * 128)
                for t0 in range(0, ntok, tile):
                    ps = self.nps()
                    for kk in range(kc):
                        k.op("pe", lambda e, kk=kk, ps=ps, w=w, mi=mi, mr=mr, t0=t0: e.matmul(
                            ps[:mr, :tile], lhsT=w[:, kk, mi * 128:mi * 128 + mr], rhs=xT[:, kk, t0:t0 + tile],
                            start=(kk == 0), stop=(kk == kc - 1)), reads=[w, xT], writes=[ps])
                    epi((m0 // 128) + mi, mr, t0, tile, ps)

    def norm_fm(self, st, z, kc, ntok, gcol, bcol, eps, center, tag):
        k = self.k
        nfeat = kc * 128
        sq = [k.sb(st, "%s_sq%d" % (tag, i), [128, 512], F32) for i in range(2)]
        mean = k.sb(st, tag + "_mean", [128, 512], F32)
        rstd = k.sb(st, tag + "_rstd", [128, 512], F32)
        tmp = [k.sb(st, "%s_tmp%d" % (tag, i), [128, 512], F32) for i in range(2)]
        for t0 in range(0, ntok, 512):
            sl = slice(t0, t0 + 512)
            ps_s = self.nps()
            ps_m = self.nps() if center else None
            for kk in range(kc):
                s = sq[kk % 2]
                k.op("act", lambda e, s=s, kk=kk: e.activation(out=s[:], in_=z[:, kk, sl], func=AF.Square),
                     reads=[z], writes=[s])
                k.op("pe", lambda e, s=s, kk=kk: e.matmul(ps_s[:, :], lhsT=self.ones32[:], rhs=s[:],
                                                          start=(kk == 0), stop=(kk == kc - 1)),
                     reads=[self.ones32, s], writes=[ps_s])
                if center:
                    k.op("pe", lambda e, kk=kk: e.matmul(ps_m[:, :], lhsT=self.ones32[:], rhs=z[:, kk, sl],
                                                         start=(kk == 0), stop=(kk == kc - 1)),
                         reads=[self.ones32, z], writes=[ps_m])
            if center:
                k.op("act", lambda e: e.activation(out=mean[:], in_=ps_m[:, :], func=AF.Copy, scale=1.0 / nfeat),
                     reads=[ps_m], writes=[mean])
                k.op("dve", lambda e: e.tensor_tensor(out=rstd[:], in0=mean[:], in1=mean[:], op=ALU.mult),
                     reads=[mean], writes=[rstd])
                k.op("dve", lambda e: e.scalar_tensor_tensor(out=rstd[:], in0=ps_s[:, :], scalar=1.0 / nfeat,
                                                             in1=rstd[:], op0=ALU.mult, op1=ALU.subtract),
                     reads=[ps_s, rstd], writes=[rstd])
                k.op("dve", lambda e: e.tensor_scalar(out=rstd[:], in0=rstd[:], scalar1=float(eps), scalar2=None,
                                                      op0=ALU.add), reads=[rstd], writes=[rstd])
            else:
                k.op("dve", lambda e: e.tensor_scalar(out=rstd[:], in0=ps_s[:, :], scalar1=1.0 / nfeat,
                                                      scalar2=float(eps), op0=ALU.mult, op1=ALU.add),
                     reads=[ps_s], writes=[rstd])
            k.op("act", lambda e: e.activation(out=rstd[:], in_=rstd[:], func=AF.Sqrt), reads=[rstd], writes=[rstd])
            k.op("dve", lambda e: e.reciprocal(out=rstd[:], in_=rstd[:]), reads=[rstd], writes=[rstd])
            for kk in range(kc):
                t = tmp[kk % 2]
                if center:
                    k.op("dve", lambda e, t=t, kk=kk: e.tensor_tensor(out=t[:], in0=z[:, kk, sl], in1=mean[:],
                                                                      op=ALU.subtract), reads=[z, mean], writes=[t])
                    k.op("pool", lambda e, t=t: e.tensor_tensor(out=t[:], in0=t[:], in1=rstd[:], op=ALU.mult),
                         reads=[t, rstd], writes=[t])
                else:
                    k.op("dve", lambda e, t=t, kk=kk: e.tensor_tensor(out=t[:], in0=z[:, kk, sl], in1=rstd[:],
                                                                      op=ALU.mult), reads=[z, rstd], writes=[t])
                if bcol is not None:
                    k.op("act", lambda e, t=t, kk=kk: e.activation(out=z[:, kk, sl], in_=t[:], func=AF.Identity,
                                                                   scale=gcol[:, kk:kk + 1], bias=bcol[:, kk:kk + 1]),
                         reads=[t, gcol, bcol], writes=[z])
                else:
                    k.op("act", lambda e, t=t, kk=kk: e.activation(out=z[:, kk, sl], in_=t[:], func=AF.Identity,
                                                                   scale=gcol[:, kk:kk + 1]),
                         reads=[t, gcol], writes=[z])

    def phase_init(self):
        k = self.k
        with k.scope() as st:
            posi = k.sb(st, "posi", [64, TC], I32)
            ang = k.sb(st, "ang", [64, TC], F32)
            rc = k.sb(st, "rc", [64, 2], F32)
            k.dma("sp", posi[:], self.inp["pos"][0:1, :].partition_broadcast(64), writes=[posi], sem=posi)
            k.dma("sp", rc[:], self.inp["ropec"][:, :], writes=[rc], sem=rc)
            k.op("dve", lambda e: e.tensor_copy(out=ang[:], in_=posi[:]), reads=[posi], writes=[ang])
            k.op("dve", lambda e: e.tensor_scalar(out=ang[:], in0=ang[:], scalar1=rc[:, 0:1],
                                                  scalar2=1.0 / (2 * math.pi), op0=ALU.mult, op1=ALU.mult),
                 reads=[ang, rc], writes=[ang])
            tabs = k.sb(st, "tabs", [64, 2, TC], F32)
            ni = k.sb(st, "ni", [64, TC], I32)
            nf = k.sb(st, "nf", [64, TC], F32)
            fr = k.sb(st, "fr", [64, TC], F32)
            for j, shift in enumerate((0.25, 0.0)):
                k.op("dve", lambda e, shift=shift: e.tensor_scalar(out=fr[:], in0=ang[:], scalar1=float(shift),
                                                                   scalar2=None, op0=ALU.add),
                     reads=[ang], writes=[fr])
                k.op("dve", lambda e: e.tensor_copy(out=ni[:], in_=fr[:]), reads=[fr], writes=[ni])
                k.op("dve", lambda e: e.tensor_copy(out=nf[:], in_=ni[:]), reads=[ni], writes=[nf])
                k.op("dve", lambda e: e.tensor_tensor(out=fr[:], in0=fr[:], in1=nf[:], op=ALU.subtract),
                     reads=[fr, nf], writes=[fr])
                k.op("dve", lambda e: e.tensor_scalar(out=nf[:], in0=fr[:], scalar1=0.5, scalar2=None, op0=ALU.is_gt),
                     reads=[fr], writes=[nf])
                k.op("dve", lambda e: e.tensor_tensor(out=fr[:], in0=fr[:], in1=nf[:], op=ALU.subtract),
                     reads=[fr, nf], writes=[fr])
                k.op("dve", lambda e: e.tensor_scalar(out=nf[:], in0=fr[:], scalar1=-0.5, scalar2=None, op0=ALU.is_lt),
                     reads=[fr], writes=[nf])
                k.op("dve", lambda e: e.tensor_tensor(out=fr[:], in0=fr[:], in1=nf[:], op=ALU.add),
                     reads=[fr, nf], writes=[fr])
                k.op("act", lambda e, j=j: e.activation(out=tabs[:, j, :], in_=fr[:], func=AF.Sin,
                                                        scale=2 * math.pi), reads=[fr], writes=[tabs])
            k.op("dve", lambda e: e.tensor_scalar(out=tabs[:, 1, :], in0=tabs[:, 1, :], scalar1=rc[:, 1:2],
                                                  scalar2=None, op0=ALU.mult), reads=[tabs, rc], writes=[tabs])
            k.dma("sp", self.agT.src.t[:, :].rearrange("(j p) t -> p j t", p=64), tabs[:], reads=[tabs],
                  writes=[self.agT.src], sem=tabs)
            self.agT.run()
        return self.end_phase("init")

    def phase_rg1(self, l, hsrc):
        k = self.k
        with k.scope() as st:
            hTb = k.sb(st, "hTb", [128, KC, TC], BF16)
            k.dma("pool", hTb[:], _fm(hsrc), writes=[hTb], sem=hTb)
            k.dma("sp", _fm(self.agH.src.t[:, :]), hTb[:], reads=[hTb], writes=[self.agH.src], sem=hTb)
            self.agH.run()
            gst = [k.sb(st, "gst%d" % i, [128, TC], BF16) for i in range(2)]

            def epi(m, mr, t0, nt, ps):
                g = gst[m % 2]
                k.op("act", lambda e: e.activation(out=g[:, t0:t0 + nt], in_=ps[:, :nt], func=AF.Gelu_apprx_tanh),
                     reads=[ps], writes=[g])
                if t0 + nt == TC:
                    k.dma("sp", self.gb_d.t[m * 128:(m + 1) * 128, :], g[:], reads=[g], writes=[self.gb_d], sem=g)

            self.linear_fm(st, hTb, KC, TC, self.inp["w_in_g"][l], D, epi, tag="wg")
        return self.end_phase("rg1_%d" % l)

    def phase_rg2(self, l):
        k = self.k
        inp = self.inp
        with k.scope() as st:
            wr = k.sb(st, "wr", [128, KC, 256], BF16)
            k.dma("pool", wr[:], _fm(inp["w_in_r"][l]), writes=[wr], sem=wr)
            gw = []
            for nm in ("ga_w", "gx_w"):
                g = k.sb(st, nm, [128, 2, 256], BF16)
                k.dma("pool", g[:], _fm(inp[nm][l]), writes=[g], sem=g)
                gw.append(g)
            cw = k.sb(st, "cw", [128, 2, 4], F32); cb = k.sb(st, "cb", [128, 2], F32)
            gab = k.sb(st, "gab", [128, 2], F32); gxb = k.sb(st, "gxb", [128, 2], F32)
            lam = k.sb(st, "lam", [128, 2], F32); c1 = k.sb(st, "c1", [128, 2], F32)
            for b, nm in ((cw, "conv_w"), (cb, "conv_b"), (gab, "ga_b"), (gxb, "gx_b"), (lam, "lam")):
                k.dma("sp", b[:], inp[nm][l], writes=[b], sem=b)
            k.op("act", lambda e: e.activation(out=c1[:], in_=lam[:], func=AF.Exp, scale=-1.0), reads=[lam], writes=[c1])
            k.op("dve", lambda e: e.tensor_scalar(out=c1[:], in0=c1[:], scalar1=1.0, scalar2=None, op0=ALU.add),
                 reads=[c1], writes=[c1])
            k.op("act", lambda e: e.activation(out=c1[:], in_=c1[:], func=AF.Ln), reads=[c1], writes=[c1])
            k.op("dve", lambda e: e.tensor_scalar(out=c1[:], in0=c1[:], scalar1=-8.0, scalar2=None, op0=ALU.mult),
                 reads=[c1], writes=[c1])
            xts = [k.sb(st, "xt%d" % i, [128, KC, 512], BF16) for i in range(2)]
            ub = [k.sb(st, "ub%d" % i, [128, 515], F32) for i in range(2)]
            xc = [k.sb(st, "xc%d" % i, [128, 512], F32) for i in range(2)]
            xcb = k.sb(st, "xcb", [128, 2, 512], BF16)
            rg = [k.sb(st, "rg%d" % i, [128, 512], F32) for i in range(2)]
            ig = [k.sb(st, "ig%d" % i, [128, 512], F32) for i in range(2)]
            av = [k.sb(st, "av%d" % i, [128, 512], F32) for i in range(2)]
            bv = [k.sb(st, "bv%d" % i, [128, 512], F32) for i in range(2)]
            rec = [k.sb(st, "rec%d" % i, [128, 512], F32) for i in range(2)]
            hst = [k.sb(st, "hst%d" % i, [128, 1], F32) for i in range(2)]
            recb = [k.sb(st, "recb%d" % i, [128, 2, 512], BF16) for i in range(2)]
            for i in range(2):
                k.op("dve", lambda e, i=i: e.memset(ub[i][:], 0.0), writes=[ub[i]])
                k.op("dve", lambda e, i=i: e.memset(hst[i][:], 0.0), writes=[hst[i]])
            def load(tt):
                self.load_fm_tile(xts[tt % 2], self.agH, tt // 2, tt % 2, D)

            load(0)
            for tt in range(16):
                if tt + 1 < 16:
                    load(tt + 1)
                x = xts[tt % 2]
                rb = recb[tt % 2]
                for mi in range(2):
                    u = ub[mi]
                    if tt > 0:
                        k.op("dve", lambda e, u=u: e.tensor_copy(out=u[:, 0:3], in_=u[:, 512:515]), reads=[u], writes=[u])
                    ps = self.nps()
                    for kk in range(KC):
                        k.op("pe", lambda e, kk=kk, ps=ps, mi=mi, x=x: e.matmul(
                            ps[:, :], lhsT=wr[:, kk, mi * 128:(mi + 1) * 128], rhs=x[:, kk, :],
                            start=(kk == 0), stop=(kk == KC - 1)), reads=[wr, x], writes=[ps])
                    k.op("act", lambda e, u=u, ps=ps: e.activation(out=u[:, 3:515], in_=ps[:, :], func=AF.Copy),
                         reads=[ps], writes=[u])
                    c = xc[mi]
                    k.op("dve", lambda e, c=c, u=u, mi=mi: e.tensor_scalar(
                        out=c[:], in0=u[:, 0:512], scalar1=cw[:, mi, 0:1], scalar2=cb[:, mi:mi + 1],
                        op0=ALU.mult, op1=ALU.add), reads=[u, cw, cb], writes=[c])
                    for j in range(1, 4):
                        k.op("dve", lambda e, c=c, u=u, mi=mi, j=j: e.scalar_tensor_tensor(
                            out=c[:], in0=u[:, j:j + 512], scalar=cw[:, mi, j:j + 1], in1=c[:],
                            op0=ALU.mult, op1=ALU.add), reads=[u, cw, c], writes=[c])
                    k.op("act", lambda e, c=c, mi=mi: e.activation(out=xcb[:, mi, :], in_=c[:], func=AF.Copy),
                         reads=[c], writes=[xcb])
                for gi, (dst, bias) in enumerate(((rg, gab), (ig, gxb))):
                    for mo in range(2):
                        ps = self.nps()
                        for ki in range(2):
                            k.op("pe", lambda e, ps=ps, ki=ki, mo=mo, gi=gi: e.matmul(
                                ps[:, :], lhsT=gw[gi][:, ki, mo * 128:(mo + 1) * 128], rhs=xcb[:, ki, :],
                                start=(ki == 0), stop=(ki == 1)), reads=[gw[gi], xcb], writes=[ps])
                        k.op("act", lambda e, ps=ps, mo=mo, dst=dst, bias=bias: e.activation(
                            out=dst[mo][:], in_=ps[:, :], func=AF.Sigmoid, bias=bias[:, mo:mo + 1]),
                             reads=[ps, bias], writes=[dst[mo]])
                for mo in range(2):
                    a, b, c = av[mo], bv[mo], xc[mo]
                    k.op("act", lambda e, a=a, mo=mo: e.activation(out=a[:], in_=rg[mo][:], func=AF.Exp,
                                                                   scale=c1[:, mo:mo + 1]), reads=[rg[mo], c1], writes=[a])
                    k.op("dve", lambda e, a=a, b=b: e.tensor_tensor(out=b[:], in0=a[:], in1=a[:], op=ALU.mult),
                         reads=[a], writes=[b])
                    k.op("dve", lambda e, b=b: e.tensor_scalar(out=b[:], in0=b[:], scalar1=-1.0, scalar2=1.0,
                                                               op0=ALU.mult, op1=ALU.add), reads=[b], writes=[b])
                    k.op("dve", lambda e, b=b: e.tensor_scalar(out=b[:], in0=b[:], scalar1=0.0, scalar2=None,
                                                               op0=ALU.max), reads=[b], writes=[b])
                    k.op("act", lambda e, b=b: e.activation(out=b[:], in_=b[:], func=AF.Sqrt), reads=[b], writes=[b])
                    k.op("dve", lambda e, c=c, mo=mo: e.tensor_tensor(out=c[:], in0=c[:], in1=ig[mo][:], op=ALU.mult),
                         reads=[c, ig[mo]], writes=[c])
                    k.op("dve", lambda e, b=b, c=c: e.tensor_tensor(out=b[:], in0=b[:], in1=c[:], op=ALU.mult),
                         reads=[b, c], writes=[b])
                    k.op("dve", lambda e, a=a, b=b, mo=mo: e.tensor_tensor_scan(
                        out=rec[mo][:], data0=a[:], data1=b[:], initial=hst[mo][:, 0:1], op0=ALU.mult, op1=ALU.add),
                         reads=[a, b, hst[mo]], writes=[rec[mo]])
                    k.op("dve", lambda e, mo=mo: e.tensor_copy(out=hst[mo][:], in_=rec[mo][:, 511:512]),
                         reads=[rec[mo]], writes=[hst[mo]])
                    k.op("act", lambda e, mo=mo, rb=rb: e.activation(out=rb[:, mo, :], in_=rec[mo][:], func=AF.Copy),
                         reads=[rec[mo]], writes=[rb])
                jb = tt // 2
                k.dma("sp", self.agF.src.t[jb * 256:(jb + 1) * 256, (tt % 2) * 512:(tt % 2 + 1) * 512].rearrange(
                    "(m p) t -> p m t", p=128), rb[:], reads=[rb], writes=[self.agF.src], sem=rb)
                if tt % 4 == 3:
                    self.agF.run_chunk(tt // 4)
        return self.end_phase("rg2_%d" % l)

    def mix_and_tail(self, l, st, mT, wsrc, hsrc, name):
        k = self.k
        inp = self.inp
        with k.scope() as stz:
            zt = k.sb(stz, "zt", [128, D], F32)
            k.op("pool", lambda e: e.memset(zt[:], 0.0), writes=[zt])
            for i in range((SEQ + CAP) // 128):
                k.dma("sp", self.ysp[0].t[i * 128:(i + 1) * 128, :], zt[:], reads=[zt], writes=[self.ysp[0]], sem=zt)
        z = k.sb(st, "z", [128, KC, TC], F32)
        k.dma("sp", z[:], _fm(hsrc), writes=[z], sem=z)
        gcol = k.sb(st, "lng", [128, KC], F32); bcol = k.sb(st, "lnb", [128, KC], F32)
        k.dma("sp", gcol[:], inp["ln_mix_g"][l], writes=[gcol], sem=gcol)
        k.dma("sp", bcol[:], inp["ln_mix_b"][l], writes=[bcol], sem=bcol)

        def epi(m, mr, t0, nt, ps):
            k.op("dve", lambda e: e.scalar_tensor_tensor(out=z[:, m, t0:t0 + nt], in0=z[:, m, t0:t0 + nt],
                                                         scalar=float(ALPHA), in1=ps[:, :nt], op0=ALU.mult, op1=ALU.add),
                 reads=[z, ps], writes=[z])

        with k.scope() as st2:
            self.linear_fm(st2, mT, KC, TC, wsrc, D, epi, tag="wo")
        if self.debug:
            k.dma("sp", _fm(self.dbg_d.t[:, :]), z[:], reads=[z], writes=[self.dbg_d], sem=z)
            k.dma("sp", _fm(self.dbg2_d.t[:, :]), mT[:], reads=[mT], writes=[self.dbg2_d], sem=mT)
        with k.scope() as st2:
            self.norm_fm(st2, z, KC, TC, gcol, bcol, LN_EPS, True, "ln1")
        k.dma("sp", _fm(self.h1T_d.t[:, :]), z[:], reads=[z], writes=[self.h1T_d], sem=z)
        with k.scope() as st2:
            rw = k.sb(st2, "rw", [128, KC, NEXP], F32)
            rbias = k.sb(st2, "rbias", [128, NEXP], F32)
            k.dma("sp", rw[:], _fm(inp["router_w"][l]), writes=[rw], sem=rw)
            k.dma("sp", rbias[:], inp["router_b"][l], writes=[rbias], sem=rbias)
            lg = k.sb(st2, "lg", [128, NEXP], F32)
            Gall = k.sb(st2, "Gall", [128, 8, NEXP], F32)
            Mall = k.sb(st2, "Mall", [128, 8, NEXP], F32)
            Pall = k.sb(st2, "Pall", [128, 8, NEXP], F32)
            top = k.sb(st2, "top", [128, 8], F32); sc = k.sb(st2, "sc", [128, 2], F32)
            ltri = k.sb(st2, "ltri", [128, 128], F32)
            iota = k.sb(st2, "iota", [128, CAP], F32)
            vals3 = k.sb(st2, "vals3", [128, 8, 3], F32)
            dumprow = k.sb(st2, "dumprow", [128, 2], F32)
            for b, nm in ((ltri, "ltri"), (iota, "iota"), (vals3, "vals3"), (dumprow, "dumprow")):
                k.dma("sp", b[:], inp[nm].ap(), writes=[b], sem=b)
            htm = [k.sb(st2, "htm%d" % i, [128, D], BF16) for i in range(2)]
            for tk in range(TC // 128):
                tsl = slice(tk * 128, (tk + 1) * 128)
                hm = htm[tk % 2]
                for q4 in range(4):
                    pst = self.nps()
                    for j in range(4):
                        kk = q4 * 4 + j
                        k.op("pe", lambda e, pst=pst, kk=kk, j=j: e.transpose(
                            out=pst[:, j * 128:(j + 1) * 128], in_=z[:, kk, tsl], identity=self.ident[:]),
                             reads=[z, self.ident], writes=[pst])
                    k.op("act" if q4 % 2 else "dve", (lambda e, pst=pst, q4=q4: e.activation(
                        out=hm[:, q4 * 512:(q4 + 1) * 512], in_=pst[:, :], func=AF.Copy)) if q4 % 2 else
                         (lambda e, pst=pst, q4=q4: e.tensor_copy(out=hm[:, q4 * 512:(q4 + 1) * 512], in_=pst[:, :])),
                         reads=[pst], writes=[hm])
                k.dma("sp", self.agHt.src.t[tsl, :], hm[:], reads=[hm], writes=[self.agHt.src], sem=hm)
                if tk % 2 == 1:
                    self.agHt.run_chunk(tk // 2)
                ps = self.nps()
                for kk in range(KC):
                    k.op("pe", lambda e, kk=kk, ps=ps: e.matmul(
                        ps[:, :NEXP], lhsT=z[:, kk, tsl], rhs=rw[:, kk, :],
                        start=(kk == 0), stop=(kk == KC - 1)), reads=[z, rw], writes=[ps])
                ex = Gall[:, tk, :]
                mk = Mall[:, tk, :]
                k.op("dve", lambda e, ps=ps: e.tensor_tensor(out=lg[:], in0=ps[:, :NEXP], in1=rbias[:], op=ALU.add),
                     reads=[ps, rbias], writes=[lg])
                k.op("dve", lambda e: e.max(out=top[:], in_=lg[:]), reads=[lg], writes=[top])
                k.op("dve", lambda e: e.tensor_scalar(out=sc[:, 0:1], in0=top[:, 0:1], scalar1=-1.0, scalar2=None,
                                                      op0=ALU.mult), reads=[top], writes=[sc])
                k.op("act", lambda e: e.activation(out=ex, in_=lg[:], func=AF.Exp, bias=sc[:, 0:1]),
                     reads=[lg, sc], writes=[Gall])
                k.op("dve", lambda e: e.tensor_scalar(out=mk, in0=lg[:], scalar1=top[:, 3:4], scalar2=None,
                                                      op0=ALU.is_ge), reads=[lg, top], writes=[Mall])
                k.op("dve", lambda e: e.tensor_tensor(out=ex, in0=ex, in1=mk, op=ALU.mult),
                     reads=[Gall, Mall], writes=[Gall])
                k.op("dve", lambda e: e.reduce_sum(out=sc[:, 1:2], in_=ex, axis=AX.X), reads=[Gall], writes=[sc])
                k.op("dve", lambda e: e.reciprocal(out=sc[:, 1:2], in_=sc[:, 1:2]), reads=[sc], writes=[sc])
                k.op("dve", lambda e: e.tensor_scalar(out=ex, in0=ex, scalar1=sc[:, 1:2], scalar2=None,
                                                      op0=ALU.mult), reads=[Gall, sc], writes=[Gall])
            assert self.agHt.ch == 256
            for tk in range(8):
                ps = self.nps()
                for t2 in range(tk + 1):
                    k.op("pe", lambda e, ps=ps, t2=t2: e.matmul(
                        ps[:, :NEXP], lhsT=(ltri[:] if t2 == tk else self.ones32[:]), rhs=Mall[:, t2, :],
                        start=(t2 == 0), stop=(t2 == tk)), reads=[ltri, self.ones32, Mall], writes=[ps])
                k.op("act", lambda e, ps=ps: e.activation(out=Pall[:, tk, :], in_=ps[:, :NEXP], func=AF.Copy),
                     reads=[ps], writes=[Pall])
            R = [self.nps(), self.nps()]
            Gt = [self.nps(), self.nps()]
            oh = [k.sb(st2, "oh%d" % i, [128, CAP], F32) for i in range(4)]
            n = 0
            for ee in range(NEXP):
                for tk in range(8):
                    o = oh[n % 4]
                    n += 1
                    k.op("dve", lambda e, o=o: e.tensor_scalar(
                        out=o[:], in0=iota[:], scalar1=Pall[:, tk, ee:ee + 1], scalar2=Mall[:, tk, ee:ee + 1],
                        op0=ALU.is_equal, op1=ALU.mult), reads=[iota, Pall, Mall], writes=[o])
                    for sb in range(2):
                        k.op("pe", lambda e, o=o, sb=sb: e.matmul(
                            R[sb][:, ee * 3:(ee + 1) * 3], lhsT=o[:, sb * 128:(sb + 1) * 128], rhs=vals3[:, tk, :],
                            start=(tk == 0), stop=(tk == 7)), reads=[o, vals3], writes=[R[sb]])
                        k.op("pe", lambda e, o=o, sb=sb: e.matmul(
                            Gt[sb][:, ee:ee + 1], lhsT=o[:, sb * 128:(sb + 1) * 128], rhs=Gall[:, tk, ee:ee + 1],
                            start=(tk == 0), stop=(tk == 7)), reads=[o, Gall], writes=[Gt[sb]])
            L = k.sb(st2, "L", [128, 2, NEXP, 4], F32)
            tmpf = k.sb(st2, "tmpf", [128, NEXP], F32)
            for sb in range(2):
                Rv = R[sb][:, 0:NEXP * 3].rearrange("p (e c) -> p e c", c=3)
                k.op("act", lambda e, sb=sb, Rv=Rv: e.activation(out=L[:, sb, :, 0], in_=Rv[:, :, 0], func=AF.Copy),
                     reads=[R[sb]], writes=[L])
                k.op("act", lambda e, sb=sb, Rv=Rv: e.activation(out=L[:, sb, :, 2], in_=Rv[:, :, 2], func=AF.Copy),
                     reads=[R[sb]], writes=[L])
                k.op("act", lambda e, sb=sb: e.activation(out=L[:, sb, :, 3], in_=Gt[sb][:, 0:NEXP], func=AF.Copy),
                     reads=[Gt[sb]], writes=[L])
                k.op("dve", lambda e, sb=sb, Rv=Rv: e.tensor_scalar(
                    out=tmpf[:], in0=Rv[:, :, 2], scalar1=-1.0, scalar2=1.0, op0=ALU.mult, op1=ALU.add),
                     reads=[R[sb]], writes=[tmpf])
                k.op("dve", lambda e, sb=sb: e.tensor_scalar(
                    out=tmpf[:], in0=tmpf[:], scalar1=dumprow[:, sb:sb + 1], scalar2=None, op0=ALU.mult),
                     reads=[tmpf, dumprow], writes=[tmpf])
                k.op("dve", lambda e, sb=sb, Rv=Rv: e.tensor_tensor(out=L[:, sb, :, 1], in0=Rv[:, :, 1], in1=tmpf[:],
                                                                    op=ALU.add), reads=[R[sb], tmpf], writes=[L])
            k.dma("sp", self.agL.src.t[:, :].rearrange("(sb p e) f -> p sb e f", sb=2, p=128), L[:], reads=[L],
                  writes=[self.agL.src], sem=L)
            self.agL.run()

    def phase_rg3(self, l, hsrc):
        k = self.k
        with k.scope() as st:
            mT = k.sb(st, "mT", [128, KC, TC], BF16)
            k.dma("sp", mT[:], _fm(self.gb_d.t[:, :]), writes=[mT], sem=mT)
            with k.scope() as st2:
                recT = k.sb(st2, "recT", [128, KC, TC], BF16)
                rows = self.agF.out.t[:, :]
                for kk in range(KC):
                    k.gather(recT[:, kk, :], rows, self.tF[:, kk:kk + 1], reads=[self.tF, self.agF.out],
                             writes=[recT], sem=recT)
                for kk in range(KC):
                    k.op("dve" if kk % 2 else "pool", lambda e, kk=kk: e.tensor_tensor(
                        out=mT[:, kk, :], in0=mT[:, kk, :], in1=recT[:, kk, :], op=ALU.mult),
                         reads=[mT, recT], writes=[mT])
            self.mix_and_tail(l, st, mT, self.inp["w_out"][l], hsrc, "rg3")
        return self.end_phase("rg3_%d" % l)

    def phase_moe_dense(self, l):
        k = self.k
        inp = self.inp
        grows = self.agG.out.t[:, :].rearrange("r (h t) -> (r h) t", t=512)
        ydst = self.ym[0]
        with k.scope() as st:
            W1 = k.sb(st, "W1", [128, KC, 2 * DEXP], BF16)
            W2 = k.sb(st, "W2", [128, DEXP // 128, D], BF16)
            b1c = k.sb(st, "b1c", [128, 16], F32); b2c = k.sb(st, "b2c", [128, 16], F32)
            gidx = k.sb(st, "gidx", [128, EPC * 16], I32)
            k.dma("sp", gidx[:], inp["idx_g"][:, :], writes=[gidx], sem=gidx)
            xts = [k.sb(st, "mx%d" % i, [128, KC, 512], BF16) for i in range(2)]
            grow = [k.sb(st, "grow%d" % i, [128, 512], F32) for i in range(2)]
            lin1 = k.sb(st, "lin1", [128, 8, 512], BF16)
            gt = [k.sb(st, "gt%d" % i, [128, 512], F32) for i in range(2)]
            sg = [k.sb(st, "sg%d" % i, [128, 512], F32) for i in range(2)]
            actT = k.sb(st, "actT", [128, 8, 512], BF16)
            yst = [k.sb(st, "yst%d" % i, [128, 4, 512], F32) for i in range(2)]
            yreg = [[Buf(None, "yreg") for _ in range(4)] for _ in range(16)]
            ysi = 0
            for el in range(EPC):
                k.dma("pool", W1[:], _fm(inp["w1"][l, el]), writes=[W1], sem=W1)
                k.dma("pool", W2[:], _fm(inp["w2"][l, el]), writes=[W2], sem=W2)
                k.dma("sp", b1c[:], inp["b1"][l, el], writes=[b1c], sem=b1c)
                k.dma("sp", b2c[:], inp["b2"][l, el], writes=[b2c], sem=b2c)

                def load(tt):
                    self.load_fm_tile(xts[tt % 2], self.agH, tt // 2, tt % 2, D)
                    g = grow[tt % 2]
                    k.gather(g[:], grows, gidx[:, el * 16 + tt:el * 16 + tt + 1], reads=[gidx, self.agG.out],
                             writes=[g], sem=g)

                load(0)
                for tt in range(16):
                    if tt + 1 < 16:
                        load(tt + 1)
                    x = xts[tt % 2]
                    g = grow[tt % 2]
                    for m in list(range(8, 16)) + list(range(8)):
                        ps = self.nps()
                        for kk in range(KC):
                            k.op("pe", lambda e, kk=kk, ps=ps, m=m, x=x: e.matmul(
                                ps[:, :], lhsT=W1[:, kk, m * 128:(m + 1) * 128], rhs=x[:, kk, :],
                                start=(kk == 0), stop=(kk == KC - 1)), reads=[W1, x], writes=[ps])
                        if m >= 8:
                            t = gt[m % 2]
                            k.op("dve", lambda e, ps=ps, m=m, t=t: e.tensor_scalar(
                                out=t[:], in0=ps[:, :], scalar1=b1c[:, m:m + 1], scalar2=7.0, op0=ALU.add, op1=ALU.min),
                                 reads=[ps, b1c], writes=[t])
                            k.op("dve", lambda e, m=m, t=t: e.tensor_scalar(
                                out=lin1[:, m - 8, :], in0=t[:], scalar1=-7.0, scalar2=1.0, op0=ALU.max, op1=ALU.add),
                                 reads=[t], writes=[lin1])
                        else:
                            t = gt[m % 2]; s = sg[m % 2]
                            k.op("dve", lambda e, ps=ps, m=m, t=t: e.tensor_scalar(
                                out=t[:], in0=ps[:, :], scalar1=b1c[:, m:m + 1], scalar2=7.0, op0=ALU.add, op1=ALU.min),
                                 reads=[ps, b1c], writes=[t])
                            k.op("act", lambda e, t=t, s=s: e.activation(out=s[:], in_=t[:], func=AF.Sigmoid, scale=1.702),
                                 reads=[t], writes=[s])
                            k.op("pool", lambda e, t=t, s=s: e.tensor_tensor(out=s[:], in0=s[:], in1=t[:], op=ALU.mult),
                                 reads=[s, t], writes=[s])
                            k.op("dve", lambda e, m=m, s=s: e.tensor_tensor(out=actT[:, m, :], in0=s[:], in1=lin1[:, m, :],
                                                                            op=ALU.mult), reads=[s, lin1], writes=[actT])
                    for f in range(16):
                        ps = self.nps()
                        for kk in range(8):
                            k.op("pe", lambda e, kk=kk, ps=ps, f=f: e.matmul(
                                ps[:, :], lhsT=W2[:, kk, f * 128:(f + 1) * 128], rhs=actT[:, kk, :],
                                start=(kk == 0), stop=(kk == 7)), reads=[W2, actT], writes=[ps])
                        ys = yst[ysi % 2]
                        k.op("dve", lambda e, ps=ps, f=f, ys=ys, g=g: e.scalar_tensor_tensor(
                            out=ys[:, f % 4, :], in0=ps[:, :], scalar=b2c[:, f:f + 1], in1=g[:],
                            op0=ALU.add, op1=ALU.mult), reads=[ps, b2c, g], writes=[ys])
                        if f % 4 == 3:
                            f0 = f - 3
                            reg = yreg[tt][f // 4]
                            dst = ydst.t[f0 * 128:(f0 + 4) * 128, tt * 512:(tt + 1) * 512].rearrange(
                                "(j p) t -> p j t", p=128)
                            if el == 0:
                                k.dma("sp", dst, ys[:], reads=[ys], writes=[reg], sem=ys)
                            else:
                                k.dma("pool", dst, ys[:], reads=[ys], writes=[reg], sem=ys, accum_op=ALU.add)
                            ysi += 1
            for row in yreg:
                for reg in row:
                    k._merge(ydst.w, reg.w)
            for i in range(D // 128):
                sl = slice(i * 128, (i + 1) * 128)
                k.collective("AllReduce", G4, self.ym[0], self.ym[1], self.ym[0].t[sl, :], self.ym[1].t[sl, :])
            for i in range(D // 128):
                sl = slice(i * 128, (i + 1) * 128)
                k.collective("AllReduce", G2, self.ym[1], self.ym[2], self.ym[1].t[sl, :], self.ym[2].t[sl, :])
        return self.end_phase("moe_%d" % l)

    def phase_moe(self, l):
        k = self.k
        inp = self.inp
        hrows = self.agHt.out.t[:, :]
        lrows = self.agL.out.t[:, :]
        ysp = self.ysp[0]
        with k.scope() as st:
            zdeps = {}
            W1 = k.sb(st, "W1", [128, KC, 2 * DEXP], BF16)
            W2 = k.sb(st, "W2", [128, DEXP // 128, D], BF16)
            b1c = k.sb(st, "b1c", [128, 16], F32)
            b2t = k.sb(st, "b2t", [128, D], F32)
            lidx = k.sb(st, "lidx", [128, EPC * NCORE * 2], I32)
            k.dma("sp", lidx[:], inp["idx_L"][:, :], writes=[lidx], sem=lidx)
            xgT = [k.sb(st, "xgT%d" % i, [128, KC, 512], BF16) for i in range(2)]
            xg = [k.sb(st, "xg%d" % i, [128, D], BF16) for i in range(4)]
            Lt = [k.sb(st, "Lt%d" % i, [128, 4, 4], F32) for i in range(2)]
            Li = [k.sb(st, "Li%d" % i, [128, 4, 2], I32) for i in range(2)]
            lin1 = k.sb(st, "lin1", [128, 8, 512], BF16)
            gt = [k.sb(st, "gt%d" % i, [128, 512], F32) for i in range(2)]
            sg = [k.sb(st, "sg%d" % i, [128, 512], F32) for i in range(2)]
            actT = k.sb(st, "actT", [128, 8, 512], BF16)
            ytm = [k.sb(st, "ytm%d" % i, [128, D], F32) for i in range(2)]
            prev = dict(zdeps)
            yi = 0
            npair = NCORE // 2
            pairs = [(0, 1), (4, 5), (2, 3), (6, 7)]
            seq = [(el, pr) for el in range(EPC) for pr in range(npair)]

            def rs_stage1(parity):
                k._merge(self.ysp[0].w, cur)
                k._merge(self.ysp[0].w, prev)
                for i in range(parity, SEQ // 512, 2):
                    k.collective("ReduceScatter", G2, self.ysp[0], self.ysp[1], self.ysp[0].t[i * 512:(i + 1) * 512, :],
                                 self.ysp[1].t[i * 256:(i + 1) * 256, :])

            def prep_gather(idx):
                el, pr = seq[idx]
                lt, li = Lt[idx % 2], Li[idx % 2]
                for b4 in range(4):
                    r, sb = pairs[pr][b4 // 2], b4 % 2
                    col = (el * NCORE + r) * 2 + sb
                    k.gather(lt[:, b4, :], lrows, lidx[:, col:col + 1], reads=[lidx, self.agL.out], writes=[lt], sem=lt)
                k.op("dve", lambda e: e.tensor_copy(out=li[:], in_=lt[:, :, 0:2]), reads=[lt], writes=[li])
                for b4 in range(4):
                    g = xg[b4]
                    k.gather(g[:], hrows, li[:, b4, 0:1], reads=[li, self.agHt.out], writes=[g], sem=g)

            def prep_transpose(idx):
                x = xgT[idx % 2]
                for b4 in range(4):
                    g = xg[b4]
                    for q4 in range(4):
                        pst = self.nps()
                        pv = pst[:, :].bitcast(BF16)
                        for j in range(4):
                            kk = q4 * 4 + j
                            k.op("pe", lambda e, pv=pv, kk=kk, j=j, g=g: e.transpose(
                                out=pv[:, j * 128:(j + 1) * 128], in_=g[:, kk * 128:(kk + 1) * 128], identity=self.identb[:]),
                                 reads=[g, self.identb], writes=[pst])
                        src = pv[:, 0:512].rearrange("p (j t) -> p j t", j=4)
                        dst = x[:, q4 * 4:(q4 + 1) * 4, b4 * 128:(b4 + 1) * 128]
                        if q4 % 2:
                            k.op("act", lambda e, src=src, dst=dst: e.activation(out=dst, in_=src, func=AF.Copy),
                                 reads=[pst], writes=[x])
                        else:
                            k.op("dve", lambda e, src=src, dst=dst: e.tensor_copy(out=dst, in_=src), reads=[pst], writes=[x])

            prep_gather(0)
            prep_transpose(0)
            cur = {}
            for idx, (el, pr) in enumerate(seq):
                if idx == 0:
                    k.dma("pool", W1[:], _fm(inp["w1"][l, el]), writes=[W1], sem=W1)
                    k.dma("pool", W2[:], _fm(inp["w2"][l, el]), writes=[W2], sem=W2)
                    k.dma("sp", b1c[:], inp["b1"][l, el], writes=[b1c], sem=b1c)
                    k.dma("sp", b2t[:], inp["b2row"][l, el:el + 1, :].partition_broadcast(128), writes=[b2t], sem=b2t)
                if pr == 0 and el > 0:
                    prev = cur
                    cur = {}
                last_pair = (pr == npair - 1 and el + 1 < EPC)
                if idx + 1 < len(seq):
                    prep_gather(idx + 1)
                x = xgT[idx % 2]
                lt, li = Lt[idx % 2], Li[idx % 2]
                for m in list(range(8, 16)) + list(range(8)):
                    ps = self.nps()
                    for kk in range(KC):
                        k.op("pe", lambda e, kk=kk, ps=ps, m=m: e.matmul(
                            ps[:, :], lhsT=W1[:, kk, m * 128:(m + 1) * 128], rhs=x[:, kk, :],
                            start=(kk == 0), stop=(kk == KC - 1)), reads=[W1, x], writes=[ps])
                    t = gt[m % 2]
                    k.op("dve", lambda e, ps=ps, m=m, t=t: e.tensor_scalar(
                        out=t[:], in0=ps[:, :], scalar1=b1c[:, m:m + 1], scalar2=7.0, op0=ALU.add, op1=ALU.min),
                         reads=[ps, b1c], writes=[t])
                    if m >= 8:
                        k.op("dve", lambda e, m=m, t=t: e.tensor_scalar(
                            out=lin1[:, m - 8, :], in0=t[:], scalar1=-7.0, scalar2=1.0, op0=ALU.max, op1=ALU.add),
                             reads=[t], writes=[lin1])
                    else:
                        sgm = sg[m % 2]
                        k.op("act", lambda e, t=t, sgm=sgm: e.activation(out=sgm[:], in_=t[:], func=AF.Sigmoid, scale=1.702),
                             reads=[t], writes=[sgm])
                        k.op("pool", lambda e, t=t, sgm=sgm: e.tensor_tensor(out=sgm[:], in0=sgm[:], in1=t[:], op=ALU.mult),
                             reads=[sgm, t], writes=[sgm])
                        k.op("dve", lambda e, m=m, sgm=sgm: e.tensor_tensor(out=actT[:, m, :], in0=sgm[:], in1=lin1[:, m, :],
                                                                            op=ALU.mult), reads=[sgm, lin1], writes=[actT])
                if last_pair:
                    k.dma("pool", W1[:], _fm(inp["w1"][l, el + 1]), writes=[W1], sem=W1)
                    k.dma("sp", b1c[:], inp["b1"][l, el + 1], writes=[b1c], sem=b1c)
                if idx + 1 < len(seq):
                    prep_transpose(idx + 1)
                for b4 in range(4):
                    ys = ytm[yi % 2]
                    yi += 1
                    for ft in range(4):
                        fs = slice(ft * 512, (ft + 1) * 512)
                        ps = self.nps()
                        for kk in range(8):
                            k.op("pe", lambda e, kk=kk, ps=ps, b4=b4, fs=fs: e.matmul(
                                ps[:, :], lhsT=actT[:, kk, b4 * 128:(b4 + 1) * 128], rhs=W2[:, kk, fs],
                                start=(kk == 0), stop=(kk == 7)), reads=[W2, actT], writes=[ps])
                        k.op("dve", lambda e, ps=ps, ys=ys, fs=fs: e.tensor_tensor(out=ys[:, fs], in0=ps[:, :], in1=b2t[:, fs],
                                                                                  op=ALU.add), reads=[ps, b2t], writes=[ys])
                        k.op("act", lambda e, ys=ys, fs=fs, b4=b4, lt=lt: e.activation(
                            out=ys[:, fs], in_=ys[:, fs], func=AF.Identity, scale=lt[:, b4, 3:4]), reads=[ys, lt], writes=[ys])
                    ev = k.scatter_add(ysp.t[:, :], li[:, b4, 1:2], ys[:], reads=[ys, li], sem=ys, extra_deps=prev)
                    k._merge(cur, dict([ev]))
                if last_pair:
                    k.dma("pool", W2[:], _fm(inp["w2"][l, el + 1]), writes=[W2], sem=W2)
                    k.dma("sp", b2t[:], inp["b2row"][l, el + 1:el + 2, :].partition_broadcast(128), writes=[b2t], sem=b2t)
                if el == EPC - 1 and pr == 1:
                    rs_stage1(0)
            rs_stage1(1)
            for j in range(SEQ // 1024):
                k.collective("ReduceScatter", G4, self.ysp[1], self.ysp[2], self.ysp[1].t[j * 512:(j + 1) * 512, :],
                             self.ysp[2].t[j * 128:(j + 1) * 128, :])
        return self.end_phase("moe_%d" % l)

    def phase_post(self, l, hdst):
        k = self.k
        inp = self.inp
        with k.scope() as st:
            z = k.sb(st, "pz", [128, KC, TC], F32)
            k.dma("sp", z[:], _fm(self.h1T_d.t[:, :]), writes=[z], sem=z)
            gcol = k.sb(st, "lng2", [128, KC], F32); bcol = k.sb(st, "lnb2", [128, KC], F32)
            k.dma("sp", gcol[:], inp["ln_ffn_g"][l], writes=[gcol], sem=gcol)
            k.dma("sp", bcol[:], inp["ln_ffn_b"][l], writes=[bcol], sem=bcol)
            yidx = k.sb(st, "yidx", [128, 8], I32)
            k.dma("sp", yidx[:], inp["idx_y"][:, :], writes=[yidx], sem=yidx)
            with k.scope() as st2:
                ytk = [k.sb(st2, "ytk%d" % i, [128, D], F32) for i in range(2)]
                for tk in range(8):
                    y = ytk[tk % 2]
                    k.dma("sp", y[:], self.ysp[2].t[tk * 128:(tk + 1) * 128, :], reads=[self.ysp[2]], writes=[y], sem=y)
                    for q4 in range(4):
                        pst = self.nps()
                        for j in range(4):
                            kk = q4 * 4 + j
                            k.op("pe", lambda e, pst=pst, kk=kk, j=j, y=y: e.transpose(
                                out=pst[:, j * 128:(j + 1) * 128], in_=y[:, kk * 128:(kk + 1) * 128], identity=self.ident[:]),
                                 reads=[y, self.ident], writes=[pst])
                        zv = z[:, q4 * 4:(q4 + 1) * 4, tk * 128:(tk + 1) * 128]
                        k.op("dve", lambda e, pst=pst, zv=zv: e.scalar_tensor_tensor(
                            out=zv, in0=zv, scalar=float(ALPHA), in1=pst[:, :].rearrange("p (j t) -> p j t", j=4),
                            op0=ALU.mult, op1=ALU.add), reads=[z, pst], writes=[z])
            with k.scope() as st2:
                self.norm_fm(st2, z, KC, TC, gcol, bcol, LN_EPS, True, "ln2")
            zb = k.sb(st, "pzb", [128, KC, TC], BF16)
            for kk in range(KC):
                if kk % 2:
                    k.op("pool", lambda e, kk=kk: e.tensor_copy(out=zb[:, kk, :], in_=z[:, kk, :]), reads=[z], writes=[zb])
                else:
                    k.op("act", lambda e, kk=kk: e.activation(out=zb[:, kk, :], in_=z[:, kk, :], func=AF.Copy),
                         reads=[z], writes=[zb])
            pTb = k.sb(st, "pTb", [128, 2, TC], BF16)
            wp = k.sb(st, "wp", [128, 2, D], BF16)
            k.dma("pool", pTb[:], _fm(inp["pT"][l]), writes=[pTb], sem=pTb)
            k.dma("pool", wp[:], _fm(inp["ple_proj"][l]), writes=[wp], sem=wp)
            sgt = [k.sb(st, "psg%d" % i, [128, 512], F32) for i in range(2)]

            def epi(m, mr, t0, nt, ps):
                s = sgt[m % 2]
                k.op("act", lambda e: e.activation(out=s[:, :nt], in_=ps[:, :nt], func=AF.Sigmoid), reads=[ps], writes=[s])
                ps2 = self.nps()
                for kk in range(2):
                    k.op("pe", lambda e, kk=kk: e.matmul(ps2[:, :nt], lhsT=wp[:, kk, m * 128:(m + 1) * 128],
                                                         rhs=pTb[:, kk, t0:t0 + nt], start=(kk == 0), stop=(kk == 1)),
                         reads=[wp, pTb], writes=[ps2])
                k.op("dve", lambda e: e.tensor_tensor(out=s[:, :nt], in0=ps2[:, :nt], in1=s[:, :nt], op=ALU.mult),
                     reads=[ps2, s], writes=[s])
                k.op("pool", lambda e: e.tensor_tensor(out=z[:, m, t0:t0 + nt], in0=z[:, m, t0:t0 + nt], in1=s[:, :nt],
                                                       op=ALU.add), reads=[z, s], writes=[z])

            self.linear_fm(st, zb, KC, TC, inp["ple_gate"][l], D, epi, tag="wpg")
            k.dma("sp", _fm(hdst), z[:], reads=[z], writes=[self.hT_d], sem=z)
        return self.end_phase("post_%d" % l)

    def phase_q(self, j, hsrc, with_kv):
        k = self.k
        inp = self.inp
        with k.scope() as st:
            hTb = k.sb(st, "qhTb", [128, KC, TC], BF16)
            k.dma("pool", hTb[:], _fm(hsrc), writes=[hTb], sem=hTb)
            cq = k.sb(st, "cq", [128, 6, TC], F32)
            qn = k.sb(st, "qn", [128, 6], F32)
            k.dma("sp", qn[:], inp["q_norm"][j], writes=[qn], sem=qn)

            def epi(m, mr, t0, nt, ps):
                k.op("act", lambda e: e.activation(out=cq[:, m, t0:t0 + nt], in_=ps[:, :nt], func=AF.Copy),
                     reads=[ps], writes=[cq])

            with k.scope() as st2:
                self.linear_fm(st2, hTb, KC, TC, inp["w_dq"][j], QLORA, epi, mblk=QLORA, tag="wdq")
            with k.scope() as st2:
                self.norm_fm(st2, cq, 6, TC, qn, None, RMS_EPS, False, "rq")
            cqb = k.sb(st, "cqb", [128, 6, TC], BF16)
            k.op("pool", lambda e: e.tensor_copy(out=cqb[:], in_=cq[:]), reads=[cq], writes=[cqb])
            k.dma("sp", _fm(self.agQ.src.t[:, :]), cqb[:], reads=[cqb], writes=[self.agQ.src], sem=cqb)
            self.agQ.run()
            if with_kv:
                ck = k.sb(st, "ck", [128, 4, TC], F32)
                kn = k.sb(st, "kn", [128, 4], F32)
                k.dma("sp", kn[:], inp["kv_norm"][:, :], writes=[kn], sem=kn)

                def epi2(m, mr, t0, nt, ps):
                    k.op("act", lambda e: e.activation(out=ck[:, m, t0:t0 + nt], in_=ps[:, :nt], func=AF.Copy),
                         reads=[ps], writes=[ck])

                with k.scope() as st2:
                    self.linear_fm(st2, hTb, KC, TC, inp["w_dkv_c"], KVLORA, epi2, tag="wdkv")
                with k.scope() as st2:
                    self.norm_fm(st2, ck, 4, TC, kn, None, RMS_EPS, False, "rk")
                ckb = k.sb(st, "ckb", [128, 4, TC], BF16)
                k.op("pool", lambda e: e.tensor_copy(out=ckb[:], in_=ck[:]), reads=[ck], writes=[ckb])
                k.dma("sp", _fm(self.agKc.src.t[:, :]), ckb[:], reads=[ckb], writes=[self.agKc.src], sem=ckb)
                self.agKc.run()
                tabs = k.sb(st, "ktabs", [64, 2, TC], F32)
                k.dma("sp", tabs[:], self.agT.src.t[:, :].rearrange("(j p) t -> p j t", p=64), writes=[tabs], sem=tabs)
                kpa = k.sb(st, "kpa", [64, TC], F32); kpb = k.sb(st, "kpb", [64, TC], F32)
                krb = k.sb(st, "krb", [64, TC], BF16)

                def epi3(m, mr, t0, nt, ps):
                    k.op("dve", lambda e: e.tensor_tensor(out=kpa[:, t0:t0 + nt], in0=ps[:64, :nt], in1=tabs[:, 0, t0:t0 + nt],
                                                          op=ALU.mult), reads=[ps, tabs], writes=[kpa])

                def epi4(m, mr, t0, nt, ps):
                    k.op("dve", lambda e: e.tensor_tensor(out=kpb[:, t0:t0 + nt], in0=ps[:64, :nt], in1=tabs[:, 1, t0:t0 + nt],
                                                          op=ALU.mult), reads=[ps, tabs], writes=[kpb])

                with k.scope() as st2:
                    self.linear_fm(st2, hTb, KC, TC, inp["w_dkv_pe"], ROPE, epi3, tag="wkpe")
                with k.scope() as st2:
                    self.linear_fm(st2, hTb, KC, TC, inp["w_dkv_pesw"], ROPE, epi4, tag="wkpes")
                k.op("dve", lambda e: e.tensor_tensor(out=krb[:], in0=kpa[:], in1=kpb[:], op=ALU.add),
                     reads=[kpa, kpb], writes=[krb])
                k.dma("sp", self.agKp.src.t[:, :], krb[:], reads=[krb], writes=[self.agKp.src], sem=krb)
                self.agKp.run()
        return self.end_phase("q_%d" % j)

    def phase_attn(self, j):
        k = self.k
        inp = self.inp
        with k.scope() as st:
            KT = k.sb(st, "KT", [128, 2, SEQ], BF16)
            KPE = k.sb(st, "KPE", [64, SEQ], BF16)
            V = k.sb(st, "V", [128, SEQ // 128, 256], BF16)
            wk = k.sb(st, "wk", [128, 4, 256], BF16); wv = k.sb(st, "wv", [128, 4, 256], BF16)
            wqn = k.sb(st, "wqn", [128, 6, 256], BF16)
            wqp = k.sb(st, "wqp", [128, 6, 128], BF16); wqs = k.sb(st, "wqs", [128, 6, 128], BF16)
            k.dma("pool", wk[:], _fm(inp["w_ukv_k"][:, :]), writes=[wk], sem=wk)
            k.dma("pool", wv[:], _fm(inp["w_ukv_v"][:, :]), writes=[wv], sem=wv)
            k.dma("pool", wqn[:], _fm(inp["w_uq_n"][j]), writes=[wqn], sem=wqn)
            k.dma("pool", wqp[:], _fm(inp["w_uq_pe"][j]), writes=[wqp], sem=wqp)
            k.dma("pool", wqs[:], _fm(inp["w_uq_pesw"][j]), writes=[wqs], sem=wqs)
            msk = k.sb(st, "msk", [128, 4, 512], BF16)
            k.dma("pool", msk[:], inp["masks"][:, :, :], writes=[msk], sem=msk)
            with k.scope() as st2:
                cts = [k.sb(st2, "ct%d" % i, [128, 4, 512], BF16) for i in range(2)]
                for tt in range(16):
                    r, half = tt // 2, tt % 2
                    ct = cts[tt % 2]
                    self.load_fm_tile(ct, self.agKc, r, half, KVLORA)
                    k.dma("sp", KPE[:, tt * 512:(tt + 1) * 512], self.agKp.rows(r, 0, ROPE)[:, half * 512:(half + 1) * 512],
                          reads=[self.agKp.out], writes=[KPE], sem=KPE)
                    for hh in range(2):
                        ps = self.nps()
                        for kk in range(4):
                            k.op("pe", lambda e, kk=kk, ps=ps, hh=hh, ct=ct: e.matmul(
                                ps[:, :], lhsT=wk[:, kk, hh * 128:(hh + 1) * 128], rhs=ct[:, kk, :],
                                start=(kk == 0), stop=(kk == 3)), reads=[wk, ct], writes=[ps])
                        k.op("act", lambda e, ps=ps, hh=hh, tt=tt: e.activation(
                            out=KT[:, hh, tt * 512:(tt + 1) * 512], in_=ps[:, :], func=AF.Copy), reads=[ps], writes=[KT])
                    for kb4 in range(4):
                        ps = self.nps()
                        for kk in range(4):
                            k.op("pe", lambda e, kk=kk, ps=ps, kb4=kb4, ct=ct: e.matmul(
                                ps[:, :256], lhsT=ct[:, kk, kb4 * 128:(kb4 + 1) * 128], rhs=wv[:, kk, :],
                                start=(kk == 0), stop=(kk == 3)), reads=[wv, ct], writes=[ps])
                        k.op("dve", lambda e, ps=ps, kb4=kb4, tt=tt: e.tensor_copy(out=V[:, tt * 4 + kb4, :], in_=ps[:, :256]),
                             reads=[ps], writes=[V])
            cqs = [k.sb(st, "cqt%d" % i, [128, 6, 512], BF16) for i in range(2)]
            tbs = [k.sb(st, "tbt%d" % i, [64, 2, 512], F32) for i in range(2)]
            qnb = [k.sb(st, "qnb%d" % i, [128, 512], BF16) for i in range(2)]
            qpb = [k.sb(st, "qpb%d" % i, [64, 512], BF16) for i in range(2)]
            r1 = [k.sb(st, "r1_%d" % i, [64, 512], F32) for i in range(2)]
            r2 = [k.sb(st, "r2_%d" % i, [64, 512], F32) for i in range(2)]
            PT = [k.sb(st, "PT%d" % i, [128, 512], BF16) for i in range(3)]
            rl = k.sb(st, "rl", [128, 512], F32)
            acc = [k.sb(st, "acc%d" % i, [128, 512], F32) for i in range(2)]
            ob = [k.sb(st, "ob%d" % i, [128, 512], BF16) for i in range(2)]
            psS = [self.ps[0], self.ps[1]]
            psO = [self.ps[2], self.ps[3]]
            psL = [self.ps[4], self.ps[5]]
            psQ = [self.ps[6], self.ps[7]]
            pti = 0
            si = 0

            def loadq(qt):
                r, half = qt // 2, qt % 2
                c = cqs[qt % 2]
                self.load_fm_tile(c, self.agQ, r, half, QLORA)
                t = tbs[qt % 2]
                k.dma("sp", t[:], self.agT.rows(r, 0, 128)[:, half * 512:(half + 1) * 512].rearrange(
                    "(j p) t -> p j t", p=64), reads=[self.agT.out], writes=[t], sem=t)

            loadq(0)
            for qt in range(16):
                if qt + 1 < 16:
                    loadq(qt + 1)
                c = cqs[qt % 2]
                tb = tbs[qt % 2]
                for hh in range(2):
                    qn_, qp_ = qnb[hh], qpb[hh]
                    ps = psQ[0]
                    for kk in range(6):
                        k.op("pe", lambda e, kk=kk, ps=ps: e.matmul(ps[:, :], lhsT=wqn[:, kk, hh * 128:(hh + 1) * 128],
                                                                    rhs=c[:, kk, :], start=(kk == 0), stop=(kk == 5)),
                             reads=[wqn, c], writes=[ps])
                    k.op("act", lambda e, ps=ps: e.activation(out=qn_[:], in_=ps[:, :], func=AF.Copy), reads=[ps], writes=[qn_])
                    ps = psQ[1]
                    for kk in range(6):
                        k.op("pe", lambda e, kk=kk, ps=ps: e.matmul(ps[:64, :], lhsT=wqp[:, kk, hh * 64:(hh + 1) * 64],
                                                                    rhs=c[:, kk, :], start=(kk == 0), stop=(kk == 5)),
                             reads=[wqp, c], writes=[ps])
                    k.op("dve", lambda e, ps=ps: e.tensor_tensor(out=r1[hh][:], in0=ps[:64, :], in1=tb[:, 0, :], op=ALU.mult),
                         reads=[ps, tb], writes=[r1[hh]])
                    ps = psQ[0]
                    for kk in range(6):
                        k.op("pe", lambda e, kk=kk, ps=ps: e.matmul(ps[:64, :], lhsT=wqs[:, kk, hh * 64:(hh + 1) * 64],
                                                                    rhs=c[:, kk, :], start=(kk == 0), stop=(kk == 5)),
                             reads=[wqs, c], writes=[ps])
                    k.op("dve", lambda e, ps=ps: e.tensor_tensor(out=r2[hh][:], in0=ps[:64, :], in1=tb[:, 1, :], op=ALU.mult),
                         reads=[ps, tb], writes=[r2[hh]])
                    k.op("dve", lambda e: e.tensor_tensor(out=qp_[:], in0=r1[hh][:], in1=r2[hh][:], op=ALU.add),
                         reads=[r1[hh], r2[hh]], writes=[qp_])
                    po, pl = psO[hh], psL[hh]
                    nkb = 4 * qt + 4
                    def qk(kb):
                        pss = psS[kb % 2]
                        k.op("pe", lambda e: e.matmul(pss[:, :], lhsT=KT[:, hh, kb * 128:(kb + 1) * 128],
                                                      rhs=qn_[:], start=True, stop=False),
                             reads=[KT, qn_], writes=[pss])
                        k.op("pe", lambda e: e.matmul(pss[:, :], lhsT=KPE[:, kb * 128:(kb + 1) * 128],
                                                      rhs=qp_[:], start=False, stop=True),
                             reads=[KPE, qp_], writes=[pss])

                    qk(0)
                    for kb in range(nkb):
                        if kb + 1 < nkb:
                            qk(kb + 1)
                        pss = psS[kb % 2]
                        p = PT[pti % 3]; pti += 1
                        k.op("act", lambda e, pss=pss, p=p: e.activation(out=p[:], in_=pss[:, :], func=AF.Exp,
                                                                         scale=float(ATTN_SCALE)), reads=[pss], writes=[p])
                        if kb >= 4 * qt:
                            jm = kb - 4 * qt
                            k.op("dve", lambda e, p=p, jm=jm: e.tensor_tensor(out=p[:], in0=p[:], in1=msk[:, jm, :],
                                                                              op=ALU.mult), reads=[p, msk], writes=[p])
                        k.op("pe", lambda e, p=p, kb=kb: e.matmul(po[:, :], lhsT=V[:, kb, hh * 128:(hh + 1) * 128], rhs=p[:],
                                                                  start=(kb == 0), stop=(kb == nkb - 1)),
                             reads=[V, p], writes=[po])
                        if kb == 0:
                            k.op("dve", lambda e, p=p: e.tensor_copy(out=acc[hh][:], in_=p[:]), reads=[p], writes=[acc[hh]])
                        else:
                            k.op("dve", lambda e, p=p: e.tensor_tensor(out=acc[hh][:], in0=acc[hh][:], in1=p[:], op=ALU.add),
                                 reads=[acc[hh], p], writes=[acc[hh]])
                    k.op("pe", lambda e: e.matmul(pl[:, :], lhsT=self.ones32[:], rhs=acc[hh][:], start=True, stop=True),
                         reads=[self.ones32, acc[hh]], writes=[pl])
                    k.op("dve", lambda e: e.reciprocal(out=rl[:], in_=pl[:, :]), reads=[pl], writes=[rl])
                    o = ob[hh]
                    k.op("dve", lambda e, o=o: e.tensor_tensor(out=o[:], in0=po[:, :], in1=rl[:], op=ALU.mult),
                         reads=[po, rl], writes=[o])
                    jb = qt // 2
                    k.dma("sp", self.agF.src.t[jb * 256 + hh * 128:jb * 256 + (hh + 1) * 128, (qt % 2) * 512:(qt % 2 + 1) * 512],
                          o[:], reads=[o], writes=[self.agF.src], sem=o)
                if qt % 4 == 3:
                    self.agF.run_chunk(qt // 4)
        return self.end_phase("attn_%d" % j)

    def phase_attn_out(self, l, j, hsrc):
        k = self.k
        with k.scope() as st:
            oT = k.sb(st, "oT", [128, KC, TC], BF16)
            rows = self.agF.out.t[:, :]
            for kk in range(KC):
                k.gather(oT[:, kk, :], rows, self.tF[:, kk:kk + 1], reads=[self.tF, self.agF.out], writes=[oT], sem=oT)
            self.mix_and_tail(l, st, oT, self.inp["w_o"][j], hsrc, "ao")
        return self.end_phase("ao_%d" % l)

    def build(self):
        nc = self.nc
        self.declare()
        self.scratch()
        k = self.k
        with contextlib.ExitStack() as cst:
            self.consts(cst)
            self._run()
            if self.stopped or self.dumps:
                self._dump()
            k.barrier()
        return nc

    def _run(self):
        if self.phase_init():
            return
        for l in range(DEPTH):
            hsrc = self.inp["xT"][:, :] if l == 0 else self.hT_d.t[:, :]
            hdst = self.outT[:, :] if l == DEPTH - 1 else self.hT_d.t[:, :]
            if l < N_A:
                if self.phase_rg1(l, hsrc):
                    return
                if self.phase_rg2(l):
                    return
                if self.phase_rg3(l, hsrc):
                    return
            else:
                j = l - N_A
                if self.phase_q(j, hsrc, j == 0):
                    return
                if self.phase_attn(j):
                    return
                if self.phase_attn_out(l, j, hsrc):
                    return
            if self.phase_moe(l):
                return
            if self.phase_post(l, hdst):
                return

    def _dump(self):
        k = self.k
        allb = {}
        for nm in ("hT_d", "h1T_d", "gb_d", "dbg_d", "dbg2_d"):
            allb[nm] = getattr(self, nm)
        for grp in ("agH", "agF", "agG", "agKc", "agKp", "agQ", "agT"):
            a = getattr(self, grp)
            for b in (a.src, a.mid, a.out):
                allb[b.name] = b
        for b in self.ym + self.ysp:
            allb[b.name] = b
        for a in (self.agHt, self.agL):
            for b in (a.src, a.mid, a.out):
                allb[b.name] = b
        for nm in self.dumps:
            b = allb[nm]
            shape = list(b.t.shape)
            o = self.nc.dram_tensor("dump_" + nm, shape, b.t.dtype, kind="ExternalOutput")
            ob = Buf(o, "dump_" + nm)
            rows = shape[0]
            step = max(1, rows // 8)
            for r0 in range(0, rows, step):
                k.dma("sp", o[r0:r0 + step, :], b.t[r0:r0 + step, :], reads=[b], writes=[ob], sem=ob)


def _cols(v):
    v = np.asarray(v, np.float32)
    lead = v.shape[:-1]
    n = v.shape[-1] // 128
    return np.ascontiguousarray(np.swapaxes(v.reshape(*lead, n, 128), -1, -2))


def make_in_maps(inp, used=None):
    f = lambda a: np.ascontiguousarray(np.asarray(a, np.float32))
    swap = np.concatenate([np.arange(32, 64), np.arange(0, 32)])
    pp = np.arange(128)[:, None]
    cache = {}

    def get(name):
        if name not in cache:
            cache[name] = np.asarray(inp[name])
        return cache[name]

    def ropec():
        inv = (1.0 / (10000.0 ** (np.arange(0, ROPE, 2, dtype=np.float32) / ROPE))).astype(np.float32)
        r = np.zeros((64, 2), np.float32)
        r[:, 0] = np.concatenate([inv, inv])
        r[:32, 1] = -1.0
        r[32:, 1] = 1.0
        return r

    def masks():
        qq = np.arange(512)[None, :]
        return f(np.stack([(pp + 128 * jm <= qq).astype(np.float32) for jm in range(4)], axis=1))

    def w_uq():
        return get("mla_w_uq").reshape(2, QLORA, 16, 192)

    def w_ukv():
        return get("kv_w_ukv").reshape(KVLORA, 16, 256)

    common = {
        "ln_mix_g": lambda: _cols(get("ln_mix_g")), "ln_mix_b": lambda: _cols(get("ln_mix_b")),
        "ln_ffn_g": lambda: _cols(get("ln_ffn_g")), "ln_ffn_b": lambda: _cols(get("ln_ffn_b")),
        "w_in_g": lambda: f(get("rg_w_in")[:, :, :D]), "w_out": lambda: f(get("rg_w_out")),
        "router_w": lambda: f(get("moe_router_w")),
        "router_b": lambda: f(np.broadcast_to(get("moe_router_b")[:, None, :], (DEPTH, 128, NEXP))),
        "ple_proj": lambda: f(get("ple_w_proj")), "ple_gate": lambda: f(get("ple_w_gate")),
        "w_dq": lambda: f(get("mla_w_dq")), "q_norm": lambda: _cols(get("mla_q_norm")), "w_o": lambda: f(get("mla_w_o")),
        "w_dkv_c": lambda: f(get("kv_w_dkv")[:, :KVLORA]), "w_dkv_pe": lambda: f(get("kv_w_dkv")[:, KVLORA:]),
        "w_dkv_pesw": lambda: f(get("kv_w_dkv")[:, KVLORA:][:, swap]),
        "kv_norm": lambda: _cols(get("kv_norm")),
        "ropec": ropec, "masks": masks, "ident": lambda: np.eye(128, dtype=np.float32),
        "ltri": lambda: np.triu(np.ones((128, 128), np.float32), 1),
        "iota": lambda: f(np.broadcast_to(np.arange(CAP, dtype=np.float32)[None, :], (128, CAP))),
        "dumprow": lambda: dumprow(),
    }

    def idx_t1(c):
        kk = np.arange(KC)[None, :]
        return ((kk * 128 + pp) * 8 + c).astype(np.int32)

    def idx_tF(c):
        ch, pq = _ag_layout(8 * 256, TC, 2)
        t = np.zeros((128, KC), np.int32)
        for kk in range(KC):
            for q in range(128):
                f = kk * 128 + q
                t[q, kk] = _ag_rowoff(ch, pq, f // 256, c * 256 + f % 256)
        return t

    def idx_L(c):
        t = np.zeros((128, EPC * NCORE * 2), np.int32)
        p = np.arange(128)
        for el in range(EPC):
            for r in range(NCORE):
                for sb in range(2):
                    t[:, (el * NCORE + r) * 2 + sb] = r * (2 * 128 * NEXP) + (sb * 128 + p) * NEXP + (c * EPC + el)
        return t

    def vals3(c):
        ch, pq = _ag_layout(TC, D, 2)
        v = np.zeros((128, 8, 3), np.float32)
        for tk in range(8):
            for q in range(128):
                v[q, tk, 0] = _ag_rowoff(ch, pq, c, tk * 128 + q)
                v[q, tk, 1] = _ysp_row(c, tk, q)
        v[:, :, 2] = 1.0
        return v

    def idx_y(c):
        return (c * TC + np.arange(8)[None, :] * 128 + pp).astype(np.int32)

    def dumprow():
        return (SEQ + np.arange(2)[None, :] * 128 + pp).astype(np.float32)

    def idx_g(c):
        ig = np.zeros((128, EPC * 16), np.int32)
        for el in range(EPC):
            for tt in range(16):
                r, half = tt // 2, tt % 2
                ig[:, el * 16 + tt] = (r * NEXP + c * EPC + el) * 2 + half
        return ig

    ts = lambda c: slice(c * TC, (c + 1) * TC)
    cs = lambda c: slice(c * 256, (c + 1) * 256)
    es = lambda c: slice(c * EPC, (c + 1) * EPC)
    percore = {
        "xT": lambda c: f(get("x")[0, ts(c)].T),
        "pT": lambda c: f(np.swapaxes(get("p")[:, 0, ts(c)], 1, 2)),
        "pos": lambda c: np.ascontiguousarray(get("positions")[:, ts(c)].astype(np.int32)),
        "w_in_r": lambda c: f(get("rg_w_in")[:, :, D + c * 256:D + (c + 1) * 256]),
        "conv_w": lambda c: f(np.transpose(get("rg_conv_w")[:, :, cs(c)].reshape(N_A, 4, 2, 128), (0, 3, 2, 1))),
        "conv_b": lambda c: _cols(get("rg_conv_b")[:, cs(c)]),
        "ga_w": lambda c: f(get("rg_gate_a_w")[:, c]), "gx_w": lambda c: f(get("rg_gate_x_w")[:, c]),
        "ga_b": lambda c: _cols(get("rg_gate_a_b")[:, c]), "gx_b": lambda c: _cols(get("rg_gate_x_b")[:, c]),
        "lam": lambda c: _cols(get("rg_lambda")[:, cs(c)]),
        "w1": lambda c: f(get("moe_w1")[:, es(c)]), "b1": lambda c: _cols(get("moe_b1")[:, es(c)]),
        "w2": lambda c: f(get("moe_w2")[:, es(c)]), "b2": lambda c: _cols(get("moe_b2")[:, es(c)]),
        "w_uq_n": lambda c: f(w_uq()[:, :, 2 * c:2 * c + 2, :128].reshape(2, QLORA, 256)),
        "w_uq_pe": lambda c: f(w_uq()[:, :, 2 * c:2 * c + 2, 128:].reshape(2, QLORA, 128)),
        "w_uq_pesw": lambda c: f(w_uq()[:, :, 2 * c:2 * c + 2, 128:][..., swap].reshape(2, QLORA, 128)),
        "w_ukv_k": lambda c: f(w_ukv()[:, 2 * c:2 * c + 2, :128].reshape(KVLORA, 256)),
        "w_ukv_v": lambda c: f(w_ukv()[:, 2 * c:2 * c + 2, 128:].reshape(KVLORA, 256)),
        "idx_t1": idx_t1, "idx_tF": idx_tF, "idx_g": idx_g, "idx_L": idx_L, "idx_y": idx_y, "vals3": vals3,
        "b2row": lambda c: f(get("moe_b2")[:, es(c)]),
    }
    names = list(common) + list(percore) if used is None else list(used)
    shared = {n: common[n]() for n in names if n in common}
    maps = []
    for c in range(NCORE):
        m = dict(shared)
        for n in names:
            if n in percore:
                m[n] = percore[n](c)
        maps.append(m)
    return maps


def build_program(stop_after=None, dumps=(), debug=False):
    nc = bass.Bass("TRN2", target_bir_lowering=False)
    prog = Prog(nc, stop_after=stop_after, dumps=dumps, debug=debug)
    with nc.allow_low_precision("bf16 matmul operands, fp32 accumulation (reference tolerance is bf16-level)"):
        prog.build()
    return nc, prog


def kernel(**inputs):
    nc, prog = build_program()
    maps = make_in_maps(inputs, used=sorted(prog.inp.keys()))
    res = run_bass_kernel_spmd(nc, maps, core_ids=list(range(NCORE)))
    out = np.concatenate([np.asarray(r["outT"], np.float32).T for r in res.results], axis=0)
    return np.ascontiguousarray(out[None]).astype(np.float32)
```

```python
import contextlib
import math
import numpy as np
import concourse.bass as bass
import concourse.mybir as mybir
from concourse.bass_utils import run_bass_kernel_spmd

F32 = mybir.dt.float32
BF16 = mybir.dt.bfloat16
I32 = mybir.dt.int32
AF = mybir.ActivationFunctionType
ALU = mybir.AluOpType
AX = mybir.AxisListType

NCORE = 8
D = 2048
KC = D // 128
SEQ = 8192
TC = SEQ // NCORE
DEPTH = 4
N_A = 2
ALPHA = (2 * DEPTH) ** 0.25
LN_EPS = 1e-5
RMS_EPS = 1e-6
NEXP = 32
EPC = NEXP // NCORE
CAP = 256
NSLOT = NCORE * CAP
DEXP = 1024
QLORA = 768
KVLORA = 512
ROPE = 64
ATTN_SCALE = (128 + 64) ** -0.5
G4 = [[0, 1, 2, 3], [4, 5, 6, 7]]
G2 = [[0, 4], [1, 5], [2, 6], [3, 7]]


class Buf:
    def __init__(self, t, name):
        self.t = t
        self.name = name
        self.w = {}
        self.r = {}
        self.dsem = None

    def __getitem__(self, idx):
        return self.t[idx]


class Ker:
    NDSEM = 48

    def __init__(self, nc):
        self.nc = nc
        self.es = contextlib.ExitStack()
        self.eng = dict(pe=nc.tensor, dve=nc.vector, act=nc.scalar, pool=nc.gpsimd, sp=nc.sync)
        self.semh = {}
        self.semcur = {}
        self.waited = {}
        for e in ("pe", "dve", "act", "pool", "cc"):
            self._mksem("s_" + e)
        self.free_dsem = []
        for i in range(self.NDSEM):
            self._mksem("d%d" % i)
            self.free_dsem.append("d%d" % i)
        self.phase_dsem = []
        self.phase_bufs = []
        self.freed = {}
        self.ninst = 0

    def _mksem(self, name):
        self.semh[name] = self.es.enter_context(self.nc.semaphore(name))
        self.semcur[name] = 0

    def sb(self, st, name, shape, dt):
        self.nsb = getattr(self, "nsb", 0) + 1
        b = Buf(st.enter_context(self.nc.sbuf_tensor("sb%d_%s" % (self.nsb, name), list(shape), dt)), name)
        b.w = dict(self.freed)
        if hasattr(st, "bufs"):
            st.bufs.append(b)
        self.phase_bufs.append(b)
        return b

    @contextlib.contextmanager
    def scope(self):
        with contextlib.ExitStack() as st:
            st.bufs = []
            yield st
            for b in st.bufs:
                self._merge(self.freed, b.w)
                self._merge(self.freed, b.r)

    def dram(self, name, shape, dt):
        return Buf(self.nc.dram_tensor(name, list(shape), dt), name)

    def _dsem(self, b):
        if b.dsem is None:
            b.dsem = self.free_dsem.pop()
            self.phase_dsem.append(b)
        return b.dsem

    def _wait(self, e, deps):
        for s, v in deps.items():
            if e == "pe" and s == "s_pe":
                continue
            if self.waited.get((e, s), 0) < v:
                self.eng[e].wait_ge(self.semh[s], v)
                self.waited[(e, s)] = v
                self.ninst += 1

    @staticmethod
    def _merge(d, o):
        for s, v in o.items():
            if d.get(s, 0) < v:
                d[s] = v

    def _deps(self, reads, writes):
        deps = {}
        for b in reads:
            self._merge(deps, b.w)
        for b in writes:
            self._merge(deps, b.w)
            self._merge(deps, b.r)
        return deps

    def _commit(self, ev, reads, writes):
        s, v = ev
        for b in reads:
            if b.r.get(s, 0) < v:
                b.r[s] = v
        for b in writes:
            if b.w.get(s, 0) < v:
                b.w[s] = v
            b.r = {}

    def op(self, e, fn, reads=(), writes=()):
        self._wait(e, self._deps(reads, writes))
        ins = fn(self.eng[e])
        s = "s_" + e
        self.semcur[s] += 1
        ins.then_inc(self.semh[s], 1)
        self._commit((s, self.semcur[s]), reads, writes)
        self.ninst += 1

    def dma(self, q, out, in_, reads=(), writes=(), sem=None, **kw):
        self._wait(q, self._deps(reads, writes))
        s = self._dsem(sem)
        ins = self.eng[q].dma_start(out=out, in_=in_, **kw)
        self.semcur[s] += 16
        ins.then_inc(self.semh[s], 16)
        self._commit((s, self.semcur[s]), reads, writes)
        self.ninst += 1

    def gather(self, out, src_rows, idx_ap, reads=(), writes=(), sem=None):
        self._wait("pool", self._deps(reads, writes))
        s = self._dsem(sem)
        ins = self.nc.gpsimd.indirect_dma_start(
            out=out, out_offset=None, in_=src_rows,
            in_offset=bass.IndirectOffsetOnAxis(ap=idx_ap, axis=0))
        self.semcur[s] += 16
        ins.then_inc(self.semh[s], 16)
        self._commit((s, self.semcur[s]), reads, writes)
        self.ninst += 1

    def scatter_add(self, dst_rows, idx_ap, src, reads=(), writes=(), sem=None, extra_deps=None):
        deps = self._deps(reads, ())
        if extra_deps:
            self._merge(deps, extra_deps)
        self._wait("pool", deps)
        s = self._dsem(sem)
        ins = self.nc.gpsimd.indirect_dma_start(
            out=dst_rows, out_offset=bass.IndirectOffsetOnAxis(ap=idx_ap, axis=0), in_=src, in_offset=None,
            compute_op=ALU.add)
        self.semcur[s] += 16
        ins.then_inc(self.semh[s], 16)
        ev = (s, self.semcur[s])
        self._commit(ev, reads, ())
        self.ninst += 1
        return ev

    def collective(self, kind, groups, src, dst, src_ap, dst_ap):
        self._wait("pool", self._deps([src], [dst]))
        op = ALU.add if kind in ("AllReduce", "ReduceScatter") else ALU.bypass
        ins = self.nc.gpsimd.collective_compute(kind, op, replica_groups=groups, ins=[src_ap], outs=[dst_ap])
        self.semcur["s_cc"] += 1
        ins.then_inc(self.semh["s_cc"], 1)
        self._commit(("s_cc", self.semcur["s_cc"]), [src], [dst])
        self.ninst += 1

    def barrier(self):
        for e in ("pe", "dve", "act", "pool", "sp"):
            for s, v in self.semcur.items():
                if v > 0 and self.waited.get((e, s), 0) < v:
                    if e == "pe" and s == "s_pe":
                        continue
                    self.eng[e].wait_ge(self.semh[s], v)
                    self.waited[(e, s)] = v
                    self.ninst += 1
        for b in self.phase_dsem:
            self.free_dsem.append(b.dsem)
            b.dsem = None
        self.phase_dsem = []
        self.phase_bufs = []
        self.freed = {}


class _LazyInputs(dict):
    def __init__(self, prog):
        super().__init__()
        self.prog = prog

    def __missing__(self, name):
        shape, dt = self.prog.shapes[name]
        t = self.prog.nc.dram_tensor(name, list(shape), dt, kind="ExternalInput")
        self[name] = t
        return t


def _fm(ap, p=128):
    return ap.rearrange("(k p) n -> p k n", p=p)


CC_MAX = 1 << 20


def _ag_layout(R, C, es):
    ch = R
    while ch * C * es > CC_MAX:
        assert ch % 2 == 0
        ch //= 2
    pq = 4
    while pq * ch * C * es > 2 * CC_MAX:
        pq //= 2
    return ch, pq


def _ysp_row(c, tk, m):
    g, q = c // 4, c % 4
    return (2 * tk + q // 2) * 512 + g * 256 + (q % 2) * 128 + m


def _ag_rowoff(ch, pq, r, rho):
    i, rp = rho // ch, rho % ch
    g, q = r // 4, r % 4
    hq, ql = q // pq, q % pq
    return ((((i * (4 // pq) + hq) * 2 + g) * pq + ql) * ch + rp)


class AG:
    def __init__(self, k, name, R, C, dt, es):
        self.k = k
        self.R, self.C = R, C
        self.ch, self.pq = _ag_layout(R, C, es)
        self.src = k.dram(name + "_in", [R, C], dt)
        self.mid = k.dram(name + "_mid", [4 * R, C], dt)
        self.out = k.dram(name + "_out", [8 * R, C], dt)

    def rowoff(self, r, rho):
        return _ag_rowoff(self.ch, self.pq, r, rho)

    def rows(self, r, rho, n):
        assert rho // self.ch == (rho + n - 1) // self.ch
        o = self.rowoff(r, rho)
        return self.out.t[o:o + n, :]

    @property
    def nchunk(self):
        return self.R // self.ch

    def run_chunk(self, i):
        k = self.k
        ch, pq = self.ch, self.pq
        k.collective("AllGather", G4, self.src, self.mid, self.src.t[i * ch:(i + 1) * ch, :],
                     self.mid.t[i * 4 * ch:(i + 1) * 4 * ch, :])
        for hq in range(4 // pq):
            a = (i * 4 + hq * pq) * ch
            b = ((i * (4 // pq) + hq) * 2) * pq * ch
            k.collective("AllGather", G2, self.mid, self.out, self.mid.t[a:a + pq * ch, :],
                         self.out.t[b:b + 2 * pq * ch, :])

    def run(self):
        for i in range(self.nchunk):
            self.run_chunk(i)


class Prog:
    def __init__(self, nc, stop_after=None, dumps=(), debug=False):
        self.nc = nc
        self.debug = debug
        self.k = Ker(nc)
        self.stop_after = stop_after
        self.dumps = list(dumps)
        self.stopped = False
        self.inp = _LazyInputs(self)
        self.out_dumps = {}

    def din(self, name, shape, dt=F32):
        t = self.nc.dram_tensor(name, list(shape), dt, kind="ExternalInput")
        self.inp[name] = t
        return t

    def declare(self):
        self.shapes = {}

        def d(name, shape, dt=F32):
            self.shapes[name] = (shape, dt)
        d("xT", [D, TC]); d("pT", [DEPTH, 256, TC]); d("pos", [1, TC], I32)
        d("ln_mix_g", [DEPTH, 128, KC]); d("ln_mix_b", [DEPTH, 128, KC])
        d("ln_ffn_g", [DEPTH, 128, KC]); d("ln_ffn_b", [DEPTH, 128, KC])
        d("w_in_g", [N_A, D, D]); d("w_in_r", [N_A, D, 256]); d("w_out", [N_A, D, D])
        d("conv_w", [N_A, 128, 2, 4]); d("conv_b", [N_A, 128, 2])
        d("ga_w", [N_A, 256, 256]); d("ga_b", [N_A, 128, 2]); d("gx_w", [N_A, 256, 256]); d("gx_b", [N_A, 128, 2])
        d("lam", [N_A, 128, 2])
        d("router_w", [DEPTH, D, NEXP]); d("router_b", [DEPTH, 128, NEXP])
        d("w1", [DEPTH, EPC, D, 2 * DEXP]); d("b1", [DEPTH, EPC, 128, 16])
        d("w2", [DEPTH, EPC, DEXP, D]); d("b2", [DEPTH, EPC, 128, 16])
        d("ple_proj", [DEPTH, 256, D]); d("ple_gate", [DEPTH, D, D])
        d("w_dq", [2, D, QLORA]); d("q_norm", [2, 128, 6]); d("w_o", [2, D, D])
        d("w_uq_n", [2, QLORA, 256]); d("w_uq_pe", [2, QLORA, 128]); d("w_uq_pesw", [2, QLORA, 128])
        d("w_dkv_c", [D, KVLORA]); d("w_dkv_pe", [D, ROPE]); d("w_dkv_pesw", [D, ROPE]); d("kv_norm", [128, 4])
        d("w_ukv_k", [KVLORA, 256]); d("w_ukv_v", [KVLORA, 256])
        d("ropec", [64, 2]); d("masks", [128, 4, 512]); d("ident", [128, 128])
        d("idx_t1", [128, KC], I32); d("idx_tF", [128, KC], I32)
        d("ltri", [128, 128]); d("iota", [128, CAP]); d("vals4", [128, 8, NEXP, 4]); d("dumprow", [128, 2])
        d("idx_L", [128, EPC * NCORE * 2], I32); d("idx_y", [128, 8], I32); d("b2row", [DEPTH, EPC, D]); d("idx_g", [128, EPC * 16], I32)
        self.outT = self.nc.dram_tensor("outT", [D, TC], F32, kind="ExternalOutput")

    def scratch(self):
        k = self.k
        self.hT_d = k.dram("hT_d", [D, TC], F32)
        self.h1T_d = k.dram("h1T_d", [D, TC], F32)
        self.gb_d = k.dram("gb_d", [D, TC], BF16)
        self.agH = AG(k, "agH", D, TC, BF16, 2)
        self.agF = AG(k, "agF", 8 * 256, TC, BF16, 2)
        self.agG = AG(k, "agG", NEXP, TC, F32, 4)
        self.agKc = AG(k, "agKc", KVLORA, TC, BF16, 2)
        self.agKp = AG(k, "agKp", ROPE, TC, BF16, 2)
        self.agQ = AG(k, "agQ", QLORA, TC, BF16, 2)
        self.agT = AG(k, "agT", 128, TC, F32, 4)
        self.agHt = AG(k, "agHt", TC, D, BF16, 2)
        self.agL = AG(k, "agL", 2 * 128 * NEXP, 4, F32, 4)
        self.ysp = [k.dram("ysp%d" % i, [SEQ + CAP, D], F32) for i in range(3)]
        self.dbg_d = k.dram("dbg_d", [D, TC], F32)
        self.dbg2_d = k.dram("dbg2_d", [D, TC], BF16)
        self.ym = [k.dram("ym0", [D, SEQ], F32), k.dram("ym1", [D, SEQ], F32), k.dram("ym2", [D, SEQ], F32)]

    def end_phase(self, name):
        self.k.barrier()
        if self.stop_after == name:
            self.stopped = True
        return self.stopped

    def consts(self, st):
        k = self.k
        self.ps = []
        for i in range(8):
            self.ps.append(Buf(st.enter_context(self.nc.psum_tensor("ps%d" % i, [128, 512], F32)), "ps%d" % i))
        self.ones32 = k.sb(st, "ones32", [128, 128], F32)
        self.onesb = k.sb(st, "onesb", [128, 128], BF16)
        self.ident = k.sb(st, "ident", [128, 128], F32)
        self.t1 = k.sb(st, "t1", [128, KC], I32)
        k.op("dve", lambda e: e.memset(self.ones32[:], 1.0), writes=[self.ones32])
        k.op("dve", lambda e: e.memset(self.onesb[:], 1.0), writes=[self.onesb])
        k.dma("sp", self.ident[:], self.inp["ident"][:, :], writes=[self.ident], sem=self.ident)
        k.dma("sp", self.t1[:], self.inp["idx_t1"][:, :], writes=[self.t1], sem=self.t1)
        self.identb = k.sb(st, "identb", [128, 128], BF16)
        k.op("dve", lambda e: e.tensor_copy(out=self.identb[:], in_=self.ident[:]), reads=[self.ident], writes=[self.identb])
        self.tF = k.sb(st, "tF", [128, KC], I32)
        k.dma("sp", self.tF[:], self.inp["idx_tF"][:, :], writes=[self.tF], sem=self.tF)
        self.psi = 0

    def load_fm_tile(self, x, ag, r, half, nrows):
        k = self.k
        ch = ag.ch
        for r0 in range(0, nrows, ch):
            n = min(ch, nrows - r0)
            k.dma("sp", x[:, r0 // 128:(r0 + n) // 128, :], _fm(ag.rows(r, r0, n)[:, half * 512:(half + 1) * 512]),
                  reads=[ag.out], writes=[x], sem=x)

    def nps(self):
        p = self.ps[self.psi % 8]
        self.psi += 1
        return p

    def linear_fm(self, st, xT, kc, ntok, wsrc, M, epi, mblk=512, tile=512, tag="w", on_load=None):
        k = self.k
        mblk = min(mblk, M)
        nblk = (M + mblk - 1) // mblk
        ws = [k.sb(st, "%s_s%d" % (tag, i), [128, kc, mblk], BF16) for i in range(min(2, nblk))]

        def load(bi):
            w = ws[bi % len(ws)]
            m0 = bi * mblk
            mw = min(mblk, M - m0)
            k.dma("pool", w[:, :, :mw], _fm(wsrc[:, m0:m0 + mw]), writes=[w], sem=w)
            if on_load is not None:
                on_load(bi)

        load(0)
        for bi in range(nblk):
            if bi + 1 < nblk:
                load(bi + 1)
            w = ws[bi % len(ws)]
            m0 = bi * mblk
            mw = min(mblk, M - m0)
            for mi in range((mw + 127) // 128):
                mr = min(128, mw - mi * 128)
                for t0 in range(0, ntok, tile):
                    ps = self.nps()
                    for kk in range(kc):
                        k.op("pe", lambda e, kk=kk, ps=ps, w=w, mi=mi, mr=mr, t0=t0: e.matmul(
                            ps[:mr, :tile], lhsT=w[:, kk, mi * 128:mi * 128 + mr], rhs=xT[:, kk, t0:t0 + tile],
                            start=(kk == 0), stop=(kk == kc - 1)), reads=[w, xT], writes=[ps])
                    epi((m0 // 128) + mi, mr, t0, tile, ps)

    def norm_fm(self, st, z, kc, ntok, gcol, bcol, eps, center, tag):
        k = self.k
        nfeat = kc * 128
        sq = [k.sb(st, "%s_sq%d" % (tag, i), [128, 512], F32) for i in range(2)]
        mean = k.sb(st, tag + "_mean", [128, 512], F32)
        rstd = k.sb(st, tag + "_rstd", [128, 512], F32)
        tmp = [k.sb(st, "%s_tmp%d" % (tag, i), [128, 512], F32) for i in range(2)]
        for t0 in range(0, ntok, 512):
            sl = slice(t0, t0 + 512)
            ps_s = self.nps()
            ps_m = self.nps() if center else None
            for kk in range(kc):
                s = sq[kk % 2]
                k.op("act", lambda e, s=s, kk=kk: e.activation(out=s[:], in_=z[:, kk, sl], func=AF.Square),
                     reads=[z], writes=[s])
                k.op("pe", lambda e, s=s, kk=kk: e.matmul(ps_s[:, :], lhsT=self.ones32[:], rhs=s[:],
                                                          start=(kk == 0), stop=(kk == kc - 1)),
                     reads=[self.ones32, s], writes=[ps_s])
                if center:
                    k.op("pe", lambda e, kk=kk: e.matmul(ps_m[:, :], lhsT=self.ones32[:], rhs=z[:, kk, sl],
                                                         start=(kk == 0), stop=(kk == kc - 1)),
                         reads=[self.ones32, z], writes=[ps_m])
            if center:
                k.op("act", lambda e: e.activation(out=mean[:], in_=ps_m[:, :], func=AF.Copy, scale=1.0 / nfeat),
                     reads=[ps_m], writes=[mean])
                k.op("dve", lambda e: e.tensor_tensor(out=rstd[:], in0=mean[:], in1=mean[:], op=ALU.mult),
                     reads=[mean], writes=[rstd])
                k.op("dve", lambda e: e.scalar_tensor_tensor(out=rstd[:], in0=ps_s[:, :], scalar=1.0 / nfeat,
                                                             in1=rstd[:], op0=ALU.mult, op1=ALU.subtract),
                     reads=[ps_s, rstd], writes=[rstd])
                k.op("dve", lambda e: e.tensor_scalar(out=rstd[:], in0=rstd[:], scalar1=float(eps), scalar2=None,
                                                      op0=ALU.add), reads=[rstd], writes=[rstd])
            else:
                k.op("dve", lambda e: e.tensor_scalar(out=rstd[:], in0=ps_s[:, :], scalar1=1.0 / nfeat,
                                                      scalar2=float(eps), op0=ALU.mult, op1=ALU.add),
                     reads=[ps_s], writes=[rstd])
            k.op("act", lambda e: e.activation(out=rstd[:], in_=rstd[:], func=AF.Sqrt), reads=[rstd], writes=[rstd])
            k.op("dve", lambda e: e.reciprocal(out=rstd[:], in_=rstd[:]), reads=[rstd], writes=[rstd])
            for kk in range(kc):
                t = tmp[kk % 2]
                if center:
                    k.op("dve", lambda e, t=t, kk=kk: e.tensor_tensor(out=t[:], in0=z[:, kk, sl], in1=mean[:],
                                                                      op=ALU.subtract), reads=[z, mean], writes=[t])
                    k.op("pool", lambda e, t=t: e.tensor_tensor(out=t[:], in0=t[:], in1=rstd[:], op=ALU.mult),
                         reads=[t, rstd], writes=[t])
                else:
                    k.op("dve", lambda e, t=t, kk=kk: e.tensor_tensor(out=t[:], in0=z[:, kk, sl], in1=rstd[:],
                                                                      op=ALU.mult), reads=[z, rstd], writes=[t])
                if bcol is not None:
                    k.op("act", lambda e, t=t, kk=kk: e.activation(out=z[:, kk, sl], in_=t[:], func=AF.Identity,
                                                                   scale=gcol[:, kk:kk + 1], bias=bcol[:, kk:kk + 1]),
                         reads=[t, gcol, bcol], writes=[z])
                else:
                    k.op("act", lambda e, t=t, kk=kk: e.activation(out=z[:, kk, sl], in_=t[:], func=AF.Identity,
                                                                   scale=gcol[:, kk:kk + 1]),
                         reads=[t, gcol], writes=[z])

    def phase_init(self):
        k = self.k
        with k.scope() as st:
            posi = k.sb(st, "posi", [64, TC], I32)
            ang = k.sb(st, "ang", [64, TC], F32)
            rc = k.sb(st, "rc", [64, 2], F32)
            k.dma("sp", posi[:], self.inp["pos"][0:1, :].partition_broadcast(64), writes=[posi], sem=posi)
            k.dma("sp", rc[:], self.inp["ropec"][:, :], writes=[rc], sem=rc)
            k.op("dve", lambda e: e.tensor_copy(out=ang[:], in_=posi[:]), reads=[posi], writes=[ang])
            k.op("dve", lambda e: e.tensor_scalar(out=ang[:], in0=ang[:], scalar1=rc[:, 0:1],
                                                  scalar2=1.0 / (2 * math.pi), op0=ALU.mult, op1=ALU.mult),
                 reads=[ang, rc], writes=[ang])
            tabs = k.sb(st, "tabs", [64, 2, TC], F32)
            ni = k.sb(st, "ni", [64, TC], I32)
            nf = k.sb(st, "nf", [64, TC], F32)
            fr = k.sb(st, "fr", [64, TC], F32)
            for j, shift in enumerate((0.25, 0.0)):
                k.op("dve", lambda e, shift=shift: e.tensor_scalar(out=fr[:], in0=ang[:], scalar1=float(shift),
                                                                   scalar2=None, op0=ALU.add),
                     reads=[ang], writes=[fr])
                k.op("dve", lambda e: e.tensor_copy(out=ni[:], in_=fr[:]), reads=[fr], writes=[ni])
                k.op("dve", lambda e: e.tensor_copy(out=nf[:], in_=ni[:]), reads=[ni], writes=[nf])
                k.op("dve", lambda e: e.tensor_tensor(out=fr[:], in0=fr[:], in1=nf[:], op=ALU.subtract),
                     reads=[fr, nf], writes=[fr])
                k.op("dve", lambda e: e.tensor_scalar(out=nf[:], in0=fr[:], scalar1=0.5, scalar2=None, op0=ALU.is_gt),
                     reads=[fr], writes=[nf])
                k.op("dve", lambda e: e.tensor_tensor(out=fr[:], in0=fr[:], in1=nf[:], op=ALU.subtract),
                     reads=[fr, nf], writes=[fr])
                k.op("dve", lambda e: e.tensor_scalar(out=nf[:], in0=fr[:], scalar1=-0.5, scalar2=None, op0=ALU.is_lt),
                     reads=[fr], writes=[nf])
                k.op("dve", lambda e: e.tensor_tensor(out=fr[:], in0=fr[:], in1=nf[:], op=ALU.add),
                     reads=[fr, nf], writes=[fr])
                k.op("act", lambda e, j=j: e.activation(out=tabs[:, j, :], in_=fr[:], func=AF.Sin,
                                                        scale=2 * math.pi), reads=[fr], writes=[tabs])
            k.op("dve", lambda e: e.tensor_scalar(out=tabs[:, 1, :], in0=tabs[:, 1, :], scalar1=rc[:, 1:2],
                                                  scalar2=None, op0=ALU.mult), reads=[tabs, rc], writes=[tabs])
            k.dma("sp", self.agT.src.t[:, :].rearrange("(j p) t -> p j t", p=64), tabs[:], reads=[tabs],
                  writes=[self.agT.src], sem=tabs)
            self.agT.run()
        return self.end_phase("init")

    def phase_rg1(self, l, hsrc):
        k = self.k
        with k.scope() as st:
            hTb = k.sb(st, "hTb", [128, KC, TC], BF16)
            k.dma("pool", hTb[:], _fm(hsrc), writes=[hTb], sem=hTb)
            k.dma("sp", _fm(self.agH.src.t[:, :]), hTb[:], reads=[hTb], writes=[self.agH.src], sem=hTb)
            gst = [k.sb(st, "gst%d" % i, [128, TC], BF16) for i in range(2)]

            def epi(m, mr, t0, nt, ps):
                g = gst[m % 2]
                k.op("act", lambda e: e.activation(out=g[:, t0:t0 + nt], in_=ps[:, :nt], func=AF.Gelu_apprx_tanh),
                     reads=[ps], writes=[g])
                if t0 + nt == TC:
                    k.dma("sp", self.gb_d.t[m * 128:(m + 1) * 128, :], g[:], reads=[g], writes=[self.gb_d], sem=g)

            self.linear_fm(st, hTb, KC, TC, self.inp["w_in_g"][l], D, epi, tag="wg",
                           on_load=lambda bi: self.agH.run_chunk(bi) if bi < self.agH.nchunk else None)
        return self.end_phase("rg1_%d" % l)

    def phase_rg2(self, l):
        k = self.k
        inp = self.inp
        with k.scope() as st:
            wr = k.sb(st, "wr", [128, KC, 256], BF16)
            k.dma("pool", wr[:], _fm(inp["w_in_r"][l]), writes=[wr], sem=wr)
            gw = []
            for nm in ("ga_w", "gx_w"):
                g = k.sb(st, nm, [128, 2, 256], BF16)
                k.dma("pool", g[:], _fm(inp[nm][l]), writes=[g], sem=g)
                gw.append(g)
            cw = k.sb(st, "cw", [128, 2, 4], F32); cb = k.sb(st, "cb", [128, 2], F32)
            gab = k.sb(st, "gab", [128, 2], F32); gxb = k.sb(st, "gxb", [128, 2], F32)
            lam = k.sb(st, "lam", [128, 2], F32); c1 = k.sb(st, "c1", [128, 2], F32)
            for b, nm in ((cw, "conv_w"), (cb, "conv_b"), (gab, "ga_b"), (gxb, "gx_b"), (lam, "lam")):
                k.dma("sp", b[:], inp[nm][l], writes=[b], sem=b)
            k.op("act", lambda e: e.activation(out=c1[:], in_=lam[:], func=AF.Exp, scale=-1.0), reads=[lam], writes=[c1])
            k.op("dve", lambda e: e.tensor_scalar(out=c1[:], in0=c1[:], scalar1=1.0, scalar2=None, op0=ALU.add),
                 reads=[c1], writes=[c1])
            k.op("act", lambda e: e.activation(out=c1[:], in_=c1[:], func=AF.Ln), reads=[c1], writes=[c1])
            k.op("dve", lambda e: e.tensor_scalar(out=c1[:], in0=c1[:], scalar1=-8.0, scalar2=None, op0=ALU.mult),
                 reads=[c1], writes=[c1])
            xts = [k.sb(st, "xt%d" % i, [128, KC, 512], BF16) for i in range(2)]
            ub = [k.sb(st, "ub%d" % i, [128, 515], F32) for i in range(2)]
            xc = [k.sb(st, "xc%d" % i, [128, 512], F32) for i in range(2)]
            xcb = k.sb(st, "xcb", [128, 2, 512], BF16)
            rg = [k.sb(st, "rg%d" % i, [128, 512], F32) for i in range(2)]
            ig = [k.sb(st, "ig%d" % i, [128, 512], F32) for i in range(2)]
            av = [k.sb(st, "av%d" % i, [128, 512], F32) for i in range(2)]
            bv = [k.sb(st, "bv%d" % i, [128, 512], F32) for i in range(2)]
            rec = [k.sb(st, "rec%d" % i, [128, 512], F32) for i in range(2)]
            hst = [k.sb(st, "hst%d" % i, [128, 1], F32) for i in range(2)]
            recb = [k.sb(st, "recb%d" % i, [128, 2, 512], BF16) for i in range(2)]
            for i in range(2):
                k.op("dve", lambda e, i=i: e.memset(ub[i][:], 0.0), writes=[ub[i]])
                k.op("dve", lambda e, i=i: e.memset(hst[i][:], 0.0), writes=[hst[i]])
            def load(tt):
                self.load_fm_tile(xts[tt % 2], self.agH, tt // 2, tt % 2, D)

            load(0)
            for tt in range(16):
                if tt + 1 < 16:
                    load(tt + 1)
                x = xts[tt % 2]
                rb = recb[tt % 2]
                for mi in range(2):
                    u = ub[mi]
                    if tt > 0:
                        k.op("dve", lambda e, u=u: e.tensor_copy(out=u[:, 0:3], in_=u[:, 512:515]), reads=[u], writes=[u])
                    ps = self.nps()
                    for kk in range(KC):
                        k.op("pe", lambda e, kk=kk, ps=ps, mi=mi, x=x: e.matmul(
                            ps[:, :], lhsT=wr[:, kk, mi * 128:(mi + 1) * 128], rhs=x[:, kk, :],
                            start=(kk == 0), stop=(kk == KC - 1)), reads=[wr, x], writes=[ps])
                    k.op("act", lambda e, u=u, ps=ps: e.activation(out=u[:, 3:515], in_=ps[:, :], func=AF.Copy),
                         reads=[ps], writes=[u])
                    c = xc[mi]
                    k.op("dve", lambda e, c=c, u=u, mi=mi: e.tensor_scalar(
                        out=c[:], in0=u[:, 0:512], scalar1=cw[:, mi, 0:1], scalar2=cb[:, mi:mi + 1],
                        op0=ALU.mult, op1=ALU.add), reads=[u, cw, cb], writes=[c])
                    for j in range(1, 4):
                        k.op("dve", lambda e, c=c, u=u, mi=mi, j=j: e.scalar_tensor_tensor(
                            out=c[:], in0=u[:, j:j + 512], scalar=cw[:, mi, j:j + 1], in1=c[:],
                            op0=ALU.mult, op1=ALU.add), reads=[u, cw, c], writes=[c])
                    k.op("act", lambda e, c=c, mi=mi: e.activation(out=xcb[:, mi, :], in_=c[:], func=AF.Copy),
                         reads=[c], writes=[xcb])
                for gi, (dst, bias) in enumerate(((rg, gab), (ig, gxb))):
                    for mo in range(2):
                        ps = self.nps()
                        for ki in range(2):
                            k.op("pe", lambda e, ps=ps, ki=ki, mo=mo, gi=gi: e.matmul(
                                ps[:, :], lhsT=gw[gi][:, ki, mo * 128:(mo + 1) * 128], rhs=xcb[:, ki, :],
                                start=(ki == 0), stop=(ki == 1)), reads=[gw[gi], xcb], writes=[ps])
                        k.op("act", lambda e, ps=ps, mo=mo, dst=dst, bias=bias: e.activation(
                            out=dst[mo][:], in_=ps[:, :], func=AF.Sigmoid, bias=bias[:, mo:mo + 1]),
                             reads=[ps, bias], writes=[dst[mo]])
                for mo in range(2):
                    a, b, c = av[mo], bv[mo], xc[mo]
                    k.op("act", lambda e, a=a, mo=mo: e.activation(out=a[:], in_=rg[mo][:], func=AF.Exp,
                                                                   scale=c1[:, mo:mo + 1]), reads=[rg[mo], c1], writes=[a])
                    k.op("dve", lambda e, a=a, b=b: e.tensor_tensor(out=b[:], in0=a[:], in1=a[:], op=ALU.mult),
                         reads=[a], writes=[b])
                    k.op("dve", lambda e, b=b: e.tensor_scalar(out=b[:], in0=b[:], scalar1=-1.0, scalar2=1.0,
                                                               op0=ALU.mult, op1=ALU.add), reads=[b], writes=[b])
                    k.op("dve", lambda e, b=b: e.tensor_scalar(out=b[:], in0=b[:], scalar1=0.0, scalar2=None,
                                                               op0=ALU.max), reads=[b], writes=[b])
                    k.op("act", lambda e, b=b: e.activation(out=b[:], in_=b[:], func=AF.Sqrt), reads=[b], writes=[b])
                    k.op("dve", lambda e, c=c, mo=mo: e.tensor_tensor(out=c[:], in0=c[:], in1=ig[mo][:], op=ALU.mult),
                         reads=[c, ig[mo]], writes=[c])
                    k.op("dve", lambda e, b=b, c=c: e.tensor_tensor(out=b[:], in0=b[:], in1=c[:], op=ALU.mult),
                         reads=[b, c], writes=[b])
                    k.op("dve", lambda e, a=a, b=b, mo=mo: e.tensor_tensor_scan(
                        out=rec[mo][:], data0=a[:], data1=b[:], initial=hst[mo][:, 0:1], op0=ALU.mult, op1=ALU.add),
                         reads=[a, b, hst[mo]], writes=[rec[mo]])
                    k.op("dve", lambda e, mo=mo: e.tensor_copy(out=hst[mo][:], in_=rec[mo][:, 511:512]),
                         reads=[rec[mo]], writes=[hst[mo]])
                    k.op("act", lambda e, mo=mo, rb=rb: e.activation(out=rb[:, mo, :], in_=rec[mo][:], func=AF.Copy),
                         reads=[rec[mo]], writes=[rb])
                jb = tt // 2
                k.dma("sp", self.agF.src.t[jb * 256:(jb + 1) * 256, (tt % 2) * 512:(tt % 2 + 1) * 512].rearrange(
                    "(m p) t -> p m t", p=128), rb[:], reads=[rb], writes=[self.agF.src], sem=rb)
                if tt % 4 == 3:
                    self.agF.run_chunk(tt // 4)
        return self.end_phase("rg2_%d" % l)

    def mix_and_tail(self, l, st, mT, wsrc, hsrc, name):
        k = self.k
        inp = self.inp
        z = k.sb(st, "z", [128, KC, TC], F32)
        k.dma("sp", z[:], _fm(hsrc), writes=[z], sem=z)
        gcol = k.sb(st, "lng", [128, KC], F32); bcol = k.sb(st, "lnb", [128, KC], F32)
        k.dma("sp", gcol[:], inp["ln_mix_g"][l], writes=[gcol], sem=gcol)
        k.dma("sp", bcol[:], inp["ln_mix_b"][l], writes=[bcol], sem=bcol)

        def epi(m, mr, t0, nt, ps):
            k.op("dve", lambda e: e.scalar_tensor_tensor(out=z[:, m, t0:t0 + nt], in0=z[:, m, t0:t0 + nt],
                                                         scalar=float(ALPHA), in1=ps[:, :nt], op0=ALU.mult, op1=ALU.add),
                 reads=[z, ps], writes=[z])

        with k.scope() as st2:
            self.linear_fm(st2, mT, KC, TC, wsrc, D, epi, tag="wo")
        with k.scope() as stz:
            zt = k.sb(stz, "zt", [128, D], F32)
            k.op("pool", lambda e: e.memset(zt[:], 0.0), writes=[zt])
            for i in range((SEQ + CAP) // 128):
                k.dma("sp", self.ysp[0].t[i * 128:(i + 1) * 128, :], zt[:], reads=[zt], writes=[self.ysp[0]], sem=zt)
        if self.debug:
            k.dma("sp", _fm(self.dbg_d.t[:, :]), z[:], reads=[z], writes=[self.dbg_d], sem=z)
            k.dma("sp", _fm(self.dbg2_d.t[:, :]), mT[:], reads=[mT], writes=[self.dbg2_d], sem=mT)
        with k.scope() as st2:
            self.norm_fm(st2, z, KC, TC, gcol, bcol, LN_EPS, True, "ln1")
        k.dma("sp", _fm(self.h1T_d.t[:, :]), z[:], reads=[z], writes=[self.h1T_d], sem=z)
        with k.scope() as st2:
            rw = k.sb(st2, "rw", [128, KC, NEXP], F32)
            rbias = k.sb(st2, "rbias", [128, NEXP], F32)
            k.dma("sp", rw[:], _fm(inp["router_w"][l]), writes=[rw], sem=rw)
            k.dma("sp", rbias[:], inp["router_b"][l], writes=[rbias], sem=rbias)
            lg = k.sb(st2, "lg", [128, NEXP], F32)
            Gall = k.sb(st2, "Gall", [128, 8, NEXP], F32)
            Mall = k.sb(st2, "Mall", [128, 8, NEXP], F32)
            Pall = k.sb(st2, "Pall", [128, 8, NEXP], F32)
            top = k.sb(st2, "top", [128, 8], F32); sc = k.sb(st2, "sc", [128, 2], F32)
            ltri = k.sb(st2, "ltri", [128, 128], F32)
            iota = k.sb(st2, "iota", [128, CAP], F32)
            vals3 = k.sb(st2, "vals4", [128, 8, NEXP, 4], F32)
            dumprow = k.sb(st2, "dumprow", [128, 2], F32)
            for b, nm in ((ltri, "ltri"), (iota, "iota"), (vals3, "vals4"), (dumprow, "dumprow")):
                k.dma("sp", b[:], inp[nm].ap(), writes=[b], sem=b)
            htm = [k.sb(st2, "htm%d" % i, [128, D], BF16) for i in range(2)]
            for tk in range(TC // 128):
                tsl = slice(tk * 128, (tk + 1) * 128)
                hm = htm[tk % 2]
                for q4 in range(4):
                    pst = self.nps()
                    for j in range(4):
                        kk = q4 * 4 + j
                        k.op("pe", lambda e, pst=pst, kk=kk, j=j: e.transpose(
                            out=pst[:, j * 128:(j + 1) * 128], in_=z[:, kk, tsl], identity=self.ident[:]),
                             reads=[z, self.ident], writes=[pst])
                    k.op("act" if q4 % 2 else "dve", (lambda e, pst=pst, q4=q4: e.activation(
                        out=hm[:, q4 * 512:(q4 + 1) * 512], in_=pst[:, :], func=AF.Copy)) if q4 % 2 else
                         (lambda e, pst=pst, q4=q4: e.tensor_copy(out=hm[:, q4 * 512:(q4 + 1) * 512], in_=pst[:, :])),
                         reads=[pst], writes=[hm])
                k.dma("sp", self.agHt.src.t[tsl, :], hm[:], reads=[hm], writes=[self.agHt.src], sem=hm)
                if tk % 2 == 1:
                    self.agHt.run_chunk(tk // 2)
                ps = self.nps()
                for kk in range(KC):
                    k.op("pe", lambda e, kk=kk, ps=ps: e.matmul(
                        ps[:, :NEXP], lhsT=z[:, kk, tsl], rhs=rw[:, kk, :],
                        start=(kk == 0), stop=(kk == KC - 1)), reads=[z, rw], writes=[ps])
                ex = Gall[:, tk, :]
                mk = Mall[:, tk, :]
                k.op("dve", lambda e, ps=ps: e.tensor_tensor(out=lg[:], in0=ps[:, :NEXP], in1=rbias[:], op=ALU.add),
                     reads=[ps, rbias], writes=[lg])
                k.op("dve", lambda e: e.max(out=top[:], in_=lg[:]), reads=[lg], writes=[top])
                k.op("dve", lambda e: e.tensor_scalar(out=sc[:, 0:1], in0=top[:, 0:1], scalar1=-1.0, scalar2=None,
                                                      op0=ALU.mult), reads=[top], writes=[sc])
                k.op("act", lambda e: e.activation(out=ex, in_=lg[:], func=AF.Exp, bias=sc[:, 0:1]),
                     reads=[lg, sc], writes=[Gall])
                k.op("dve", lambda e: e.tensor_scalar(out=mk, in0=lg[:], scalar1=top[:, 3:4], scalar2=None,
                                                      op0=ALU.is_ge), reads=[lg, top], writes=[Mall])
                k.op("dve", lambda e: e.tensor_tensor(out=ex, in0=ex, in1=mk, op=ALU.mult),
                     reads=[Gall, Mall], writes=[Gall])
                k.op("dve", lambda e: e.reduce_sum(out=sc[:, 1:2], in_=ex, axis=AX.X), reads=[Gall], writes=[sc])
                k.op("dve", lambda e: e.reciprocal(out=sc[:, 1:2], in_=sc[:, 1:2]), reads=[sc], writes=[sc])
                k.op("dve", lambda e: e.tensor_scalar(out=ex, in0=ex, scalar1=sc[:, 1:2], scalar2=None,
                                                      op0=ALU.mult), reads=[Gall, sc], writes=[Gall])
            assert self.agHt.ch == 256
            for tk in range(8):
                ps = self.nps()
                for t2 in range(tk + 1):
                    k.op("pe", lambda e, ps=ps, t2=t2: e.matmul(
                        ps[:, :NEXP], lhsT=(ltri[:] if t2 == tk else self.ones32[:]), rhs=Mall[:, t2, :],
                        start=(t2 == 0), stop=(t2 == tk)), reads=[ltri, self.ones32, Mall], writes=[ps])
                k.op("act", lambda e, ps=ps: e.activation(out=Pall[:, tk, :], in_=ps[:, :NEXP], func=AF.Copy),
                     reads=[ps], writes=[Pall])
            k.op("dve", lambda e: e.tensor_copy(out=vals3[:, :, :, 3], in_=Gall[:, :, :]), reads=[Gall], writes=[vals3])
            R = [self.nps(), self.nps()]
            oh = [k.sb(st2, "oh%d" % i, [128, CAP], F32) for i in range(4)]
            n = 0
            for ee in range(NEXP):
                for tk in range(8):
                    o = oh[n % 4]
                    n += 1
                    k.op("dve", lambda e, o=o: e.tensor_scalar(
                        out=o[:], in0=iota[:], scalar1=Pall[:, tk, ee:ee + 1], scalar2=Mall[:, tk, ee:ee + 1],
                        op0=ALU.is_equal, op1=ALU.mult), reads=[iota, Pall, Mall], writes=[o])
                    for sb in range(2):
                        k.op("pe", lambda e, o=o, sb=sb: e.matmul(
                            R[sb][:, ee * 4:(ee + 1) * 4], lhsT=o[:, sb * 128:(sb + 1) * 128], rhs=vals3[:, tk, ee, :],
                            start=(tk == 0), stop=(tk == 7)), reads=[o, vals3], writes=[R[sb]])
            L = k.sb(st2, "L", [128, 2, NEXP, 4], F32)
            tmpf = k.sb(st2, "tmpf", [128, NEXP], F32)
            for sb in range(2):
                Rv = R[sb][:, 0:NEXP * 4].rearrange("p (e c) -> p e c", c=4)
                k.op("act", lambda e, sb=sb, Rv=Rv: e.activation(out=L[:, sb, :, 0], in_=Rv[:, :, 0], func=AF.Copy),
                     reads=[R[sb]], writes=[L])
                k.op("act", lambda e, sb=sb, Rv=Rv: e.activation(out=L[:, sb, :, 2], in_=Rv[:, :, 2], func=AF.Copy),
                     reads=[R[sb]], writes=[L])
                k.op("act", lambda e, sb=sb, Rv=Rv: e.activation(out=L[:, sb, :, 3], in_=Rv[:, :, 3], func=AF.Copy),
                     reads=[R[sb]], writes=[L])
                k.op("dve", lambda e, sb=sb, Rv=Rv: e.tensor_scalar(
                    out=tmpf[:], in0=Rv[:, :, 2], scalar1=-1.0, scalar2=1.0, op0=ALU.mult, op1=ALU.add),
                     reads=[R[sb]], writes=[tmpf])
                k.op("dve", lambda e, sb=sb: e.tensor_scalar(
                    out=tmpf[:], in0=tmpf[:], scalar1=dumprow[:, sb:sb + 1], scalar2=None, op0=ALU.mult),
                     reads=[tmpf, dumprow], writes=[tmpf])
                k.op("dve", lambda e, sb=sb, Rv=Rv: e.tensor_tensor(out=L[:, sb, :, 1], in0=Rv[:, :, 1], in1=tmpf[:],
                                                                    op=ALU.add), reads=[R[sb], tmpf], writes=[L])
            k.dma("sp", self.agL.src.t[:, :].rearrange("(sb p e) f -> p sb e f", sb=2, p=128), L[:], reads=[L],
                  writes=[self.agL.src], sem=L)
            self.agL.run()

    def phase_rg3(self, l, hsrc):
        k = self.k
        with k.scope() as st:
            mT = k.sb(st, "mT", [128, KC, TC], BF16)
            k.dma("sp", mT[:], _fm(self.gb_d.t[:, :]), writes=[mT], sem=mT)
            with k.scope() as st2:
                recT = k.sb(st2, "recT", [128, KC, TC], BF16)
                rows = self.agF.out.t[:, :]
                for kk in range(KC):
                    k.gather(recT[:, kk, :], rows, self.tF[:, kk:kk + 1], reads=[self.tF, self.agF.out],
                             writes=[recT], sem=recT)
                for kk in range(KC):
                    k.op("dve" if kk % 2 else "pool", lambda e, kk=kk: e.tensor_tensor(
                        out=mT[:, kk, :], in0=mT[:, kk, :], in1=recT[:, kk, :], op=ALU.mult),
                         reads=[mT, recT], writes=[mT])
            self.mix_and_tail(l, st, mT, self.inp["w_out"][l], hsrc, "rg3")
        return self.end_phase("rg3_%d" % l)

    def phase_moe_dense(self, l):
        k = self.k
        inp = self.inp
        grows = self.agG.out.t[:, :].rearrange("r (h t) -> (r h) t", t=512)
        ydst = self.ym[0]
        with k.scope() as st:
            W1 = k.sb(st, "W1", [128, KC, 2 * DEXP], BF16)
            W2 = k.sb(st, "W2", [128, DEXP // 128, D], BF16)
            b1c = k.sb(st, "b1c", [128, 16], F32); b2c = k.sb(st, "b2c", [128, 16], F32)
            gidx = k.sb(st, "gidx", [128, EPC * 16], I32)
            k.dma("sp", gidx[:], inp["idx_g"][:, :], writes=[gidx], sem=gidx)
            xts = [k.sb(st, "mx%d" % i, [128, KC, 512], BF16) for i in range(2)]
            grow = [k.sb(st, "grow%d" % i, [128, 512], F32) for i in range(2)]
            lin1 = k.sb(st, "lin1", [128, 8, 512], BF16)
            gt = [k.sb(st, "gt%d" % i, [128, 512], F32) for i in range(2)]
            sg = [k.sb(st, "sg%d" % i, [128, 512], F32) for i in range(2)]
            actT = k.sb(st, "actT", [128, 8, 512], BF16)
            yst = [k.sb(st, "yst%d" % i, [128, 4, 512], F32) for i in range(2)]
            yreg = [[Buf(None, "yreg") for _ in range(4)] for _ in range(16)]
            ysi = 0
            for el in range(EPC):
                k.dma("pool", W1[:], _fm(inp["w1"][l, el]), writes=[W1], sem=W1)
                k.dma("pool", W2[:], _fm(inp["w2"][l, el]), writes=[W2], sem=W2)
                k.dma("sp", b1c[:], inp["b1"][l, el], writes=[b1c], sem=b1c)
                k.dma("sp", b2c[:], inp["b2"][l, el], writes=[b2c], sem=b2c)

                def load(tt):
                    self.load_fm_tile(xts[tt % 2], self.agH, tt // 2, tt % 2, D)
                    g = grow[tt % 2]
                    k.gather(g[:], grows, gidx[:, el * 16 + tt:el * 16 + tt + 1], reads=[gidx, self.agG.out],
                             writes=[g], sem=g)

                load(0)
                for tt in range(16):
                    if tt + 1 < 16:
                        load(tt + 1)
                    x = xts[tt % 2]
                    g = grow[tt % 2]
                    for m in list(range(8, 16)) + list(range(8)):
                        ps = self.nps()
                        for kk in range(KC):
                            k.op("pe", lambda e, kk=kk, ps=ps, m=m, x=x: e.matmul(
                                ps[:, :], lhsT=W1[:, kk, m * 128:(m + 1) * 128], rhs=x[:, kk, :],
                                start=(kk == 0), stop=(kk == KC - 1)), reads=[W1, x], writes=[ps])
                        if m >= 8:
                            t = gt[m % 2]
                            k.op("dve", lambda e, ps=ps, m=m, t=t: e.tensor_scalar(
                                out=t[:], in0=ps[:, :], scalar1=b1c[:, m:m + 1], scalar2=7.0, op0=ALU.add, op1=ALU.min),
                                 reads=[ps, b1c], writes=[t])
                            k.op("dve", lambda e, m=m, t=t: e.tensor_scalar(
                                out=lin1[:, m - 8, :], in0=t[:], scalar1=-7.0, scalar2=1.0, op0=ALU.max, op1=ALU.add),
                                 reads=[t], writes=[lin1])
                        else:
                            t = gt[m % 2]; s = sg[m % 2]
                            k.op("dve", lambda e, ps=ps, m=m, t=t: e.tensor_scalar(
                                out=t[:], in0=ps[:, :], scalar1=b1c[:, m:m + 1], scalar2=7.0, op0=ALU.add, op1=ALU.min),
                                 reads=[ps, b1c], writes=[t])
                            k.op("act", lambda e, t=t, s=s: e.activation(out=s[:], in_=t[:], func=AF.Sigmoid, scale=1.702),
                                 reads=[t], writes=[s])
                            k.op("pool", lambda e, t=t, s=s: e.tensor_tensor(out=s[:], in0=s[:], in1=t[:], op=ALU.mult),
                                 reads=[s, t], writes=[s])
                            k.op("dve", lambda e, m=m, s=s: e.tensor_tensor(out=actT[:, m, :], in0=s[:], in1=lin1[:, m, :],
                                                                            op=ALU.mult), reads=[s, lin1], writes=[actT])
                    for f in range(16):
                        ps = self.nps()
                        for kk in range(8):
                            k.op("pe", lambda e, kk=kk, ps=ps, f=f: e.matmul(
                                ps[:, :], lhsT=W2[:, kk, f * 128:(f + 1) * 128], rhs=actT[:, kk, :],
                                start=(kk == 0), stop=(kk == 7)), reads=[W2, actT], writes=[ps])
                        ys = yst[ysi % 2]
                        k.op("dve", lambda e, ps=ps, f=f, ys=ys, g=g: e.scalar_tensor_tensor(
                            out=ys[:, f % 4, :], in0=ps[:, :], scalar=b2c[:, f:f + 1], in1=g[:],
                            op0=ALU.add, op1=ALU.mult), reads=[ps, b2c, g], writes=[ys])
                        if f % 4 == 3:
                            f0 = f - 3
                            reg = yreg[tt][f // 4]
                            dst = ydst.t[f0 * 128:(f0 + 4) * 128, tt * 512:(tt + 1) * 512].rearrange(
                                "(j p) t -> p j t", p=128)
                            if el == 0:
                                k.dma("sp", dst, ys[:], reads=[ys], writes=[reg], sem=ys)
                            else:
                                k.dma("pool", dst, ys[:], reads=[ys], writes=[reg], sem=ys, accum_op=ALU.add)
                            ysi += 1
            for row in yreg:
                for reg in row:
                    k._merge(ydst.w, reg.w)
            for i in range(D // 128):
                sl = slice(i * 128, (i + 1) * 128)
                k.collective("AllReduce", G4, self.ym[0], self.ym[1], self.ym[0].t[sl, :], self.ym[1].t[sl, :])
            for i in range(D // 128):
                sl = slice(i * 128, (i + 1) * 128)
                k.collective("AllReduce", G2, self.ym[1], self.ym[2], self.ym[1].t[sl, :], self.ym[2].t[sl, :])
        return self.end_phase("moe_%d" % l)

    def phase_moe(self, l):
        k = self.k
        inp = self.inp
        hrows = self.agHt.out.t[:, :]
        lrows = self.agL.out.t[:, :]
        ysp = self.ysp[0]
        with k.scope() as st:
            zdeps = {}
            W1 = k.sb(st, "W1", [128, KC, 2 * DEXP], BF16)
            W2 = k.sb(st, "W2", [128, DEXP // 128, D], BF16)
            b1c = k.sb(st, "b1c", [128, 16], F32)
            b2t = k.sb(st, "b2t", [128, D], F32)
            lidx = k.sb(st, "lidx", [128, EPC * NCORE * 2], I32)
            k.dma("sp", lidx[:], inp["idx_L"][:, :], writes=[lidx], sem=lidx)
            xgT = [k.sb(st, "xgT%d" % i, [128, KC, 512], BF16) for i in range(2)]
            xg = [k.sb(st, "xg%d" % i, [128, D], BF16) for i in range(4)]
            Lt = [k.sb(st, "Lt%d" % i, [128, 4, 4], F32) for i in range(2)]
            Li = [k.sb(st, "Li%d" % i, [128, 4, 2], I32) for i in range(2)]
            lin1 = k.sb(st, "lin1", [128, 8, 512], BF16)
            gt = [k.sb(st, "gt%d" % i, [128, 512], F32) for i in range(2)]
            sg = [k.sb(st, "sg%d" % i, [128, 512], F32) for i in range(2)]
            actT = k.sb(st, "actT", [128, 8, 512], BF16)
            ytm = [k.sb(st, "ytm%d" % i, [128, D], F32) for i in range(2)]
            prev = dict(zdeps)
            yi = 0
            npair = NCORE // 2
            seq = [(el, pr) for el in range(EPC) for pr in range(npair)]

            def prep_gather(idx):
                el, pr = seq[idx]
                lt, li = Lt[idx % 2], Li[idx % 2]
                for b4 in range(4):
                    r, sb = 2 * pr + b4 // 2, b4 % 2
                    col = (el * NCORE + r) * 2 + sb
                    k.gather(lt[:, b4, :], lrows, lidx[:, col:col + 1], reads=[lidx, self.agL.out], writes=[lt], sem=lt)
                k.op("dve", lambda e: e.tensor_copy(out=li[:], in_=lt[:, :, 0:2]), reads=[lt], writes=[li])
                for b4 in range(4):
                    g = xg[b4]
                    k.gather(g[:], hrows, li[:, b4, 0:1], reads=[li, self.agHt.out], writes=[g], sem=g)

            def prep_transpose(idx):
                x = xgT[idx % 2]
                for b4 in range(4):
                    g = xg[b4]
                    for q4 in range(4):
                        pst = self.nps()
                        pv = pst[:, :].bitcast(BF16)
                        for j in range(4):
                            kk = q4 * 4 + j
                            k.op("pe", lambda e, pv=pv, kk=kk, j=j, g=g: e.transpose(
                                out=pv[:, j * 128:(j + 1) * 128], in_=g[:, kk * 128:(kk + 1) * 128], identity=self.identb[:]),
                                 reads=[g, self.identb], writes=[pst])
                        src = pv[:, 0:512].rearrange("p (j t) -> p j t", j=4)
                        dst = x[:, q4 * 4:(q4 + 1) * 4, b4 * 128:(b4 + 1) * 128]
                        if q4 % 2:
                            k.op("act", lambda e, src=src, dst=dst: e.activation(out=dst, in_=src, func=AF.Copy),
                                 reads=[pst], writes=[x])
                        else:
                            k.op("dve", lambda e, src=src, dst=dst: e.tensor_copy(out=dst, in_=src), reads=[pst], writes=[x])

            prep_gather(0)
            prep_transpose(0)
            cur = {}
            for idx, (el, pr) in enumerate(seq):
                if idx == 0:
                    k.dma("pool", W1[:], _fm(inp["w1"][l, el]), writes=[W1], sem=W1)
                    k.dma("pool", W2[:], _fm(inp["w2"][l, el]), writes=[W2], sem=W2)
                    k.dma("sp", b1c[:], inp["b1"][l, el], writes=[b1c], sem=b1c)
                    k.dma("sp", b2t[:], inp["b2row"][l, el:el + 1, :].partition_broadcast(128), writes=[b2t], sem=b2t)
                if pr == 0 and el > 0:
                    prev = cur
                    cur = {}
                last_pair = (pr == npair - 1 and el + 1 < EPC)
                if idx + 1 < len(seq):
                    prep_gather(idx + 1)
                x = xgT[idx % 2]
                lt, li = Lt[idx % 2], Li[idx % 2]
                for m in list(range(8, 16)) + list(range(8)):
                    ps = self.nps()
                    for kk in range(KC):
                        k.op("pe", lambda e, kk=kk, ps=ps, m=m: e.matmul(
                            ps[:, :], lhsT=W1[:, kk, m * 128:(m + 1) * 128], rhs=x[:, kk, :],
                            start=(kk == 0), stop=(kk == KC - 1)), reads=[W1, x], writes=[ps])
                    t = gt[m % 2]
                    k.op("dve", lambda e, ps=ps, m=m, t=t: e.tensor_scalar(
                        out=t[:], in0=ps[:, :], scalar1=b1c[:, m:m + 1], scalar2=7.0, op0=ALU.add, op1=ALU.min),
                         reads=[ps, b1c], writes=[t])
                    if m >= 8:
                        k.op("dve", lambda e, m=m, t=t: e.tensor_scalar(
                            out=lin1[:, m - 8, :], in0=t[:], scalar1=-7.0, scalar2=1.0, op0=ALU.max, op1=ALU.add),
                             reads=[t], writes=[lin1])
                    else:
                        sgm = sg[m % 2]
                        k.op("act", lambda e, t=t, sgm=sgm: e.activation(out=sgm[:], in_=t[:], func=AF.Sigmoid, scale=1.702),
                             reads=[t], writes=[sgm])
                        k.op("pool", lambda e, t=t, sgm=sgm: e.tensor_tensor(out=sgm[:], in0=sgm[:], in1=t[:], op=ALU.mult),
                             reads=[sgm, t], writes=[sgm])
                        k.op("dve", lambda e, m=m, sgm=sgm: e.tensor_tensor(out=actT[:, m, :], in0=sgm[:], in1=lin1[:, m, :],
                                                                            op=ALU.mult), reads=[sgm, lin1], writes=[actT])
                if last_pair:
                    k.dma("pool", W1[:], _fm(inp["w1"][l, el + 1]), writes=[W1], sem=W1)
                    k.dma("sp", b1c[:], inp["b1"][l, el + 1], writes=[b1c], sem=b1c)
                if idx + 1 < len(seq):
                    prep_transpose(idx + 1)
                for b4 in range(4):
                    ys = ytm[yi % 2]
                    yi += 1
                    for ft in range(4):
                        fs = slice(ft * 512, (ft + 1) * 512)
                        ps = self.nps()
                        for kk in range(8):
                            k.op("pe", lambda e, kk=kk, ps=ps, b4=b4, fs=fs: e.matmul(
                                ps[:, :], lhsT=actT[:, kk, b4 * 128:(b4 + 1) * 128], rhs=W2[:, kk, fs],
                                start=(kk == 0), stop=(kk == 7)), reads=[W2, actT], writes=[ps])
                        k.op("dve", lambda e, ps=ps, ys=ys, fs=fs: e.tensor_tensor(out=ys[:, fs], in0=ps[:, :], in1=b2t[:, fs],
                                                                                  op=ALU.add), reads=[ps, b2t], writes=[ys])
                        k.op("act", lambda e, ys=ys, fs=fs, b4=b4, lt=lt: e.activation(
                            out=ys[:, fs], in_=ys[:, fs], func=AF.Identity, scale=lt[:, b4, 3:4]), reads=[ys, lt], writes=[ys])
                    ev = k.scatter_add(ysp.t[:, :], li[:, b4, 1:2], ys[:], reads=[ys, li], sem=ys, extra_deps=prev)
                    k._merge(cur, dict([ev]))
                if last_pair:
                    k.dma("pool", W2[:], _fm(inp["w2"][l, el + 1]), writes=[W2], sem=W2)
                    k.dma("sp", b2t[:], inp["b2row"][l, el + 1:el + 2, :].partition_broadcast(128), writes=[b2t], sem=b2t)
            k._merge(ysp.w, cur)
            k._merge(ysp.w, prev)
            for i in range(SEQ // 512):
                k.collective("ReduceScatter", G2, self.ysp[0], self.ysp[1], self.ysp[0].t[i * 512:(i + 1) * 512, :],
                             self.ysp[1].t[i * 256:(i + 1) * 256, :])
            for j in range(SEQ // 1024):
                k.collective("ReduceScatter", G4, self.ysp[1], self.ysp[2], self.ysp[1].t[j * 512:(j + 1) * 512, :],
                             self.ysp[2].t[j * 128:(j + 1) * 128, :])
        return self.end_phase("moe_%d" % l)

    def phase_post(self, l, hdst):
        k = self.k
        inp = self.inp
        with k.scope() as st:
            z = k.sb(st, "pz", [128, KC, TC], F32)
            k.dma("sp", z[:], _fm(self.h1T_d.t[:, :]), writes=[z], sem=z)
            gcol = k.sb(st, "lng2", [128, KC], F32); bcol = k.sb(st, "lnb2", [128, KC], F32)
            k.dma("sp", gcol[:], inp["ln_ffn_g"][l], writes=[gcol], sem=gcol)
            k.dma("sp", bcol[:], inp["ln_ffn_b"][l], writes=[bcol], sem=bcol)
            yidx = k.sb(st, "yidx", [128, 8], I32)
            k.dma("sp", yidx[:], inp["idx_y"][:, :], writes=[yidx], sem=yidx)
            with k.scope() as st2:
                ytk = [k.sb(st2, "ytk%d" % i, [128, D], F32) for i in range(2)]
                for tk in range(8):
                    y = ytk[tk % 2]
                    k.dma("sp", y[:], self.ysp[2].t[tk * 128:(tk + 1) * 128, :], reads=[self.ysp[2]], writes=[y], sem=y)
                    for q4 in range(4):
                        pst = self.nps()
                        for j in range(4):
                            kk = q4 * 4 + j
                            k.op("pe", lambda e, pst=pst, kk=kk, j=j, y=y: e.transpose(
                                out=pst[:, j * 128:(j + 1) * 128], in_=y[:, kk * 128:(kk + 1) * 128], identity=self.ident[:]),
                                 reads=[y, self.ident], writes=[pst])
                        zv = z[:, q4 * 4:(q4 + 1) * 4, tk * 128:(tk + 1) * 128]
                        k.op("dve", lambda e, pst=pst, zv=zv: e.scalar_tensor_tensor(
                            out=zv, in0=zv, scalar=float(ALPHA), in1=pst[:, :].rearrange("p (j t) -> p j t", j=4),
                            op0=ALU.mult, op1=ALU.add), reads=[z, pst], writes=[z])
            with k.scope() as st2:
                self.norm_fm(st2, z, KC, TC, gcol, bcol, LN_EPS, True, "ln2")
            zb = k.sb(st, "pzb", [128, KC, TC], BF16)
            for kk in range(KC):
                if kk % 2:
                    k.op("pool", lambda e, kk=kk: e.tensor_copy(out=zb[:, kk, :], in_=z[:, kk, :]), reads=[z], writes=[zb])
                else:
                    k.op("act", lambda e, kk=kk: e.activation(out=zb[:, kk, :], in_=z[:, kk, :], func=AF.Copy),
                         reads=[z], writes=[zb])
            pTb = k.sb(st, "pTb", [128, 2, TC], BF16)
            wp = k.sb(st, "wp", [128, 2, D], BF16)
            k.dma("pool", pTb[:], _fm(inp["pT"][l]), writes=[pTb], sem=pTb)
            k.dma("pool", wp[:], _fm(inp["ple_proj"][l]), writes=[wp], sem=wp)
            sgt = [k.sb(st, "psg%d" % i, [128, 512], F32) for i in range(2)]

            def epi(m, mr, t0, nt, ps):
                s = sgt[m % 2]
                k.op("act", lambda e: e.activation(out=s[:, :nt], in_=ps[:, :nt], func=AF.Sigmoid), reads=[ps], writes=[s])
                ps2 = self.nps()
                for kk in range(2):
                    k.op("pe", lambda e, kk=kk: e.matmul(ps2[:, :nt], lhsT=wp[:, kk, m * 128:(m + 1) * 128],
                                                         rhs=pTb[:, kk, t0:t0 + nt], start=(kk == 0), stop=(kk == 1)),
                         reads=[wp, pTb], writes=[ps2])
                k.op("dve", lambda e: e.tensor_tensor(out=s[:, :nt], in0=ps2[:, :nt], in1=s[:, :nt], op=ALU.mult),
                     reads=[ps2, s], writes=[s])
                k.op("pool", lambda e: e.tensor_tensor(out=z[:, m, t0:t0 + nt], in0=z[:, m, t0:t0 + nt], in1=s[:, :nt],
                                                       op=ALU.add), reads=[z, s], writes=[z])

            self.linear_fm(st, zb, KC, TC, inp["ple_gate"][l], D, epi, tag="wpg")
            k.dma("sp", _fm(hdst), z[:], reads=[z], writes=[self.hT_d], sem=z)
        return self.end_phase("post_%d" % l)

    def phase_q(self, j, hsrc, with_kv):
        k = self.k
        inp = self.inp
        with k.scope() as st:
            hTb = k.sb(st, "qhTb", [128, KC, TC], BF16)
            k.dma("pool", hTb[:], _fm(hsrc), writes=[hTb], sem=hTb)
            cq = k.sb(st, "cq", [128, 6, TC], F32)
            qn = k.sb(st, "qn", [128, 6], F32)
            k.dma("sp", qn[:], inp["q_norm"][j], writes=[qn], sem=qn)

            def epi(m, mr, t0, nt, ps):
                k.op("act", lambda e: e.activation(out=cq[:, m, t0:t0 + nt], in_=ps[:, :nt], func=AF.Copy),
                     reads=[ps], writes=[cq])

            with k.scope() as st2:
                self.linear_fm(st2, hTb, KC, TC, inp["w_dq"][j], QLORA, epi, mblk=QLORA, tag="wdq")
            with k.scope() as st2:
                self.norm_fm(st2, cq, 6, TC, qn, None, RMS_EPS, False, "rq")
            cqb = k.sb(st, "cqb", [128, 6, TC], BF16)
            k.op("pool", lambda e: e.tensor_copy(out=cqb[:], in_=cq[:]), reads=[cq], writes=[cqb])
            k.dma("sp", _fm(self.agQ.src.t[:, :]), cqb[:], reads=[cqb], writes=[self.agQ.src], sem=cqb)
            self.agQ.run()
            if with_kv:
                ck = k.sb(st, "ck", [128, 4, TC], F32)
                kn = k.sb(st, "kn", [128, 4], F32)
                k.dma("sp", kn[:], inp["kv_norm"][:, :], writes=[kn], sem=kn)

                def epi2(m, mr, t0, nt, ps):
                    k.op("act", lambda e: e.activation(out=ck[:, m, t0:t0 + nt], in_=ps[:, :nt], func=AF.Copy),
                         reads=[ps], writes=[ck])

                with k.scope() as st2:
                    self.linear_fm(st2, hTb, KC, TC, inp["w_dkv_c"], KVLORA, epi2, tag="wdkv")
                with k.scope() as st2:
                    self.norm_fm(st2, ck, 4, TC, kn, None, RMS_EPS, False, "rk")
                ckb = k.sb(st, "ckb", [128, 4, TC], BF16)
                k.op("pool", lambda e: e.tensor_copy(out=ckb[:], in_=ck[:]), reads=[ck], writes=[ckb])
                k.dma("sp", _fm(self.agKc.src.t[:, :]), ckb[:], reads=[ckb], writes=[self.agKc.src], sem=ckb)
                self.agKc.run()
                tabs = k.sb(st, "ktabs", [64, 2, TC], F32)
                k.dma("sp", tabs[:], self.agT.src.t[:, :].rearrange("(j p) t -> p j t", p=64), writes=[tabs], sem=tabs)
                kpa = k.sb(st, "kpa", [64, TC], F32); kpb = k.sb(st, "kpb", [64, TC], F32)
                krb = k.sb(st, "krb", [64, TC], BF16)

                def epi3(m, mr, t0, nt, ps):
                    k.op("dve", lambda e: e.tensor_tensor(out=kpa[:, t0:t0 + nt], in0=ps[:64, :nt], in1=tabs[:, 0, t0:t0 + nt],
                                                          op=ALU.mult), reads=[ps, tabs], writes=[kpa])

                def epi4(m, mr, t0, nt, ps):
                    k.op("dve", lambda e: e.tensor_tensor(out=kpb[:, t0:t0 + nt], in0=ps[:64, :nt], in1=tabs[:, 1, t0:t0 + nt],
                                                          op=ALU.mult), reads=[ps, tabs], writes=[kpb])

                with k.scope() as st2:
                    self.linear_fm(st2, hTb, KC, TC, inp["w_dkv_pe"], ROPE, epi3, tag="wkpe")
                with k.scope() as st2:
                    self.linear_fm(st2, hTb, KC, TC, inp["w_dkv_pesw"], ROPE, epi4, tag="wkpes")
                k.op("dve", lambda e: e.tensor_tensor(out=krb[:], in0=kpa[:], in1=kpb[:], op=ALU.add),
                     reads=[kpa, kpb], writes=[krb])
                k.dma("sp", self.agKp.src.t[:, :], krb[:], reads=[krb], writes=[self.agKp.src], sem=krb)
                self.agKp.run()
        return self.end_phase("q_%d" % j)

    def phase_attn(self, j):
        k = self.k
        inp = self.inp
        with k.scope() as st:
            KT = k.sb(st, "KT", [128, 2, SEQ], BF16)
            KPE = k.sb(st, "KPE", [64, SEQ], BF16)
            V = k.sb(st, "V", [128, SEQ // 128, 256], BF16)
            wk = k.sb(st, "wk", [128, 4, 256], BF16); wv = k.sb(st, "wv", [128, 4, 256], BF16)
            wqn = k.sb(st, "wqn", [128, 6, 256], BF16)
            wqp = k.sb(st, "wqp", [128, 6, 128], BF16); wqs = k.sb(st, "wqs", [128, 6, 128], BF16)
            k.dma("pool", wk[:], _fm(inp["w_ukv_k"][:, :]), writes=[wk], sem=wk)
            k.dma("pool", wv[:], _fm(inp["w_ukv_v"][:, :]), writes=[wv], sem=wv)
            k.dma("pool", wqn[:], _fm(inp["w_uq_n"][j]), writes=[wqn], sem=wqn)
            k.dma("pool", wqp[:], _fm(inp["w_uq_pe"][j]), writes=[wqp], sem=wqp)
            k.dma("pool", wqs[:], _fm(inp["w_uq_pesw"][j]), writes=[wqs], sem=wqs)
            msk = k.sb(st, "msk", [128, 4, 512], BF16)
            k.dma("pool", msk[:], inp["masks"][:, :, :], writes=[msk], sem=msk)
            with k.scope() as st2:
                cts = [k.sb(st2, "ct%d" % i, [128, 4, 512], BF16) for i in range(2)]
                for tt in range(16):
                    r, half = tt // 2, tt % 2
                    ct = cts[tt % 2]
                    self.load_fm_tile(ct, self.agKc, r, half, KVLORA)
                    k.dma("sp", KPE[:, tt * 512:(tt + 1) * 512], self.agKp.rows(r, 0, ROPE)[:, half * 512:(half + 1) * 512],
                          reads=[self.agKp.out], writes=[KPE], sem=KPE)
                    for hh in range(2):
                        ps = self.nps()
                        for kk in range(4):
                            k.op("pe", lambda e, kk=kk, ps=ps, hh=hh, ct=ct: e.matmul(
                                ps[:, :], lhsT=wk[:, kk, hh * 128:(hh + 1) * 128], rhs=ct[:, kk, :],
                                start=(kk == 0), stop=(kk == 3)), reads=[wk, ct], writes=[ps])
                        k.op("act", lambda e, ps=ps, hh=hh, tt=tt: e.activation(
                            out=KT[:, hh, tt * 512:(tt + 1) * 512], in_=ps[:, :], func=AF.Copy), reads=[ps], writes=[KT])
                    for kb4 in range(4):
                        ps = self.nps()
                        for kk in range(4):
                            k.op("pe", lambda e, kk=kk, ps=ps, kb4=kb4, ct=ct: e.matmul(
                                ps[:, :256], lhsT=ct[:, kk, kb4 * 128:(kb4 + 1) * 128], rhs=wv[:, kk, :],
                                start=(kk == 0), stop=(kk == 3)), reads=[wv, ct], writes=[ps])
                        k.op("dve", lambda e, ps=ps, kb4=kb4, tt=tt: e.tensor_copy(out=V[:, tt * 4 + kb4, :], in_=ps[:, :256]),
                             reads=[ps], writes=[V])
            cqs = [k.sb(st, "cqt%d" % i, [128, 6, 512], BF16) for i in range(2)]
            tbs = [k.sb(st, "tbt%d" % i, [64, 2, 512], F32) for i in range(2)]
            qnb = [k.sb(st, "qnb%d" % i, [128, 512], BF16) for i in range(2)]
            qpb = [k.sb(st, "qpb%d" % i, [64, 512], BF16) for i in range(2)]
            r1 = [k.sb(st, "r1_%d" % i, [64, 512], F32) for i in range(2)]
            r2 = [k.sb(st, "r2_%d" % i, [64, 512], F32) for i in range(2)]
            PT = [k.sb(st, "PT%d" % i, [128, 512], BF16) for i in range(3)]
            rl = k.sb(st, "rl", [128, 512], F32)
            ob = [k.sb(st, "ob%d" % i, [128, 512], BF16) for i in range(2)]
            psS = [self.ps[0], self.ps[1]]
            psO = [self.ps[2], self.ps[3]]
            psL = [self.ps[4], self.ps[5]]
            psQ = [self.ps[6], self.ps[7]]
            pti = 0
            si = 0

            def loadq(qt):
                r, half = qt // 2, qt % 2
                c = cqs[qt % 2]
                self.load_fm_tile(c, self.agQ, r, half, QLORA)
                t = tbs[qt % 2]
                k.dma("sp", t[:], self.agT.rows(r, 0, 128)[:, half * 512:(half + 1) * 512].rearrange(
                    "(j p) t -> p j t", p=64), reads=[self.agT.out], writes=[t], sem=t)

            loadq(0)
            for qt in range(16):
                if qt + 1 < 16:
                    loadq(qt + 1)
                c = cqs[qt % 2]
                tb = tbs[qt % 2]
                for hh in range(2):
                    qn_, qp_ = qnb[hh], qpb[hh]
                    ps = psQ[0]
                    for kk in range(6):
                        k.op("pe", lambda e, kk=kk, ps=ps: e.matmul(ps[:, :], lhsT=wqn[:, kk, hh * 128:(hh + 1) * 128],
                                                                    rhs=c[:, kk, :], start=(kk == 0), stop=(kk == 5)),
                             reads=[wqn, c], writes=[ps])
                    k.op("act", lambda e, ps=ps: e.activation(out=qn_[:], in_=ps[:, :], func=AF.Copy), reads=[ps], writes=[qn_])
                    ps = psQ[1]
                    for kk in range(6):
                        k.op("pe", lambda e, kk=kk, ps=ps: e.matmul(ps[:64, :], lhsT=wqp[:, kk, hh * 64:(hh + 1) * 64],
                                                                    rhs=c[:, kk, :], start=(kk == 0), stop=(kk == 5)),
                             reads=[wqp, c], writes=[ps])
                    k.op("dve", lambda e, ps=ps: e.tensor_tensor(out=r1[hh][:], in0=ps[:64, :], in1=tb[:, 0, :], op=ALU.mult),
                         reads=[ps, tb], writes=[r1[hh]])
                    ps = psQ[0]
                    for kk in range(6):
                        k.op("pe", lambda e, kk=kk, ps=ps: e.matmul(ps[:64, :], lhsT=wqs[:, kk, hh * 64:(hh + 1) * 64],
                                                                    rhs=c[:, kk, :], start=(kk == 0), stop=(kk == 5)),
                             reads=[wqs, c], writes=[ps])
                    k.op("dve", lambda e, ps=ps: e.tensor_tensor(out=r2[hh][:], in0=ps[:64, :], in1=tb[:, 1, :], op=ALU.mult),
                         reads=[ps, tb], writes=[r2[hh]])
                    k.op("dve", lambda e: e.tensor_tensor(out=qp_[:], in0=r1[hh][:], in1=r2[hh][:], op=ALU.add),
                         reads=[r1[hh], r2[hh]], writes=[qp_])
                    po, pl = psO[hh], psL[hh]
                    nkb = 4 * qt + 4
                    def qk(kb):
                        pss = psS[kb % 2]
                        k.op("pe", lambda e: e.matmul(pss[:, :], lhsT=KT[:, hh, kb * 128:(kb + 1) * 128],
                                                      rhs=qn_[:], start=True, stop=False),
                             reads=[KT, qn_], writes=[pss])
                        k.op("pe", lambda e: e.matmul(pss[:, :], lhsT=KPE[:, kb * 128:(kb + 1) * 128],
                                                      rhs=qp_[:], start=False, stop=True),
                             reads=[KPE, qp_], writes=[pss])

                    qk(0)
                    for kb in range(nkb):
                        if kb + 1 < nkb:
                            qk(kb + 1)
                        pss = psS[kb % 2]
                        p = PT[pti % 3]; pti += 1
                        k.op("act", lambda e, pss=pss, p=p: e.activation(out=p[:], in_=pss[:, :], func=AF.Exp,
                                                                         scale=float(ATTN_SCALE)), reads=[pss], writes=[p])
                        if kb >= 4 * qt:
                            jm = kb - 4 * qt
                            k.op("dve", lambda e, p=p, jm=jm: e.tensor_tensor(out=p[:], in0=p[:], in1=msk[:, jm, :],
                                                                              op=ALU.mult), reads=[p, msk], writes=[p])
                        k.op("pe", lambda e, p=p, kb=kb: e.matmul(po[:, :], lhsT=V[:, kb, hh * 128:(hh + 1) * 128], rhs=p[:],
                                                                  start=(kb == 0), stop=(kb == nkb - 1)),
                             reads=[V, p], writes=[po])
                        k.op("pe", lambda e, p=p, kb=kb: e.matmul(pl[:, :], lhsT=self.onesb[:], rhs=p[:],
                                                                  start=(kb == 0), stop=(kb == nkb - 1)),
                             reads=[self.onesb, p], writes=[pl])
                    k.op("dve", lambda e: e.reciprocal(out=rl[:], in_=pl[:, :]), reads=[pl], writes=[rl])
                    o = ob[hh]
                    k.op("dve", lambda e, o=o: e.tensor_tensor(out=o[:], in0=po[:, :], in1=rl[:], op=ALU.mult),
                         reads=[po, rl], writes=[o])
                    jb = qt // 2
                    k.dma("sp", self.agF.src.t[jb * 256 + hh * 128:jb * 256 + (hh + 1) * 128, (qt % 2) * 512:(qt % 2 + 1) * 512],
                          o[:], reads=[o], writes=[self.agF.src], sem=o)
                if qt % 4 == 3:
                    self.agF.run_chunk(qt // 4)
        return self.end_phase("attn_%d" % j)

    def phase_attn_out(self, l, j, hsrc):
        k = self.k
        with k.scope() as st:
            oT = k.sb(st, "oT", [128, KC, TC], BF16)
            rows = self.agF.out.t[:, :]
            for kk in range(KC):
                k.gather(oT[:, kk, :], rows, self.tF[:, kk:kk + 1], reads=[self.tF, self.agF.out], writes=[oT], sem=oT)
            self.mix_and_tail(l, st, oT, self.inp["w_o"][j], hsrc, "ao")
        return self.end_phase("ao_%d" % l)

    def build(self):
        nc = self.nc
        self.declare()
        self.scratch()
        k = self.k
        with contextlib.ExitStack() as cst:
            self.consts(cst)
            self._run()
            if self.stopped or self.dumps:
                self._dump()
            k.barrier()
        return nc

    def _run(self):
        if self.phase_init():
            return
        for l in range(DEPTH):
            hsrc = self.inp["xT"][:, :] if l == 0 else self.hT_d.t[:, :]
            hdst = self.outT[:, :] if l == DEPTH - 1 else self.hT_d.t[:, :]
            if l < N_A:
                if self.phase_rg1(l, hsrc):
                    return
                if self.phase_rg2(l):
                    return
                if self.phase_rg3(l, hsrc):
                    return
            else:
                j = l - N_A
                if self.phase_q(j, hsrc, j == 0):
                    return
                if self.phase_attn(j):
                    return
                if self.phase_attn_out(l, j, hsrc):
                    return
            if self.phase_moe(l):
                return
            if self.phase_post(l, hdst):
                return

    def _dump(self):
        k = self.k
        allb = {}
        for nm in ("hT_d", "h1T_d", "gb_d", "dbg_d", "dbg2_d"):
            allb[nm] = getattr(self, nm)
        for grp in ("agH", "agF", "agG", "agKc", "agKp", "agQ", "agT"):
            a = getattr(self, grp)
            for b in (a.src, a.mid, a.out):
                allb[b.name] = b
        for b in self.ym + self.ysp:
            allb[b.name] = b
        for a in (self.agHt, self.agL):
            for b in (a.src, a.mid, a.out):
                allb[b.name] = b
        for nm in self.dumps:
            b = allb[nm]
            shape = list(b.t.shape)
            o = self.nc.dram_tensor("dump_" + nm, shape, b.t.dtype, kind="ExternalOutput")
            ob = Buf(o, "dump_" + nm)
            rows = shape[0]
            step = max(1, rows // 8)
            for r0 in range(0, rows, step):
                k.dma("sp", o[r0:r0 + step, :], b.t[r0:r0 + step, :], reads=[b], writes=[ob], sem=ob)


def _cols(v):
    v = np.asarray(v, np.float32)
    lead = v.shape[:-1]
    n = v.shape[-1] // 128
    return np.ascontiguousarray(np.swapaxes(v.reshape(*lead, n, 128), -1, -2))


def make_in_maps(inp, used=None):
    f = lambda a: np.ascontiguousarray(np.asarray(a, np.float32))
    swap = np.concatenate([np.arange(32, 64), np.arange(0, 32)])
    pp = np.arange(128)[:, None]
    cache = {}

    def get(name):
        if name not in cache:
            cache[name] = np.asarray(inp[name])
        return cache[name]

    def ropec():
        inv = (1.0 / (10000.0 ** (np.arange(0, ROPE, 2, dtype=np.float32) / ROPE))).astype(np.float32)
        r = np.zeros((64, 2), np.float32)
        r[:, 0] = np.concatenate([inv, inv])
        r[:32, 1] = -1.0
        r[32:, 1] = 1.0
        return r

    def masks():
        qq = np.arange(512)[None, :]
        return f(np.stack([(pp + 128 * jm <= qq).astype(np.float32) for jm in range(4)], axis=1))

    def w_uq():
        return get("mla_w_uq").reshape(2, QLORA, 16, 192)

    def w_ukv():
        return get("kv_w_ukv").reshape(KVLORA, 16, 256)

    common = {
        "ln_mix_g": lambda: _cols(get("ln_mix_g")), "ln_mix_b": lambda: _cols(get("ln_mix_b")),
        "ln_ffn_g": lambda: _cols(get("ln_ffn_g")), "ln_ffn_b": lambda: _cols(get("ln_ffn_b")),
        "w_in_g": lambda: f(get("rg_w_in")[:, :, :D]), "w_out": lambda: f(get("rg_w_out")),
        "router_w": lambda: f(get("moe_router_w")),
        "router_b": lambda: f(np.broadcast_to(get("moe_router_b")[:, None, :], (DEPTH, 128, NEXP))),
        "ple_proj": lambda: f(get("ple_w_proj")), "ple_gate": lambda: f(get("ple_w_gate")),
        "w_dq": lambda: f(get("mla_w_dq")), "q_norm": lambda: _cols(get("mla_q_norm")), "w_o": lambda: f(get("mla_w_o")),
        "w_dkv_c": lambda: f(get("kv_w_dkv")[:, :KVLORA]), "w_dkv_pe": lambda: f(get("kv_w_dkv")[:, KVLORA:]),
        "w_dkv_pesw": lambda: f(get("kv_w_dkv")[:, KVLORA:][:, swap]),
        "kv_norm": lambda: _cols(get("kv_norm")),
        "ropec": ropec, "masks": masks, "ident": lambda: np.eye(128, dtype=np.float32),
        "ltri": lambda: np.triu(np.ones((128, 128), np.float32), 1),
        "iota": lambda: f(np.broadcast_to(np.arange(CAP, dtype=np.float32)[None, :], (128, CAP))),
        "dumprow": lambda: dumprow(),
    }

    def idx_t1(c):
        kk = np.arange(KC)[None, :]
        return ((kk * 128 + pp) * 8 + c).astype(np.int32)

    def idx_tF(c):
        ch, pq = _ag_layout(8 * 256, TC, 2)
        t = np.zeros((128, KC), np.int32)
        for kk in range(KC):
            for q in range(128):
                f = kk * 128 + q
                t[q, kk] = _ag_rowoff(ch, pq, f // 256, c * 256 + f % 256)
        return t

    def idx_L(c):
        t = np.zeros((128, EPC * NCORE * 2), np.int32)
        p = np.arange(128)
        for el in range(EPC):
            for r in range(NCORE):
                for sb in range(2):
                    t[:, (el * NCORE + r) * 2 + sb] = r * (2 * 128 * NEXP) + (sb * 128 + p) * NEXP + (c * EPC + el)
        return t

    def vals4(c):
        ch, pq = _ag_layout(TC, D, 2)
        v = np.zeros((128, 8, NEXP, 4), np.float32)
        for tk in range(8):
            for q in range(128):
                v[q, tk, :, 0] = _ag_rowoff(ch, pq, c, tk * 128 + q)
                v[q, tk, :, 1] = _ysp_row(c, tk, q)
        v[:, :, :, 2] = 1.0
        return v

    def idx_y(c):
        return (c * TC + np.arange(8)[None, :] * 128 + pp).astype(np.int32)

    def dumprow():
        return (SEQ + np.arange(2)[None, :] * 128 + pp).astype(np.float32)

    def idx_g(c):
        ig = np.zeros((128, EPC * 16), np.int32)
        for el in range(EPC):
            for tt in range(16):
                r, half = tt // 2, tt % 2
                ig[:, el * 16 + tt] = (r * NEXP + c * EPC + el) * 2 + half
        return ig

    ts = lambda c: slice(c * TC, (c + 1) * TC)
    cs = lambda c: slice(c * 256, (c + 1) * 256)
    es = lambda c: slice(c * EPC, (c + 1) * EPC)
    percore = {
        "xT": lambda c: f(get("x")[0, ts(c)].T),
        "pT": lambda c: f(np.swapaxes(get("p")[:, 0, ts(c)], 1, 2)),
        "pos": lambda c: np.ascontiguousarray(get("positions")[:, ts(c)].astype(np.int32)),
        "w_in_r": lambda c: f(get("rg_w_in")[:, :, D + c * 256:D + (c + 1) * 256]),
        "conv_w": lambda c: f(np.transpose(get("rg_conv_w")[:, :, cs(c)].reshape(N_A, 4, 2, 128), (0, 3, 2, 1))),
        "conv_b": lambda c: _cols(get("rg_conv_b")[:, cs(c)]),
        "ga_w": lambda c: f(get("rg_gate_a_w")[:, c]), "gx_w": lambda c: f(get("rg_gate_x_w")[:, c]),
        "ga_b": lambda c: _cols(get("rg_gate_a_b")[:, c]), "gx_b": lambda c: _cols(get("rg_gate_x_b")[:, c]),
        "lam": lambda c: _cols(get("rg_lambda")[:, cs(c)]),
        "w1": lambda c: f(get("moe_w1")[:, es(c)]), "b1": lambda c: _cols(get("moe_b1")[:, es(c)]),
        "w2": lambda c: f(get("moe_w2")[:, es(c)]), "b2": lambda c: _cols(get("moe_b2")[:, es(c)]),
        "w_uq_n": lambda c: f(w_uq()[:, :, 2 * c:2 * c + 2, :128].reshape(2, QLORA, 256)),
        "w_uq_pe": lambda c: f(w_uq()[:, :, 2 * c:2 * c + 2, 128:].reshape(2, QLORA, 128)),
        "w_uq_pesw": lambda c: f(w_uq()[:, :, 2 * c:2 * c + 2, 128:][..., swap].reshape(2, QLORA, 128)),
        "w_ukv_k": lambda c: f(w_ukv()[:, 2 * c:2 * c + 2, :128].reshape(KVLORA, 256)),
        "w_ukv_v": lambda c: f(w_ukv()[:, 2 * c:2 * c + 2, 128:].reshape(KVLORA, 256)),
        "idx_t1": idx_t1, "idx_tF": idx_tF, "idx_g": idx_g, "idx_L": idx_L, "idx_y": idx_y, "vals4": vals4,
        "b2row": lambda c: f(get("moe_b2")[:, es(c)]),
    }
    names = list(common) + list(percore) if used is None else list(used)
    shared = {n: common[n]() for n in names if n in common}
    maps = []
    for c in range(NCORE):
        m = dict(shared)
        for n in names:
            if n in percore:
                m[n] = percore[n](c)
        maps.append(m)
    return maps


def build_program(stop_after=None, dumps=(), debug=False):
    nc = bass.Bass("TRN2", target_bir_lowering=False)
    prog = Prog(nc, stop_after=stop_after, dumps=dumps, debug=debug)
    with nc.allow_low_precision("bf16 matmul operands, fp32 accumulation (reference tolerance is bf16-level)"):
        prog.build()
    return nc, prog


def kernel(**inputs):
    nc, prog = build_program()
    maps = make_in_maps(inputs, used=sorted(prog.inp.keys()))
    res = run_bass_kernel_spmd(nc, maps, core_ids=list(range(NCORE)))
    out = np.concatenate([np.asarray(r["outT"], np.float32).T for r in res.results], axis=0)
    return np.ascontiguousarray(out[None]).astype(np.float32)
```

```python
import contextlib
import math
import numpy as np
import concourse.bass as bass
import concourse.mybir as mybir
from concourse.bass_utils import run_bass_kernel_spmd

F32 = mybir.dt.float32
BF16 = mybir.dt.bfloat16
I32 = mybir.dt.int32
AF = mybir.ActivationFunctionType
ALU = mybir.AluOpType
AX = mybir.AxisListType

NCORE = 8
D = 2048
KC = D // 128
SEQ = 8192
TC = SEQ // NCORE
DEPTH = 4
N_A = 2
ALPHA = (2 * DEPTH) ** 0.25
LN_EPS = 1e-5
RMS_EPS = 1e-6
NEXP = 32
EPC = NEXP // NCORE
CAP = 256
NSLOT = NCORE * CAP
DEXP = 1024
QLORA = 768
KVLORA = 512
ROPE = 64
ATTN_SCALE = (128 + 64) ** -0.5
G4 = [[0, 1, 2, 3], [4, 5, 6, 7]]
G2 = [[0, 4], [1, 5], [2, 6], [3, 7]]


class Buf:
    def __init__(self, t, name):
        self.t = t
        self.name = name
        self.w = {}
        self.r = {}
        self.dsem = None

    def __getitem__(self, idx):
        return self.t[idx]


class Ker:
    NDSEM = 48

    def __init__(self, nc):
        self.nc = nc
        self.es = contextlib.ExitStack()
        self.eng = dict(pe=nc.tensor, dve=nc.vector, act=nc.scalar, pool=nc.gpsimd, sp=nc.sync)
        self.semh = {}
        self.semcur = {}
        self.waited = {}
        for e in ("pe", "dve", "act", "pool", "cc"):
            self._mksem("s_" + e)
        self.free_dsem = []
        for i in range(self.NDSEM):
            self._mksem("d%d" % i)
            self.free_dsem.append("d%d" % i)
        self.phase_dsem = []
        self.phase_bufs = []
        self.freed = {}
        self.ninst = 0

    def _mksem(self, name):
        self.semh[name] = self.es.enter_context(self.nc.semaphore(name))
        self.semcur[name] = 0

    def sb(self, st, name, shape, dt):
        self.nsb = getattr(self, "nsb", 0) + 1
        b = Buf(st.enter_context(self.nc.sbuf_tensor("sb%d_%s" % (self.nsb, name), list(shape), dt)), name)
        b.w = dict(self.freed)
        if hasattr(st, "bufs"):
            st.bufs.append(b)
        self.phase_bufs.append(b)
        return b

    @contextlib.contextmanager
    def scope(self):
        with contextlib.ExitStack() as st:
            st.bufs = []
            yield st
            for b in st.bufs:
                self._merge(self.freed, b.w)
                self._merge(self.freed, b.r)

    def dram(self, name, shape, dt):
        return Buf(self.nc.dram_tensor(name, list(shape), dt), name)

    def _dsem(self, b):
        if b.dsem is None:
            b.dsem = self.free_dsem.pop()
            self.phase_dsem.append(b)
        return b.dsem

    def _wait(self, e, deps):
        for s, v in deps.items():
            if e == "pe" and s == "s_pe":
                continue
            if self.waited.get((e, s), 0) < v:
                self.eng[e].wait_ge(self.semh[s], v)
                self.waited[(e, s)] = v
                self.ninst += 1

    @staticmethod
    def _merge(d, o):
        for s, v in o.items():
            if d.get(s, 0) < v:
                d[s] = v

    def _deps(self, reads, writes):
        deps = {}
        for b in reads:
            self._merge(deps, b.w)
        for b in writes:
            self._merge(deps, b.w)
            self._merge(deps, b.r)
        return deps

    def _commit(self, ev, reads, writes):
        s, v = ev
        for b in reads:
            if b.r.get(s, 0) < v:
                b.r[s] = v
        for b in writes:
            if b.w.get(s, 0) < v:
                b.w[s] = v
            b.r = {}

    def op(self, e, fn, reads=(), writes=()):
        self._wait(e, self._deps(reads, writes))
        ins = fn(self.eng[e])
        s = "s_" + e
        self.semcur[s] += 1
        ins.then_inc(self.semh[s], 1)
        self._commit((s, self.semcur[s]), reads, writes)
        self.ninst += 1

    def dma(self, q, out, in_, reads=(), writes=(), sem=None, **kw):
        self._wait(q, self._deps(reads, writes))
        s = self._dsem(sem)
        ins = self.eng[q].dma_start(out=out, in_=in_, **kw)
        self.semcur[s] += 16
        ins.then_inc(self.semh[s], 16)
        self._commit((s, self.semcur[s]), reads, writes)
        self.ninst += 1

    def gather(self, out, src_rows, idx_ap, reads=(), writes=(), sem=None):
        self._wait("pool", self._deps(reads, writes))
        s = self._dsem(sem)
        ins = self.nc.gpsimd.indirect_dma_start(
            out=out, out_offset=None, in_=src_rows,
            in_offset=bass.IndirectOffsetOnAxis(ap=idx_ap, axis=0))
        self.semcur[s] += 16
        ins.then_inc(self.semh[s], 16)
        self._commit((s, self.semcur[s]), reads, writes)
        self.ninst += 1

    def scatter_add(self, dst_rows, idx_ap, src, reads=(), writes=(), sem=None, extra_deps=None):
        deps = self._deps(reads, ())
        if extra_deps:
            self._merge(deps, extra_deps)
        self._wait("pool", deps)
        s = self._dsem(sem)
        ins = self.nc.gpsimd.indirect_dma_start(
            out=dst_rows, out_offset=bass.IndirectOffsetOnAxis(ap=idx_ap, axis=0), in_=src, in_offset=None,
            compute_op=ALU.add)
        self.semcur[s] += 16
        ins.then_inc(self.semh[s], 16)
        ev = (s, self.semcur[s])
        self._commit(ev, reads, ())
        self.ninst += 1
        return ev

    def collective(self, kind, groups, src, dst, src_ap, dst_ap):
        self._wait("pool", self._deps([src], [dst]))
        op = ALU.add if kind in ("AllReduce", "ReduceScatter") else ALU.bypass
        ins = self.nc.gpsimd.collective_compute(kind, op, replica_groups=groups, ins=[src_ap], outs=[dst_ap])
        self.semcur["s_cc"] += 1
        ins.then_inc(self.semh["s_cc"], 1)
        self._commit(("s_cc", self.semcur["s_cc"]), [src], [dst])
        self.ninst += 1

    def barrier(self):
        for e in ("pe", "dve", "act", "pool", "sp"):
            for s, v in self.semcur.items():
                if v > 0 and self.waited.get((e, s), 0) < v:
                    if e == "pe" and s == "s_pe":
                        continue
                    self.eng[e].wait_ge(self.semh[s], v)
                    self.waited[(e, s)] = v
                    self.ninst += 1
        for b in self.phase_dsem:
            self.free_dsem.append(b.dsem)
            b.dsem = None
        self.phase_dsem = []
        self.phase_bufs = []
        self.freed = {}


class _LazyInputs(dict):
    def __init__(self, prog):
        super().__init__()
        self.prog = prog

    def __missing__(self, name):
        shape, dt = self.prog.shapes[name]
        t = self.prog.nc.dram_tensor(name, list(shape), dt, kind="ExternalInput")
        self[name] = t
        return t


def _fm(ap, p=128):
    return ap.rearrange("(k p) n -> p k n", p=p)


CC_MAX = 1 << 20


def _ag_layout(R, C, es):
    ch = R
    while ch * C * es > CC_MAX:
        assert ch % 2 == 0
        ch //= 2
    pq = 4
    while pq * ch * C * es > 2 * CC_MAX:
        pq //= 2
    return ch, pq


def _ysp_row(c, tk, m):
    g, q = c // 4, c % 4
    return (2 * tk + q // 2) * 512 + g * 256 + (q % 2) * 128 + m


def _ag_rowoff(ch, pq, r, rho):
    i, rp = rho // ch, rho % ch
    g, q = r // 4, r % 4
    hq, ql = q // pq, q % pq
    return ((((i * (4 // pq) + hq) * 2 + g) * pq + ql) * ch + rp)


class AG:
    def __init__(self, k, name, R, C, dt, es):
        self.k = k
        self.R, self.C = R, C
        self.ch, self.pq = _ag_layout(R, C, es)
        self.src = k.dram(name + "_in", [R, C], dt)
        self.mid = k.dram(name + "_mid", [4 * R, C], dt)
        self.out = k.dram(name + "_out", [8 * R, C], dt)

    def rowoff(self, r, rho):
        return _ag_rowoff(self.ch, self.pq, r, rho)

    def rows(self, r, rho, n):
        assert rho // self.ch == (rho + n - 1) // self.ch
        o = self.rowoff(r, rho)
        return self.out.t[o:o + n, :]

    @property
    def nchunk(self):
        return self.R // self.ch

    def run_chunk(self, i):
        k = self.k
        ch, pq = self.ch, self.pq
        k.collective("AllGather", G4, self.src, self.mid, self.src.t[i * ch:(i + 1) * ch, :],
                     self.mid.t[i * 4 * ch:(i + 1) * 4 * ch, :])
        for hq in range(4 // pq):
            a = (i * 4 + hq * pq) * ch
            b = ((i * (4 // pq) + hq) * 2) * pq * ch
            k.collective("AllGather", G2, self.mid, self.out, self.mid.t[a:a + pq * ch, :],
                         self.out.t[b:b + 2 * pq * ch, :])

    def run(self):
        for i in range(self.nchunk):
            self.run_chunk(i)


class Prog:
    def __init__(self, nc, stop_after=None, dumps=(), debug=False):
        self.nc = nc
        self.debug = debug
        self.k = Ker(nc)
        self.stop_after = stop_after
        self.dumps = list(dumps)
        self.stopped = False
        self.inp = _LazyInputs(self)
        self.out_dumps = {}

    def din(self, name, shape, dt=F32):
        t = self.nc.dram_tensor(name, list(shape), dt, kind="ExternalInput")
        self.inp[name] = t
        return t

    def declare(self):
        self.shapes = {}

        def d(name, shape, dt=F32):
            self.shapes[name] = (shape, dt)
        d("xT", [D, TC]); d("pT", [DEPTH, 256, TC]); d("pos", [1, TC], I32)
        d("ln_mix_g", [DEPTH, 128, KC]); d("ln_mix_b", [DEPTH, 128, KC])
        d("ln_ffn_g", [DEPTH, 128, KC]); d("ln_ffn_b", [DEPTH, 128, KC])
        d("w_in_g", [N_A, D, D]); d("w_in_r", [N_A, D, 256]); d("w_out", [N_A, D, D])
        d("conv_w", [N_A, 128, 2, 4]); d("conv_b", [N_A, 128, 2])
        d("ga_w", [N_A, 256, 256]); d("ga_b", [N_A, 128, 2]); d("gx_w", [N_A, 256, 256]); d("gx_b", [N_A, 128, 2])
        d("lam", [N_A, 128, 2])
        d("router_w", [DEPTH, D, NEXP]); d("router_b", [DEPTH, 128, NEXP])
        d("w1", [DEPTH, EPC, D, 2 * DEXP]); d("b1", [DEPTH, EPC, 128, 16])
        d("w2", [DEPTH, EPC, DEXP, D]); d("b2", [DEPTH, EPC, 128, 16])
        d("ple_proj", [DEPTH, 256, D]); d("ple_gate", [DEPTH, D, D])
        d("w_dq", [2, D, QLORA]); d("q_norm", [2, 128, 6]); d("w_o", [2, D, D])
        d("w_uq_n", [2, QLORA, 256]); d("w_uq_pe", [2, QLORA, 128]); d("w_uq_pesw", [2, QLORA, 128])
        d("w_dkv_c", [D, KVLORA]); d("w_dkv_pe", [D, ROPE]); d("w_dkv_pesw", [D, ROPE]); d("kv_norm", [128, 4])
        d("w_ukv_k", [KVLORA, 256]); d("w_ukv_v", [KVLORA, 256])
        d("ropec", [64, 2]); d("masks", [128, 4, 512]); d("ident", [128, 128])
        d("idx_t1", [128, KC], I32); d("idx_tF", [128, KC], I32)
        d("ltri", [128, 128]); d("iota", [128, CAP]); d("vals4", [128, 8, NEXP, 4]); d("dumprow", [128, 2])
        d("idx_L", [128, EPC * NCORE * 2], I32); d("idx_y", [128, 8], I32); d("b2row", [DEPTH, EPC, D]); d("idx_g", [128, EPC * 16], I32)
        self.outT = self.nc.dram_tensor("outT", [D, TC], F32, kind="ExternalOutput")

    def scratch(self):
        k = self.k
        self.hT_d = k.dram("hT_d", [D, TC], F32)
        self.h1T_d = k.dram("h1T_d", [D, TC], F32)
        self.gb_d = k.dram("gb_d", [D, TC], BF16)
        self.agH = AG(k, "agH", D, TC, BF16, 2)
        self.agF = AG(k, "agF", 8 * 256, TC, BF16, 2)
        self.agG = AG(k, "agG", NEXP, TC, F32, 4)
        self.agKc = AG(k, "agKc", KVLORA, TC, BF16, 2)
        self.agKp = AG(k, "agKp", ROPE, TC, BF16, 2)
        self.agQ = AG(k, "agQ", QLORA, TC, BF16, 2)
        self.agT = AG(k, "agT", 128, TC, F32, 4)
        self.agHt = AG(k, "agHt", TC, D, BF16, 2)
        self.agL = AG(k, "agL", 2 * 128 * NEXP, 4, F32, 4)
        self.ysp = [k.dram("ysp%d" % i, [SEQ + CAP, D], F32) for i in range(3)]
        self.dbg_d = k.dram("dbg_d", [D, TC], F32)
        self.dbg2_d = k.dram("dbg2_d", [D, TC], BF16)
        self.ym = [k.dram("ym0", [D, SEQ], F32), k.dram("ym1", [D, SEQ], F32), k.dram("ym2", [D, SEQ], F32)]

    def end_phase(self, name):
        self.k.barrier()
        if self.stop_after == name:
            self.stopped = True
        return self.stopped

    def consts(self, st):
        k = self.k
        self.ps = []
        for i in range(8):
            self.ps.append(Buf(st.enter_context(self.nc.psum_tensor("ps%d" % i, [128, 512], F32)), "ps%d" % i))
        self.ones32 = k.sb(st, "ones32", [128, 128], F32)
        self.onesb = k.sb(st, "onesb", [128, 128], BF16)
        self.ident = k.sb(st, "ident", [128, 128], F32)
        self.t1 = k.sb(st, "t1", [128, KC], I32)
        k.op("dve", lambda e: e.memset(self.ones32[:], 1.0), writes=[self.ones32])
        k.op("dve", lambda e: e.memset(self.onesb[:], 1.0), writes=[self.onesb])
        k.dma("sp", self.ident[:], self.inp["ident"][:, :], writes=[self.ident], sem=self.ident)
        k.dma("sp", self.t1[:], self.inp["idx_t1"][:, :], writes=[self.t1], sem=self.t1)
        self.identb = k.sb(st, "identb", [128, 128], BF16)
        k.op("dve", lambda e: e.tensor_copy(out=self.identb[:], in_=self.ident[:]), reads=[self.ident], writes=[self.identb])
        self.tF = k.sb(st, "tF", [128, KC], I32)
        k.dma("sp", self.tF[:], self.inp["idx_tF"][:, :], writes=[self.tF], sem=self.tF)
        self.psi = 0

    def load_fm_tile(self, x, ag, r, half, nrows):
        k = self.k
        ch = ag.ch
        for r0 in range(0, nrows, ch):
            n = min(ch, nrows - r0)
            k.dma("sp", x[:, r0 // 128:(r0 + n) // 128, :], _fm(ag.rows(r, r0, n)[:, half * 512:(half + 1) * 512]),
                  reads=[ag.out], writes=[x], sem=x)

    def nps(self):
        p = self.ps[self.psi % 8]
        self.psi += 1
        return p

    def linear_fm(self, st, xT, kc, ntok, wsrc, M, epi, mblk=512, tile=512, tag="w", on_load=None):
        k = self.k
        mblk = min(mblk, M)
        nblk = (M + mblk - 1) // mblk
        ws = [k.sb(st, "%s_s%d" % (tag, i), [128, kc, mblk], BF16) for i in range(min(2, nblk))]

        def load(bi):
            w = ws[bi % len(ws)]
            m0 = bi * mblk
            mw = min(mblk, M - m0)
            k.dma("pool", w[:, :, :mw], _fm(wsrc[:, m0:m0 + mw]), writes=[w], sem=w)
            if on_load is not None:
                on_load(bi)

        load(0)
        for bi in range(nblk):
            if bi + 1 < nblk:
                load(bi + 1)
            w = ws[bi % len(ws)]
            m0 = bi * mblk
            mw = min(mblk, M - m0)
            for mi in range((mw + 127) // 128):
                mr = min(128, mw - mi * 128)
                for t0 in range(0, ntok, tile):
                    ps = self.nps()
                    for kk in range(kc):
                        k.op("pe", lambda e, kk=kk, ps=ps, w=w, mi=mi, mr=mr, t0=t0: e.matmul(
                            ps[:mr, :tile], lhsT=w[:, kk, mi * 128:mi * 128 + mr], rhs=xT[:, kk, t0:t0 + tile],
                            start=(kk == 0), stop=(kk == kc - 1)), reads=[w, xT], writes=[ps])
                    epi((m0 // 128) + mi, mr, t0, tile, ps)

    def norm_fm(self, st, z, kc, ntok, gcol, bcol, eps, center, tag):
        k = self.k
        nfeat = kc * 128
        sq = [k.sb(st, "%s_sq%d" % (tag, i), [128, 512], F32) for i in range(2)]
        mean = k.sb(st, tag + "_mean", [128, 512], F32)
        rstd = k.sb(st, tag + "_rstd", [128, 512], F32)
        tmp = [k.sb(st, "%s_tmp%d" % (tag, i), [128, 512], F32) for i in range(2)]
        for t0 in range(0, ntok, 512):
            sl = slice(t0, t0 + 512)
            ps_s = self.nps()
            ps_m = self.nps() if center else None
            for kk in range(kc):
                s = sq[kk % 2]
                k.op("act", lambda e, s=s, kk=kk: e.activation(out=s[:], in_=z[:, kk, sl], func=AF.Square),
                     reads=[z], writes=[s])
                k.op("pe", lambda e, s=s, kk=kk: e.matmul(ps_s[:, :], lhsT=self.ones32[:], rhs=s[:],
                                                          start=(kk == 0), stop=(kk == kc - 1)),
                     reads=[self.ones32, s], writes=[ps_s])
                if center:
                    k.op("pe", lambda e, kk=kk: e.matmul(ps_m[:, :], lhsT=self.ones32[:], rhs=z[:, kk, sl],
                                                         start=(kk == 0), stop=(kk == kc - 1)),
                         reads=[self.ones32, z], writes=[ps_m])
            if center:
                k.op("act", lambda e: e.activation(out=mean[:], in_=ps_m[:, :], func=AF.Copy, scale=1.0 / nfeat),
                     reads=[ps_m], writes=[mean])
                k.op("dve", lambda e: e.tensor_tensor(out=rstd[:], in0=mean[:], in1=mean[:], op=ALU.mult),
                     reads=[mean], writes=[rstd])
                k.op("dve", lambda e: e.scalar_tensor_tensor(out=rstd[:], in0=ps_s[:, :], scalar=1.0 / nfeat,
                                                             in1=rstd[:], op0=ALU.mult, op1=ALU.subtract),
                     reads=[ps_s, rstd], writes=[rstd])
                k.op("dve", lambda e: e.tensor_scalar(out=rstd[:], in0=rstd[:], scalar1=float(eps), scalar2=None,
                                                      op0=ALU.add), reads=[rstd], writes=[rstd])
            else:
                k.op("dve", lambda e: e.tensor_scalar(out=rstd[:], in0=ps_s[:, :], scalar1=1.0 / nfeat,
                                                      scalar2=float(eps), op0=ALU.mult, op1=ALU.add),
                     reads=[ps_s], writes=[rstd])
            k.op("act", lambda e: e.activation(out=rstd[:], in_=rstd[:], func=AF.Sqrt), reads=[rstd], writes=[rstd])
            k.op("dve", lambda e: e.reciprocal(out=rstd[:], in_=rstd[:]), reads=[rstd], writes=[rstd])
            for kk in range(kc):
                t = tmp[kk % 2]
                if center:
                    k.op("dve", lambda e, t=t, kk=kk: e.tensor_tensor(out=t[:], in0=z[:, kk, sl], in1=mean[:],
                                                                      op=ALU.subtract), reads=[z, mean], writes=[t])
                    k.op("pool", lambda e, t=t: e.tensor_tensor(out=t[:], in0=t[:], in1=rstd[:], op=ALU.mult),
                         reads=[t, rstd], writes=[t])
                else:
                    k.op("dve", lambda e, t=t, kk=kk: e.tensor_tensor(out=t[:], in0=z[:, kk, sl], in1=rstd[:],
                                                                      op=ALU.mult), reads=[z, rstd], writes=[t])
                if bcol is not None:
                    k.op("act", lambda e, t=t, kk=kk: e.activation(out=z[:, kk, sl], in_=t[:], func=AF.Identity,
                                                                   scale=gcol[:, kk:kk + 1], bias=bcol[:, kk:kk + 1]),
                         reads=[t, gcol, bcol], writes=[z])
                else:
                    k.op("act", lambda e, t=t, kk=kk: e.activation(out=z[:, kk, sl], in_=t[:], func=AF.Identity,
                                                                   scale=gcol[:, kk:kk + 1]),
                         reads=[t, gcol], writes=[z])

    def phase_init(self):
        k = self.k
        with k.scope() as st:
            posi = k.sb(st, "posi", [64, TC], I32)
            ang = k.sb(st, "ang", [64, TC], F32)
            rc = k.sb(st, "rc", [64, 2], F32)
            k.dma("sp", posi[:], self.inp["pos"][0:1, :].partition_broadcast(64), writes=[posi], sem=posi)
            k.dma("sp", rc[:], self.inp["ropec"][:, :], writes=[rc], sem=rc)
            k.op("dve", lambda e: e.tensor_copy(out=ang[:], in_=posi[:]), reads=[posi], writes=[ang])
            k.op("dve", lambda e: e.tensor_scalar(out=ang[:], in0=ang[:], scalar1=rc[:, 0:1],
                                                  scalar2=1.0 / (2 * math.pi), op0=ALU.mult, op1=ALU.mult),
                 reads=[ang, rc], writes=[ang])
            tabs = k.sb(st, "tabs", [64, 2, TC], F32)
            ni = k.sb(st, "ni", [64, TC], I32)
            nf = k.sb(st, "nf", [64, TC], F32)
            fr = k.sb(st, "fr", [64, TC], F32)
            for j, shift in enumerate((0.25, 0.0)):
                k.op("dve", lambda e, shift=shift: e.tensor_scalar(out=fr[:], in0=ang[:], scalar1=float(shift),
                                                                   scalar2=None, op0=ALU.add),
                     reads=[ang], writes=[fr])
                k.op("dve", lambda e: e.tensor_copy(out=ni[:], in_=fr[:]), reads=[fr], writes=[ni])
                k.op("dve", lambda e: e.tensor_copy(out=nf[:], in_=ni[:]), reads=[ni], writes=[nf])
                k.op("dve", lambda e: e.tensor_tensor(out=fr[:], in0=fr[:], in1=nf[:], op=ALU.subtract),
                     reads=[fr, nf], writes=[fr])
                k.op("dve", lambda e: e.tensor_scalar(out=nf[:], in0=fr[:], scalar1=0.5, scalar2=None, op0=ALU.is_gt),
                     reads=[fr], writes=[nf])
                k.op("dve", lambda e: e.tensor_tensor(out=fr[:], in0=fr[:], in1=nf[:], op=ALU.subtract),
                     reads=[fr, nf], writes=[fr])
                k.op("dve", lambda e: e.tensor_scalar(out=nf[:], in0=fr[:], scalar1=-0.5, scalar2=None, op0=ALU.is_lt),
                     reads=[fr], writes=[nf])
                k.op("dve", lambda e: e.tensor_tensor(out=fr[:], in0=fr[:], in1=nf[:], op=ALU.add),
                     reads=[fr, nf], writes=[fr])
                k.op("act", lambda e, j=j: e.activation(out=tabs[:, j, :], in_=fr[:], func=AF.Sin,
                                                        scale=2 * math.pi), reads=[fr], writes=[tabs])
            k.op("dve", lambda e: e.tensor_scalar(out=tabs[:, 1, :], in0=tabs[:, 1, :], scalar1=rc[:, 1:2],
                                                  scalar2=None, op0=ALU.mult), reads=[tabs, rc], writes=[tabs])
            k.dma("sp", self.agT.src.t[:, :].rearrange("(j p) t -> p j t", p=64), tabs[:], reads=[tabs],
                  writes=[self.agT.src], sem=tabs)
            self.agT.run()
        return self.end_phase("init")

    def phase_rg1(self, l, hsrc):
        k = self.k
        with k.scope() as st:
            hTb = k.sb(st, "hTb", [128, KC, TC], BF16)
            k.dma("pool", hTb[:], _fm(hsrc), writes=[hTb], sem=hTb)
            k.dma("sp", _fm(self.agH.src.t[:, :]), hTb[:], reads=[hTb], writes=[self.agH.src], sem=hTb)
            gst = [k.sb(st, "gst%d" % i, [128, TC], BF16) for i in range(2)]

            def epi(m, mr, t0, nt, ps):
                g = gst[m % 2]
                k.op("act", lambda e: e.activation(out=g[:, t0:t0 + nt], in_=ps[:, :nt], func=AF.Gelu_apprx_tanh),
                     reads=[ps], writes=[g])
                if t0 + nt == TC:
                    k.dma("sp", self.gb_d.t[m * 128:(m + 1) * 128, :], g[:], reads=[g], writes=[self.gb_d], sem=g)

            self.linear_fm(st, hTb, KC, TC, self.inp["w_in_g"][l], D, epi, tag="wg",
                           on_load=lambda bi: self.agH.run_chunk(bi) if bi < self.agH.nchunk else None)
        return self.end_phase("rg1_%d" % l)

    def phase_rg2(self, l):
        k = self.k
        inp = self.inp
        with k.scope() as st:
            wr = k.sb(st, "wr", [128, KC, 256], BF16)
            k.dma("pool", wr[:], _fm(inp["w_in_r"][l]), writes=[wr], sem=wr)
            gw = []
            for nm in ("ga_w", "gx_w"):
                g = k.sb(st, nm, [128, 2, 256], BF16)
                k.dma("pool", g[:], _fm(inp[nm][l]), writes=[g], sem=g)
                gw.append(g)
            cw = k.sb(st, "cw", [128, 2, 4], F32); cb = k.sb(st, "cb", [128, 2], F32)
            gab = k.sb(st, "gab", [128, 2], F32); gxb = k.sb(st, "gxb", [128, 2], F32)
            lam = k.sb(st, "lam", [128, 2], F32); c1 = k.sb(st, "c1", [128, 2], F32)
            for b, nm in ((cw, "conv_w"), (cb, "conv_b"), (gab, "ga_b"), (gxb, "gx_b"), (lam, "lam")):
                k.dma("sp", b[:], inp[nm][l], writes=[b], sem=b)
            k.op("act", lambda e: e.activation(out=c1[:], in_=lam[:], func=AF.Exp, scale=-1.0), reads=[lam], writes=[c1])
            k.op("dve", lambda e: e.tensor_scalar(out=c1[:], in0=c1[:], scalar1=1.0, scalar2=None, op0=ALU.add),
                 reads=[c1], writes=[c1])
            k.op("act", lambda e: e.activation(out=c1[:], in_=c1[:], func=AF.Ln), reads=[c1], writes=[c1])
            k.op("dve", lambda e: e.tensor_scalar(out=c1[:], in0=c1[:], scalar1=-8.0, scalar2=None, op0=ALU.mult),
                 reads=[c1], writes=[c1])
            xts = [k.sb(st, "xt%d" % i, [128, KC, 512], BF16) for i in range(2)]
            ub = [k.sb(st, "ub%d" % i, [128, 515], F32) for i in range(2)]
            xc = [k.sb(st, "xc%d" % i, [128, 512], F32) for i in range(2)]
            xcb = k.sb(st, "xcb", [128, 2, 512], BF16)
            rg = [k.sb(st, "rg%d" % i, [128, 512], F32) for i in range(2)]
            ig = [k.sb(st, "ig%d" % i, [128, 512], F32) for i in range(2)]
            av = [k.sb(st, "av%d" % i, [128, 512], F32) for i in range(2)]
            bv = [k.sb(st, "bv%d" % i, [128, 512], F32) for i in range(2)]
            rec = [k.sb(st, "rec%d" % i, [128, 512], F32) for i in range(2)]
            hst = [k.sb(st, "hst%d" % i, [128, 1], F32) for i in range(2)]
            recb = [k.sb(st, "recb%d" % i, [128, 2, 512], BF16) for i in range(2)]
            for i in range(2):
                k.op("dve", lambda e, i=i: e.memset(ub[i][:], 0.0), writes=[ub[i]])
                k.op("dve", lambda e, i=i: e.memset(hst[i][:], 0.0), writes=[hst[i]])
            def load(tt):
                self.load_fm_tile(xts[tt % 2], self.agH, tt // 2, tt % 2, D)

            load(0)
            for tt in range(16):
                if tt + 1 < 16:
                    load(tt + 1)
                x = xts[tt % 2]
                rb = recb[tt % 2]
                for mi in range(2):
                    u = ub[mi]
                    if tt > 0:
                        k.op("dve", lambda e, u=u: e.tensor_copy(out=u[:, 0:3], in_=u[:, 512:515]), reads=[u], writes=[u])
                    ps = self.nps()
                    for kk in range(KC):
                        k.op("pe", lambda e, kk=kk, ps=ps, mi=mi, x=x: e.matmul(
                            ps[:, :], lhsT=wr[:, kk, mi * 128:(mi + 1) * 128], rhs=x[:, kk, :],
                            start=(kk == 0), stop=(kk == KC - 1)), reads=[wr, x], writes=[ps])
                    k.op("act", lambda e, u=u, ps=ps: e.activation(out=u[:, 3:515], in_=ps[:, :], func=AF.Copy),
                         reads=[ps], writes=[u])
                    c = xc[mi]
                    k.op("dve", lambda e, c=c, u=u, mi=mi: e.tensor_scalar(
                        out=c[:], in0=u[:, 0:512], scalar1=cw[:, mi, 0:1], scalar2=cb[:, mi:mi + 1],
                        op0=ALU.mult, op1=ALU.add), reads=[u, cw, cb], writes=[c])
                    for j in range(1, 4):
                        k.op("dve", lambda e, c=c, u=u, mi=mi, j=j: e.scalar_tensor_tensor(
                            out=c[:], in0=u[:, j:j + 512], scalar=cw[:, mi, j:j + 1], in1=c[:],
                            op0=ALU.mult, op1=ALU.add), reads=[u, cw, c], writes=[c])
                    k.op("act", lambda e, c=c, mi=mi: e.activation(out=xcb[:, mi, :], in_=c[:], func=AF.Copy),
                         reads=[c], writes=[xcb])
                for gi, (dst, bias) in enumerate(((rg, gab), (ig, gxb))):
                    for mo in range(2):
                        ps = self.nps()
                        for ki in range(2):
                            k.op("pe", lambda e, ps=ps, ki=ki, mo=mo, gi=gi: e.matmul(
                                ps[:, :], lhsT=gw[gi][:, ki, mo * 128:(mo + 1) * 128], rhs=xcb[:, ki, :],
                                start=(ki == 0), stop=(ki == 1)), reads=[gw[gi], xcb], writes=[ps])
                        k.op("act", lambda e, ps=ps, mo=mo, dst=dst, bias=bias: e.activation(
                            out=dst[mo][:], in_=ps[:, :], func=AF.Sigmoid, bias=bias[:, mo:mo + 1]),
                             reads=[ps, bias], writes=[dst[mo]])
                for mo in range(2):
                    a, b, c = av[mo], bv[mo], xc[mo]
                    k.op("act", lambda e, a=a, mo=mo: e.activation(out=a[:], in_=rg[mo][:], func=AF.Exp,
                                                                   scale=c1[:, mo:mo + 1]), reads=[rg[mo], c1], writes=[a])
                    k.op("dve", lambda e, a=a, b=b: e.tensor_tensor(out=b[:], in0=a[:], in1=a[:], op=ALU.mult),
                         reads=[a], writes=[b])
                    k.op("dve", lambda e, b=b: e.tensor_scalar(out=b[:], in0=b[:], scalar1=-1.0, scalar2=1.0,
                                                               op0=ALU.mult, op1=ALU.add), reads=[b], writes=[b])
                    k.op("dve", lambda e, b=b: e.tensor_scalar(out=b[:], in0=b[:], scalar1=0.0, scalar2=None,
                                                               op0=ALU.max), reads=[b], writes=[b])
                    k.op("act", lambda e, b=b: e.activation(out=b[:], in_=b[:], func=AF.Sqrt), reads=[b], writes=[b])
                    k.op("dve", lambda e, c=c, mo=mo: e.tensor_tensor(out=c[:], in0=c[:], in1=ig[mo][:], op=ALU.mult),
                         reads=[c, ig[mo]], writes=[c])
                    k.op("dve", lambda e, b=b, c=c: e.tensor_tensor(out=b[:], in0=b[:], in1=c[:], op=ALU.mult),
                         reads=[b, c], writes=[b])
                    k.op("dve", lambda e, a=a, b=b, mo=mo: e.tensor_tensor_scan(
                        out=rec[mo][:], data0=a[:], data1=b[:], initial=hst[mo][:, 0:1], op0=ALU.mult, op1=ALU.add),
                         reads=[a, b, hst[mo]], writes=[rec[mo]])
                    k.op("dve", lambda e, mo=mo: e.tensor_copy(out=hst[mo][:], in_=rec[mo][:, 511:512]),
                         reads=[rec[mo]], writes=[hst[mo]])
                    k.op("act", lambda e, mo=mo, rb=rb: e.activation(out=rb[:, mo, :], in_=rec[mo][:], func=AF.Copy),
                         reads=[rec[mo]], writes=[rb])
                jb = tt // 2
                k.dma("sp", self.agF.src.t[jb * 256:(jb + 1) * 256, (tt % 2) * 512:(tt % 2 + 1) * 512].rearrange(
                    "(m p) t -> p m t", p=128), rb[:], reads=[rb], writes=[self.agF.src], sem=rb)
                if tt % 4 == 3:
                    self.agF.run_chunk(tt // 4)
        return self.end_phase("rg2_%d" % l)

    def mix_and_tail(self, l, st, mT, wsrc, hsrc, name):
        k = self.k
        inp = self.inp
        z = k.sb(st, "z", [128, KC, TC], F32)
        k.dma("sp", z[:], _fm(hsrc), writes=[z], sem=z)
        gcol = k.sb(st, "lng", [128, KC], F32); bcol = k.sb(st, "lnb", [128, KC], F32)
        k.dma("sp", gcol[:], inp["ln_mix_g"][l], writes=[gcol], sem=gcol)
        k.dma("sp", bcol[:], inp["ln_mix_b"][l], writes=[bcol], sem=bcol)

        def epi(m, mr, t0, nt, ps):
            k.op("dve", lambda e: e.scalar_tensor_tensor(out=z[:, m, t0:t0 + nt], in0=z[:, m, t0:t0 + nt],
                                                         scalar=float(ALPHA), in1=ps[:, :nt], op0=ALU.mult, op1=ALU.add),
                 reads=[z, ps], writes=[z])

        with k.scope() as st2:
            self.linear_fm(st2, mT, KC, TC, wsrc, D, epi, tag="wo")
        with k.scope() as stz:
            zt = k.sb(stz, "zt", [128, D], F32)
            k.op("pool", lambda e: e.memset(zt[:], 0.0), writes=[zt])
            for i in range((SEQ + CAP) // 128):
                k.dma("sp", self.ysp[0].t[i * 128:(i + 1) * 128, :], zt[:], reads=[zt], writes=[self.ysp[0]], sem=zt)
        if self.debug:
            k.dma("sp", _fm(self.dbg_d.t[:, :]), z[:], reads=[z], writes=[self.dbg_d], sem=z)
            k.dma("sp", _fm(self.dbg2_d.t[:, :]), mT[:], reads=[mT], writes=[self.dbg2_d], sem=mT)
        with k.scope() as st2:
            self.norm_fm(st2, z, KC, TC, gcol, bcol, LN_EPS, True, "ln1")
        k.dma("sp", _fm(self.h1T_d.t[:, :]), z[:], reads=[z], writes=[self.h1T_d], sem=z)
        with k.scope() as st2:
            rw = k.sb(st2, "rw", [128, KC, NEXP], F32)
            rbias = k.sb(st2, "rbias", [128, NEXP], F32)
            k.dma("sp", rw[:], _fm(inp["router_w"][l]), writes=[rw], sem=rw)
            k.dma("sp", rbias[:], inp["router_b"][l], writes=[rbias], sem=rbias)
            lg = k.sb(st2, "lg", [128, NEXP], F32)
            Gall = k.sb(st2, "Gall", [128, 8, NEXP], F32)
            Mall = k.sb(st2, "Mall", [128, 8, NEXP], F32)
            Pall = k.sb(st2, "Pall", [128, 8, NEXP], F32)
            top = k.sb(st2, "top", [128, 8], F32); sc = k.sb(st2, "sc", [128, 2], F32)
            ltri = k.sb(st2, "ltri", [128, 128], F32)
            iota = k.sb(st2, "iota", [128, CAP], F32)
            vals3 = k.sb(st2, "vals4", [128, 8, NEXP, 4], F32)
            dumprow = k.sb(st2, "dumprow", [128, 2], F32)
            for b, nm in ((ltri, "ltri"), (iota, "iota"), (vals3, "vals4"), (dumprow, "dumprow")):
                k.dma("sp", b[:], inp[nm].ap(), writes=[b], sem=b)
            htm = [k.sb(st2, "htm%d" % i, [128, D], BF16) for i in range(2)]
            for tk in range(TC // 128):
                tsl = slice(tk * 128, (tk + 1) * 128)
                hm = htm[tk % 2]
                for q4 in range(4):
                    pst = self.nps()
                    for j in range(4):
                        kk = q4 * 4 + j
                        k.op("pe", lambda e, pst=pst, kk=kk, j=j: e.transpose(
                            out=pst[:, j * 128:(j + 1) * 128], in_=z[:, kk, tsl], identity=self.ident[:]),
                             reads=[z, self.ident], writes=[pst])
                    k.op("act" if q4 % 2 else "dve", (lambda e, pst=pst, q4=q4: e.activation(
                        out=hm[:, q4 * 512:(q4 + 1) * 512], in_=pst[:, :], func=AF.Copy)) if q4 % 2 else
                         (lambda e, pst=pst, q4=q4: e.tensor_copy(out=hm[:, q4 * 512:(q4 + 1) * 512], in_=pst[:, :])),
                         reads=[pst], writes=[hm])
                k.dma("sp", self.agHt.src.t[tsl, :], hm[:], reads=[hm], writes=[self.agHt.src], sem=hm)
                if tk % 2 == 1:
                    self.agHt.run_chunk(tk // 2)
                ps = self.nps()
                for kk in range(KC):
                    k.op("pe", lambda e, kk=kk, ps=ps: e.matmul(
                        ps[:, :NEXP], lhsT=z[:, kk, tsl], rhs=rw[:, kk, :],
                        start=(kk == 0), stop=(kk == KC - 1)), reads=[z, rw], writes=[ps])
                ex = Gall[:, tk, :]
                mk = Mall[:, tk, :]
                k.op("dve", lambda e, ps=ps: e.tensor_tensor(out=lg[:], in0=ps[:, :NEXP], in1=rbias[:], op=ALU.add),
                     reads=[ps, rbias], writes=[lg])
                k.op("dve", lambda e: e.max(out=top[:], in_=lg[:]), reads=[lg], writes=[top])
                k.op("dve", lambda e: e.tensor_scalar(out=sc[:, 0:1], in0=top[:, 0:1], scalar1=-1.0, scalar2=None,
                                                      op0=ALU.mult), reads=[top], writes=[sc])
                k.op("act", lambda e: e.activation(out=ex, in_=lg[:], func=AF.Exp, bias=sc[:, 0:1]),
                     reads=[lg, sc], writes=[Gall])
                k.op("dve", lambda e: e.tensor_scalar(out=mk, in0=lg[:], scalar1=top[:, 3:4], scalar2=None,
                                                      op0=ALU.is_ge), reads=[lg, top], writes=[Mall])
                k.op("dve", lambda e: e.tensor_tensor(out=ex, in0=ex, in1=mk, op=ALU.mult),
                     reads=[Gall, Mall], writes=[Gall])
                k.op("dve", lambda e: e.reduce_sum(out=sc[:, 1:2], in_=ex, axis=AX.X), reads=[Gall], writes=[sc])
                k.op("dve", lambda e: e.reciprocal(out=sc[:, 1:2], in_=sc[:, 1:2]), reads=[sc], writes=[sc])
                k.op("dve", lambda e: e.tensor_scalar(out=ex, in0=ex, scalar1=sc[:, 1:2], scalar2=None,
                                                      op0=ALU.mult), reads=[Gall, sc], writes=[Gall])
            assert self.agHt.ch == 256
            for tk in range(8):
                ps = self.nps()
                for t2 in range(tk + 1):
                    k.op("pe", lambda e, ps=ps, t2=t2: e.matmul(
                        ps[:, :NEXP], lhsT=(ltri[:] if t2 == tk else self.ones32[:]), rhs=Mall[:, t2, :],
                        start=(t2 == 0), stop=(t2 == tk)), reads=[ltri, self.ones32, Mall], writes=[ps])
                k.op("act", lambda e, ps=ps: e.activation(out=Pall[:, tk, :], in_=ps[:, :NEXP], func=AF.Copy),
                     reads=[ps], writes=[Pall])
            k.op("dve", lambda e: e.tensor_copy(out=vals3[:, :, :, 3], in_=Gall[:, :, :]), reads=[Gall], writes=[vals3])
            R = [self.nps(), self.nps()]
            oh = [k.sb(st2, "oh%d" % i, [128, CAP], F32) for i in range(4)]
            n = 0
            for ee in range(NEXP):
                for tk in range(8):
                    o = oh[n % 4]
                    n += 1
                    k.op("dve", lambda e, o=o: e.tensor_scalar(
                        out=o[:], in0=iota[:], scalar1=Pall[:, tk, ee:ee + 1], scalar2=Mall[:, tk, ee:ee + 1],
                        op0=ALU.is_equal, op1=ALU.mult), reads=[iota, Pall, Mall], writes=[o])
                    for sb in range(2):
                        k.op("pe", lambda e, o=o, sb=sb: e.matmul(
                            R[sb][:, ee * 4:(ee + 1) * 4], lhsT=o[:, sb * 128:(sb + 1) * 128], rhs=vals3[:, tk, ee, :],
                            start=(tk == 0), stop=(tk == 7)), reads=[o, vals3], writes=[R[sb]])
            L = k.sb(st2, "L", [128, 2, NEXP, 4], F32)
            tmpf = k.sb(st2, "tmpf", [128, NEXP], F32)
            for sb in range(2):
                Rv = R[sb][:, 0:NEXP * 4].rearrange("p (e c) -> p e c", c=4)
                k.op("act", lambda e, sb=sb, Rv=Rv: e.activation(out=L[:, sb, :, 0], in_=Rv[:, :, 0], func=AF.Copy),
                     reads=[R[sb]], writes=[L])
                k.op("act", lambda e, sb=sb, Rv=Rv: e.activation(out=L[:, sb, :, 2], in_=Rv[:, :, 2], func=AF.Copy),
                     reads=[R[sb]], writes=[L])
                k.op("act", lambda e, sb=sb, Rv=Rv: e.activation(out=L[:, sb, :, 3], in_=Rv[:, :, 3], func=AF.Copy),
                     reads=[R[sb]], writes=[L])
                k.op("dve", lambda e, sb=sb, Rv=Rv: e.tensor_scalar(
                    out=tmpf[:], in0=Rv[:, :, 2], scalar1=-1.0, scalar2=1.0, op0=ALU.mult, op1=ALU.add),
                     reads=[R[sb]], writes=[tmpf])
                k.op("dve", lambda e, sb=sb: e.tensor_scalar(
                    out=tmpf[:], in0=tmpf[:], scalar1=dumprow[:, sb:sb + 1], scalar2=None, op0=ALU.mult),
                     reads=[tmpf, dumprow], writes=[tmpf])
                k.op("dve", lambda e, sb=sb, Rv=Rv: e.tensor_tensor(out=L[:, sb, :, 1], in0=Rv[:, :, 1], in1=tmpf[:],
                                                                    op=ALU.add), reads=[R[sb], tmpf], writes=[L])
            k.dma("sp", self.agL.src.t[:, :].rearrange("(sb p e) f -> p sb e f", sb=2, p=128), L[:], reads=[L],
                  writes=[self.agL.src], sem=L)
            self.agL.run()

    def phase_rg3(self, l, hsrc):
        k = self.k
        with k.scope() as st:
            mT = k.sb(st, "mT", [128, KC, TC], BF16)
            k.dma("sp", mT[:], _fm(self.gb_d.t[:, :]), writes=[mT], sem=mT)
            with k.scope() as st2:
                recT = k.sb(st2, "recT", [128, KC, TC], BF16)
                rows = self.agF.out.t[:, :]
                for kk in range(KC):
                    k.gather(recT[:, kk, :], rows, self.tF[:, kk:kk + 1], reads=[self.tF, self.agF.out],
                             writes=[recT], sem=recT)
                for kk in range(KC):
                    k.op("dve" if kk % 2 else "pool", lambda e, kk=kk: e.tensor_tensor(
                        out=mT[:, kk, :], in0=mT[:, kk, :], in1=recT[:, kk, :], op=ALU.mult),
                         reads=[mT, recT], writes=[mT])
            self.mix_and_tail(l, st, mT, self.inp["w_out"][l], hsrc, "rg3")
        return self.end_phase("rg3_%d" % l)

    def phase_moe_dense(self, l):
        k = self.k
        inp = self.inp
        grows = self.agG.out.t[:, :].rearrange("r (h t) -> (r h) t", t=512)
        ydst = self.ym[0]
        with k.scope() as st:
            W1 = k.sb(st, "W1", [128, KC, 2 * DEXP], BF16)
            W2 = k.sb(st, "W2", [128, DEXP // 128, D], BF16)
            b1c = k.sb(st, "b1c", [128, 16], F32); b2c = k.sb(st, "b2c", [128, 16], F32)
            gidx = k.sb(st, "gidx", [128, EPC * 16], I32)
            k.dma("sp", gidx[:], inp["idx_g"][:, :], writes=[gidx], sem=gidx)
            xts = [k.sb(st, "mx%d" % i, [128, KC, 512], BF16) for i in range(2)]
            grow = [k.sb(st, "grow%d" % i, [128, 512], F32) for i in range(2)]
            lin1 = k.sb(st, "lin1", [128, 8, 512], BF16)
            gt = [k.sb(st, "gt%d" % i, [128, 512], F32) for i in range(2)]
            sg = [k.sb(st, "sg%d" % i, [128, 512], F32) for i in range(2)]
            actT = k.sb(st, "actT", [128, 8, 512], BF16)
            yst = [k.sb(st, "yst%d" % i, [128, 4, 512], F32) for i in range(2)]
            yreg = [[Buf(None, "yreg") for _ in range(4)] for _ in range(16)]
            ysi = 0
            for el in range(EPC):
                k.dma("pool", W1[:], _fm(inp["w1"][l, el]), writes=[W1], sem=W1)
                k.dma("pool", W2[:], _fm(inp["w2"][l, el]), writes=[W2], sem=W2)
                k.dma("sp", b1c[:], inp["b1"][l, el], writes=[b1c], sem=b1c)
                k.dma("sp", b2c[:], inp["b2"][l, el], writes=[b2c], sem=b2c)

                def load(tt):
                    self.load_fm_tile(xts[tt % 2], self.agH, tt // 2, tt % 2, D)
                    g = grow[tt % 2]
                    k.gather(g[:], grows, gidx[:, el * 16 + tt:el * 16 + tt + 1], reads=[gidx, self.agG.out],
                             writes=[g], sem=g)

                load(0)
                for tt in range(16):
                    if tt + 1 < 16:
                        load(tt + 1)
                    x = xts[tt % 2]
                    g = grow[tt % 2]
                    for m in list(range(8, 16)) + list(range(8)):
                        ps = self.nps()
                        for kk in range(KC):
                            k.op("pe", lambda e, kk=kk, ps=ps, m=m, x=x: e.matmul(
                                ps[:, :], lhsT=W1[:, kk, m * 128:(m + 1) * 128], rhs=x[:, kk, :],
                                start=(kk == 0), stop=(kk == KC - 1)), reads=[W1, x], writes=[ps])
                        if m >= 8:
                            t = gt[m % 2]
                            k.op("dve", lambda e, ps=ps, m=m, t=t: e.tensor_scalar(
                                out=t[:], in0=ps[:, :], scalar1=b1c[:, m:m + 1], scalar2=7.0, op0=ALU.add, op1=ALU.min),
                                 reads=[ps, b1c], writes=[t])
                            k.op("dve", lambda e, m=m, t=t: e.tensor_scalar(
                                out=lin1[:, m - 8, :], in0=t[:], scalar1=-7.0, scalar2=1.0, op0=ALU.max, op1=ALU.add),
                                 reads=[t], writes=[lin1])
                        else:
                            t = gt[m % 2]; s = sg[m % 2]
                            k.op("dve", lambda e, ps=ps, m=m, t=t: e.tensor_scalar(
                                out=t[:], in0=ps[:, :], scalar1=b1c[:, m:m + 1], scalar2=7.0, op0=ALU.add, op1=ALU.min),
                                 reads=[ps, b1c], writes=[t])
                            k.op("act", lambda e, t=t, s=s: e.activation(out=s[:], in_=t[:], func=AF.Sigmoid, scale=1.702),
                                 reads=[t], writes=[s])
                            k.op("pool", lambda e, t=t, s=s: e.tensor_tensor(out=s[:], in0=s[:], in1=t[:], op=ALU.mult),
                                 reads=[s, t], writes=[s])
                            k.op("dve", lambda e, m=m, s=s: e.tensor_tensor(out=actT[:, m, :], in0=s[:], in1=lin1[:, m, :],
                                                                            op=ALU.mult), reads=[s, lin1], writes=[actT])
                    for f in range(16):
                        ps = self.nps()
                        for kk in range(8):
                            k.op("pe", lambda e, kk=kk, ps=ps, f=f: e.matmul(
                                ps[:, :], lhsT=W2[:, kk, f * 128:(f + 1) * 128], rhs=actT[:, kk, :],
                                start=(kk == 0), stop=(kk == 7)), reads=[W2, actT], writes=[ps])
                        ys = yst[ysi % 2]
                        k.op("dve", lambda e, ps=ps, f=f, ys=ys, g=g: e.scalar_tensor_tensor(
                            out=ys[:, f % 4, :], in0=ps[:, :], scalar=b2c[:, f:f + 1], in1=g[:],
                            op0=ALU.add, op1=ALU.mult), reads=[ps, b2c, g], writes=[ys])
                        if f % 4 == 3:
                            f0 = f - 3
                            reg = yreg[tt][f // 4]
                            dst = ydst.t[f0 * 128:(f0 + 4) * 128, tt * 512:(tt + 1) * 512].rearrange(
                                "(j p) t -> p j t", p=128)
                            if el == 0:
                                k.dma("sp", dst, ys[:], reads=[ys], writes=[reg], sem=ys)
                            else:
                                k.dma("pool", dst, ys[:], reads=[ys], writes=[reg], sem=ys, accum_op=ALU.add)
                            ysi += 1
            for row in yreg:
                for reg in row:
                    k._merge(ydst.w, reg.w)
            for i in range(D // 128):
                sl = slice(i * 128, (i + 1) * 128)
                k.collective("AllReduce", G4, self.ym[0], self.ym[1], self.ym[0].t[sl, :], self.ym[1].t[sl, :])
            for i in range(D // 128):
                sl = slice(i * 128, (i + 1) * 128)
                k.collective("AllReduce", G2, self.ym[1], self.ym[2], self.ym[1].t[sl, :], self.ym[2].t[sl, :])
        return self.end_phase("moe_%d" % l)

    def phase_moe(self, l):
        k = self.k
        inp = self.inp
        hrows = self.agHt.out.t[:, :]
        lrows = self.agL.out.t[:, :]
        ysp = self.ysp[0]
        with k.scope() as st:
            zdeps = {}
            W1 = k.sb(st, "W1", [128, KC, 2 * DEXP], BF16)
            W2 = k.sb(st, "W2", [128, DEXP // 128, D], BF16)
            b1c = k.sb(st, "b1c", [128, 16], F32)
            b2t = k.sb(st, "b2t", [128, D], F32)
            lidx = k.sb(st, "lidx", [128, EPC * NCORE * 2], I32)
            k.dma("sp", lidx[:], inp["idx_L"][:, :], writes=[lidx], sem=lidx)
            xgT = [k.sb(st, "xgT%d" % i, [128, KC, 512], BF16) for i in range(2)]
            xg = [k.sb(st, "xg%d" % i, [128, D], BF16) for i in range(4)]
            Lt = [k.sb(st, "Lt%d" % i, [128, 4, 4], F32) for i in range(2)]
            Li = [k.sb(st, "Li%d" % i, [128, 4, 2], I32) for i in range(2)]
            lin1 = k.sb(st, "lin1", [128, 8, 512], BF16)
            gt = [k.sb(st, "gt%d" % i, [128, 512], F32) for i in range(2)]
            sg = [k.sb(st, "sg%d" % i, [128, 512], F32) for i in range(2)]
            actT = k.sb(st, "actT", [128, 8, 512], BF16)
            ytm = [k.sb(st, "ytm%d" % i, [128, D], F32) for i in range(2)]
            prev = dict(zdeps)
            yi = 0
            npair = NCORE // 2
            pairs = [(0, 1), (4, 5), (2, 3), (6, 7)]
            seq = [(el, pr) for el in range(EPC) for pr in range(npair)]

            def rs_stage1(parity):
                k._merge(self.ysp[0].w, cur)
                k._merge(self.ysp[0].w, prev)
                for i in range(parity, SEQ // 512, 2):
                    k.collective("ReduceScatter", G2, self.ysp[0], self.ysp[1], self.ysp[0].t[i * 512:(i + 1) * 512, :],
                                 self.ysp[1].t[i * 256:(i + 1) * 256, :])

            def prep_gather(idx):
                el, pr = seq[idx]
                lt, li = Lt[idx % 2], Li[idx % 2]
                for b4 in range(4):
                    r, sb = pairs[pr][b4 // 2], b4 % 2
                    col = (el * NCORE + r) * 2 + sb
                    k.gather(lt[:, b4, :], lrows, lidx[:, col:col + 1], reads=[lidx, self.agL.out], writes=[lt], sem=lt)
                k.op("dve", lambda e: e.tensor_copy(out=li[:], in_=lt[:, :, 0:2]), reads=[lt], writes=[li])
                for b4 in range(4):
                    g = xg[b4]
                    k.gather(g[:], hrows, li[:, b4, 0:1], reads=[li, self.agHt.out], writes=[g], sem=g)

            def prep_transpose(idx):
                x = xgT[idx % 2]
                for b4 in range(4):
                    g = xg[b4]
                    for q4 in range(4):
                        pst = self.nps()
                        pv = pst[:, :].bitcast(BF16)
                        for j in range(4):
                            kk = q4 * 4 + j
                            k.op("pe", lambda e, pv=pv, kk=kk, j=j, g=g: e.transpose(
                                out=pv[:, j * 128:(j + 1) * 128], in_=g[:, kk * 128:(kk + 1) * 128], identity=self.identb[:]),
                                 reads=[g, self.identb], writes=[pst])
                        src = pv[:, 0:512].rearrange("p (j t) -> p j t", j=4)
                        dst = x[:, q4 * 4:(q4 + 1) * 4, b4 * 128:(b4 + 1) * 128]
                        if q4 % 2:
                            k.op("act", lambda e, src=src, dst=dst: e.activation(out=dst, in_=src, func=AF.Copy),
                                 reads=[pst], writes=[x])
                        else:
                            k.op("dve", lambda e, src=src, dst=dst: e.tensor_copy(out=dst, in_=src), reads=[pst], writes=[x])

            prep_gather(0)
            prep_transpose(0)
            cur = {}
            for idx, (el, pr) in enumerate(seq):
                if idx == 0:
                    k.dma("pool", W1[:], _fm(inp["w1"][l, el]), writes=[W1], sem=W1)
                    k.dma("pool", W2[:], _fm(inp["w2"][l, el]), writes=[W2], sem=W2)
                    k.dma("sp", b1c[:], inp["b1"][l, el], writes=[b1c], sem=b1c)
                    k.dma("sp", b2t[:], inp["b2row"][l, el:el + 1, :].partition_broadcast(128), writes=[b2t], sem=b2t)
                if pr == 0 and el > 0:
                    prev = cur
                    cur = {}
                last_pair = (pr == npair - 1 and el + 1 < EPC)
                if idx + 1 < len(seq):
                    prep_gather(idx + 1)
                x = xgT[idx % 2]
                lt, li = Lt[idx % 2], Li[idx % 2]
                for m in list(range(8, 16)) + list(range(8)):
                    ps = self.nps()
                    for kk in range(KC):
                        k.op("pe", lambda e, kk=kk, ps=ps, m=m: e.matmul(
                            ps[:, :], lhsT=W1[:, kk, m * 128:(m + 1) * 128], rhs=x[:, kk, :],
                            start=(kk == 0), stop=(kk == KC - 1)), reads=[W1, x], writes=[ps])
                    t = gt[m % 2]
                    k.op("dve", lambda e, ps=ps, m=m, t=t: e.tensor_scalar(
                        out=t[:], in0=ps[:, :], scalar1=b1c[:, m:m + 1], scalar2=7.0, op0=ALU.add, op1=ALU.min),
                         reads=[ps, b1c], writes=[t])
                    if m >= 8:
                        k.op("dve", lambda e, m=m, t=t: e.tensor_scalar(
                            out=lin1[:, m - 8, :], in0=t[:], scalar1=-7.0, scalar2=1.0, op0=ALU.max, op1=ALU.add),
                             reads=[t], writes=[lin1])
                    else:
                        sgm = sg[m % 2]
                        k.op("act", lambda e, t=t, sgm=sgm: e.activation(out=sgm[:], in_=t[:], func=AF.Sigmoid, scale=1.702),
                             reads=[t], writes=[sgm])
                        k.op("pool", lambda e, t=t, sgm=sgm: e.tensor_tensor(out=sgm[:], in0=sgm[:], in1=t[:], op=ALU.mult),
                             reads=[sgm, t], writes=[sgm])
                        k.op("dve", lambda e, m=m, sgm=sgm: e.tensor_tensor(out=actT[:, m, :], in0=sgm[:], in1=lin1[:, m, :],
                                                                            op=ALU.mult), reads=[sgm, lin1], writes=[actT])
                if last_pair:
                    k.dma("pool", W1[:], _fm(inp["w1"][l, el + 1]), writes=[W1], sem=W1)
                    k.dma("sp", b1c[:], inp["b1"][l, el + 1], writes=[b1c], sem=b1c)
                if idx + 1 < len(seq):
                    prep_transpose(idx + 1)
                for b4 in range(4):
                    ys = ytm[yi % 2]
                    yi += 1
                    for ft in range(4):
                        fs = slice(ft * 512, (ft + 1) * 512)
                        ps = self.nps()
                        for kk in range(8):
                            k.op("pe", lambda e, kk=kk, ps=ps, b4=b4, fs=fs: e.matmul(
                                ps[:, :], lhsT=actT[:, kk, b4 * 128:(b4 + 1) * 128], rhs=W2[:, kk, fs],
                                start=(kk == 0), stop=(kk == 7)), reads=[W2, actT], writes=[ps])
                        k.op("dve", lambda e, ps=ps, ys=ys, fs=fs: e.tensor_tensor(out=ys[:, fs], in0=ps[:, :], in1=b2t[:, fs],
                                                                                  op=ALU.add), reads=[ps, b2t], writes=[ys])
                        k.op("act", lambda e, ys=ys, fs=fs, b4=b4, lt=lt: e.activation(
                            out=ys[:, fs], in_=ys[:, fs], func=AF.Identity, scale=lt[:, b4, 3:4]), reads=[ys, lt], writes=[ys])
                    ev = k.scatter_add(ysp.t[:, :], li[:, b4, 1:2], ys[:], reads=[ys, li], sem=ys, extra_deps=prev)
                    k._merge(cur, dict([ev]))
                if last_pair:
                    k.dma("pool", W2[:], _fm(inp["w2"][l, el + 1]), writes=[W2], sem=W2)
                    k.dma("sp", b2t[:], inp["b2row"][l, el + 1:el + 2, :].partition_broadcast(128), writes=[b2t], sem=b2t)
                if el == EPC - 1 and pr == npair - 2:
                    rs_stage1(0)
            rs_stage1(1)
            for j in range(SEQ // 1024):
                k.collective("ReduceScatter", G4, self.ysp[1], self.ysp[2], self.ysp[1].t[j * 512:(j + 1) * 512, :],
                             self.ysp[2].t[j * 128:(j + 1) * 128, :])
        return self.end_phase("moe_%d" % l)

    def phase_post(self, l, hdst):
        k = self.k
        inp = self.inp
        with k.scope() as st:
            z = k.sb(st, "pz", [128, KC, TC], F32)
            k.dma("sp", z[:], _fm(self.h1T_d.t[:, :]), writes=[z], sem=z)
            gcol = k.sb(st, "lng2", [128, KC], F32); bcol = k.sb(st, "lnb2", [128, KC], F32)
            k.dma("sp", gcol[:], inp["ln_ffn_g"][l], writes=[gcol], sem=gcol)
            k.dma("sp", bcol[:], inp["ln_ffn_b"][l], writes=[bcol], sem=bcol)
            yidx = k.sb(st, "yidx", [128, 8], I32)
            k.dma("sp", yidx[:], inp["idx_y"][:, :], writes=[yidx], sem=yidx)
            with k.scope() as st2:
                ytk = [k.sb(st2, "ytk%d" % i, [128, D], F32) for i in range(2)]
                for tk in range(8):
                    y = ytk[tk % 2]
                    k.dma("sp", y[:], self.ysp[2].t[tk * 128:(tk + 1) * 128, :], reads=[self.ysp[2]], writes=[y], sem=y)
                    for q4 in range(4):
                        pst = self.nps()
                        for j in range(4):
                            kk = q4 * 4 + j
                            k.op("pe", lambda e, pst=pst, kk=kk, j=j, y=y: e.transpose(
                                out=pst[:, j * 128:(j + 1) * 128], in_=y[:, kk * 128:(kk + 1) * 128], identity=self.ident[:]),
                                 reads=[y, self.ident], writes=[pst])
                        zv = z[:, q4 * 4:(q4 + 1) * 4, tk * 128:(tk + 1) * 128]
                        k.op("dve", lambda e, pst=pst, zv=zv: e.scalar_tensor_tensor(
                            out=zv, in0=zv, scalar=float(ALPHA), in1=pst[:, :].rearrange("p (j t) -> p j t", j=4),
                            op0=ALU.mult, op1=ALU.add), reads=[z, pst], writes=[z])
            with k.scope() as st2:
                self.norm_fm(st2, z, KC, TC, gcol, bcol, LN_EPS, True, "ln2")
            zb = k.sb(st, "pzb", [128, KC, TC], BF16)
            for kk in range(KC):
                if kk % 2:
                    k.op("pool", lambda e, kk=kk: e.tensor_copy(out=zb[:, kk, :], in_=z[:, kk, :]), reads=[z], writes=[zb])
                else:
                    k.op("act", lambda e, kk=kk: e.activation(out=zb[:, kk, :], in_=z[:, kk, :], func=AF.Copy),
                         reads=[z], writes=[zb])
            pTb = k.sb(st, "pTb", [128, 2, TC], BF16)
            wp = k.sb(st, "wp", [128, 2, D], BF16)
            k.dma("pool", pTb[:], _fm(inp["pT"][l]), writes=[pTb], sem=pTb)
            k.dma("pool", wp[:], _fm(inp["ple_proj"][l]), writes=[wp], sem=wp)
            sgt = [k.sb(st, "psg%d" % i, [128, 512], F32) for i in range(2)]

            def epi(m, mr, t0, nt, ps):
                s = sgt[m % 2]
                k.op("act", lambda e: e.activation(out=s[:, :nt], in_=ps[:, :nt], func=AF.Sigmoid), reads=[ps], writes=[s])
                ps2 = self.nps()
                for kk in range(2):
                    k.op("pe", lambda e, kk=kk: e.matmul(ps2[:, :nt], lhsT=wp[:, kk, m * 128:(m + 1) * 128],
                                                         rhs=pTb[:, kk, t0:t0 + nt], start=(kk == 0), stop=(kk == 1)),
                         reads=[wp, pTb], writes=[ps2])
                k.op("dve", lambda e: e.tensor_tensor(out=s[:, :nt], in0=ps2[:, :nt], in1=s[:, :nt], op=ALU.mult),
                     reads=[ps2, s], writes=[s])
                k.op("pool", lambda e: e.tensor_tensor(out=z[:, m, t0:t0 + nt], in0=z[:, m, t0:t0 + nt], in1=s[:, :nt],
                                                       op=ALU.add), reads=[z, s], writes=[z])

            self.linear_fm(st, zb, KC, TC, inp["ple_gate"][l], D, epi, tag="wpg")
            k.dma("sp", _fm(hdst), z[:], reads=[z], writes=[self.hT_d], sem=z)
        return self.end_phase("post_%d" % l)

    def phase_q(self, j, hsrc, with_kv):
        k = self.k
        inp = self.inp
        with k.scope() as st:
            hTb = k.sb(st, "qhTb", [128, KC, TC], BF16)
            k.dma("pool", hTb[:], _fm(hsrc), writes=[hTb], sem=hTb)
            cq = k.sb(st, "cq", [128, 6, TC], F32)
            qn = k.sb(st, "qn", [128, 6], F32)
            k.dma("sp", qn[:], inp["q_norm"][j], writes=[qn], sem=qn)

            def epi(m, mr, t0, nt, ps):
                k.op("act", lambda e: e.activation(out=cq[:, m, t0:t0 + nt], in_=ps[:, :nt], func=AF.Copy),
                     reads=[ps], writes=[cq])

            with k.scope() as st2:
                self.linear_fm(st2, hTb, KC, TC, inp["w_dq"][j], QLORA, epi, mblk=QLORA, tag="wdq")
            with k.scope() as st2:
                self.norm_fm(st2, cq, 6, TC, qn, None, RMS_EPS, False, "rq")
            cqb = k.sb(st, "cqb", [128, 6, TC], BF16)
            k.op("pool", lambda e: e.tensor_copy(out=cqb[:], in_=cq[:]), reads=[cq], writes=[cqb])
            k.dma("sp", _fm(self.agQ.src.t[:, :]), cqb[:], reads=[cqb], writes=[self.agQ.src], sem=cqb)
            self.agQ.run()
            if with_kv:
                ck = k.sb(st, "ck", [128, 4, TC], F32)
                kn = k.sb(st, "kn", [128, 4], F32)
                k.dma("sp", kn[:], inp["kv_norm"][:, :], writes=[kn], sem=kn)

                def epi2(m, mr, t0, nt, ps):
                    k.op("act", lambda e: e.activation(out=ck[:, m, t0:t0 + nt], in_=ps[:, :nt], func=AF.Copy),
                         reads=[ps], writes=[ck])

                with k.scope() as st2:
                    self.linear_fm(st2, hTb, KC, TC, inp["w_dkv_c"], KVLORA, epi2, tag="wdkv")
                with k.scope() as st2:
                    self.norm_fm(st2, ck, 4, TC, kn, None, RMS_EPS, False, "rk")
                ckb = k.sb(st, "ckb", [128, 4, TC], BF16)
                k.op("pool", lambda e: e.tensor_copy(out=ckb[:], in_=ck[:]), reads=[ck], writes=[ckb])
                k.dma("sp", _fm(self.agKc.src.t[:, :]), ckb[:], reads=[ckb], writes=[self.agKc.src], sem=ckb)
                self.agKc.run()
                tabs = k.sb(st, "ktabs", [64, 2, TC], F32)
                k.dma("sp", tabs[:], self.agT.src.t[:, :].rearrange("(j p) t -> p j t", p=64), writes=[tabs], sem=tabs)
                kpa = k.sb(st, "kpa", [64, TC], F32); kpb = k.sb(st, "kpb", [64, TC], F32)
                krb = k.sb(st, "krb", [64, TC], BF16)

                def epi3(m, mr, t0, nt, ps):
                    k.op("dve", lambda e: e.tensor_tensor(out=kpa[:, t0:t0 + nt], in0=ps[:64, :nt], in1=tabs[:, 0, t0:t0 + nt],
                                                          op=ALU.mult), reads=[ps, tabs], writes=[kpa])

                def epi4(m, mr, t0, nt, ps):
                    k.op("dve", lambda e: e.tensor_tensor(out=kpb[:, t0:t0 + nt], in0=ps[:64, :nt], in1=tabs[:, 1, t0:t0 + nt],
                                                          op=ALU.mult), reads=[ps, tabs], writes=[kpb])

                with k.scope() as st2:
                    self.linear_fm(st2, hTb, KC, TC, inp["w_dkv_pe"], ROPE, epi3, tag="wkpe")
                with k.scope() as st2:
                    self.linear_fm(st2, hTb, KC, TC, inp["w_dkv_pesw"], ROPE, epi4, tag="wkpes")
                k.op("dve", lambda e: e.tensor_tensor(out=krb[:], in0=kpa[:], in1=kpb[:], op=ALU.add),
                     reads=[kpa, kpb], writes=[krb])
                k.dma("sp", self.agKp.src.t[:, :], krb[:], reads=[krb], writes=[self.agKp.src], sem=krb)
                self.agKp.run()
        return self.end_phase("q_%d" % j)

    def phase_attn(self, j):
        k = self.k
        inp = self.inp
        with k.scope() as st:
            KT = k.sb(st, "KT", [128, 2, SEQ], BF16)
            KPE = k.sb(st, "KPE", [64, SEQ], BF16)
            V = k.sb(st, "V", [128, SEQ // 128, 256], BF16)
            wk = k.sb(st, "wk", [128, 4, 256], BF16); wv = k.sb(st, "wv", [128, 4, 256], BF16)
            wqn = k.sb(st, "wqn", [128, 6, 256], BF16)
            wqp = k.sb(st, "wqp", [128, 6, 128], BF16); wqs = k.sb(st, "wqs", [128, 6, 128], BF16)
            k.dma("pool", wk[:], _fm(inp["w_ukv_k"][:, :]), writes=[wk], sem=wk)
            k.dma("pool", wv[:], _fm(inp["w_ukv_v"][:, :]), writes=[wv], sem=wv)
            k.dma("pool", wqn[:], _fm(inp["w_uq_n"][j]), writes=[wqn], sem=wqn)
            k.dma("pool", wqp[:], _fm(inp["w_uq_pe"][j]), writes=[wqp], sem=wqp)
            k.dma("pool", wqs[:], _fm(inp["w_uq_pesw"][j]), writes=[wqs], sem=wqs)
            msk = k.sb(st, "msk", [128, 4, 512], BF16)
            k.dma("pool", msk[:], inp["masks"][:, :, :], writes=[msk], sem=msk)
            with k.scope() as st2:
                cts = [k.sb(st2, "ct%d" % i, [128, 4, 512], BF16) for i in range(2)]
                for tt in range(16):
                    r, half = tt // 2, tt % 2
                    ct = cts[tt % 2]
                    self.load_fm_tile(ct, self.agKc, r, half, KVLORA)
                    k.dma("sp", KPE[:, tt * 512:(tt + 1) * 512], self.agKp.rows(r, 0, ROPE)[:, half * 512:(half + 1) * 512],
                          reads=[self.agKp.out], writes=[KPE], sem=KPE)
                    for hh in range(2):
                        ps = self.nps()
                        for kk in range(4):
                            k.op("pe", lambda e, kk=kk, ps=ps, hh=hh, ct=ct: e.matmul(
                                ps[:, :], lhsT=wk[:, kk, hh * 128:(hh + 1) * 128], rhs=ct[:, kk, :],
                                start=(kk == 0), stop=(kk == 3)), reads=[wk, ct], writes=[ps])
                        k.op("act", lambda e, ps=ps, hh=hh, tt=tt: e.activation(
                            out=KT[:, hh, tt * 512:(tt + 1) * 512], in_=ps[:, :], func=AF.Copy), reads=[ps], writes=[KT])
                    for kb4 in range(4):
                        ps = self.nps()
                        for kk in range(4):
                            k.op("pe", lambda e, kk=kk, ps=ps, kb4=kb4, ct=ct: e.matmul(
                                ps[:, :256], lhsT=ct[:, kk, kb4 * 128:(kb4 + 1) * 128], rhs=wv[:, kk, :],
                                start=(kk == 0), stop=(kk == 3)), reads=[wv, ct], writes=[ps])
                        k.op("dve", lambda e, ps=ps, kb4=kb4, tt=tt: e.tensor_copy(out=V[:, tt * 4 + kb4, :], in_=ps[:, :256]),
                             reads=[ps], writes=[V])
            cqs = [k.sb(st, "cqt%d" % i, [128, 6, 512], BF16) for i in range(2)]
            tbs = [k.sb(st, "tbt%d" % i, [64, 2, 512], F32) for i in range(2)]
            qnb = [k.sb(st, "qnb%d" % i, [128, 512], BF16) for i in range(2)]
            qpb = [k.sb(st, "qpb%d" % i, [64, 512], BF16) for i in range(2)]
            r1 = [k.sb(st, "r1_%d" % i, [64, 512], F32) for i in range(2)]
            r2 = [k.sb(st, "r2_%d" % i, [64, 512], F32) for i in range(2)]
            PT = [k.sb(st, "PT%d" % i, [128, 512], BF16) for i in range(3)]
            rl = k.sb(st, "rl", [128, 512], F32)
            ob = [k.sb(st, "ob%d" % i, [128, 512], BF16) for i in range(2)]
            psS = [self.ps[0], self.ps[1]]
            psO = [self.ps[2], self.ps[3]]
            psL = [self.ps[4], self.ps[5]]
            psQ = [self.ps[6], self.ps[7]]
            pti = 0
            si = 0

            def loadq(qt):
                r, half = qt // 2, qt % 2
                c = cqs[qt % 2]
                self.load_fm_tile(c, self.agQ, r, half, QLORA)
                t = tbs[qt % 2]
                k.dma("sp", t[:], self.agT.rows(r, 0, 128)[:, half * 512:(half + 1) * 512].rearrange(
                    "(j p) t -> p j t", p=64), reads=[self.agT.out], writes=[t], sem=t)

            loadq(0)
            for qt in range(16):
                if qt + 1 < 16:
                    loadq(qt + 1)
                c = cqs[qt % 2]
                tb = tbs[qt % 2]
                for hh in range(2):
                    qn_, qp_ = qnb[hh], qpb[hh]
                    ps = psQ[0]
                    for kk in range(6):
                        k.op("pe", lambda e, kk=kk, ps=ps: e.matmul(ps[:, :], lhsT=wqn[:, kk, hh * 128:(hh + 1) * 128],
                                                                    rhs=c[:, kk, :], start=(kk == 0), stop=(kk == 5)),
                             reads=[wqn, c], writes=[ps])
                    k.op("act", lambda e, ps=ps: e.activation(out=qn_[:], in_=ps[:, :], func=AF.Copy), reads=[ps], writes=[qn_])
                    ps = psQ[1]
                    for kk in range(6):
                        k.op("pe", lambda e, kk=kk, ps=ps: e.matmul(ps[:64, :], lhsT=wqp[:, kk, hh * 64:(hh + 1) * 64],
                                                                    rhs=c[:, kk, :], start=(kk == 0), stop=(kk == 5)),
                             reads=[wqp, c], writes=[ps])
                    k.op("dve", lambda e, ps=ps: e.tensor_tensor(out=r1[hh][:], in0=ps[:64, :], in1=tb[:, 0, :], op=ALU.mult),
                         reads=[ps, tb], writes=[r1[hh]])
                    ps = psQ[0]
                    for kk in range(6):
                        k.op("pe", lambda e, kk=kk, ps=ps: e.matmul(ps[:64, :], lhsT=wqs[:, kk, hh * 64:(hh + 1) * 64],
                                                                    rhs=c[:, kk, :], start=(kk == 0), stop=(kk == 5)),
                             reads=[wqs, c], writes=[ps])
                    k.op("dve", lambda e, ps=ps: e.tensor_tensor(out=r2[hh][:], in0=ps[:64, :], in1=tb[:, 1, :], op=ALU.mult),
                         reads=[ps, tb], writes=[r2[hh]])
                    k.op("dve", lambda e: e.tensor_tensor(out=qp_[:], in0=r1[hh][:], in1=r2[hh][:], op=ALU.add),
                         reads=[r1[hh], r2[hh]], writes=[qp_])
                    po, pl = psO[hh], psL[hh]
                    nkb = 4 * qt + 4
                    def qk(kb):
                        pss = psS[kb % 2]
                        k.op("pe", lambda e: e.matmul(pss[:, :], lhsT=KT[:, hh, kb * 128:(kb + 1) * 128],
                                                      rhs=qn_[:], start=True, stop=False),
                             reads=[KT, qn_], writes=[pss])
                        k.op("pe", lambda e: e.matmul(pss[:, :], lhsT=KPE[:, kb * 128:(kb + 1) * 128],
                                                      rhs=qp_[:], start=False, stop=True),
                             reads=[KPE, qp_], writes=[pss])

                    qk(0)
                    for kb in range(nkb):
                        if kb + 1 < nkb:
                            qk(kb + 1)
                        pss = psS[kb % 2]
                        p = PT[pti % 3]; pti += 1
                        k.op("act", lambda e, pss=pss, p=p: e.activation(out=p[:], in_=pss[:, :], func=AF.Exp,
                                                                         scale=float(ATTN_SCALE)), reads=[pss], writes=[p])
                        if kb >= 4 * qt:
                            jm = kb - 4 * qt
                            k.op("dve", lambda e, p=p, jm=jm: e.tensor_tensor(out=p[:], in0=p[:], in1=msk[:, jm, :],
                                                                              op=ALU.mult), reads=[p, msk], writes=[p])
                        k.op("pe", lambda e, p=p, kb=kb: e.matmul(po[:, :], lhsT=V[:, kb, hh * 128:(hh + 1) * 128], rhs=p[:],
                                                                  start=(kb == 0), stop=(kb == nkb - 1)),
                             reads=[V, p], writes=[po])
                        k.op("pe", lambda e, p=p, kb=kb: e.matmul(pl[:, :], lhsT=self.onesb[:], rhs=p[:],
                                                                  start=(kb == 0), stop=(kb == nkb - 1)),
                             reads=[self.onesb, p], writes=[pl])
                    k.op("dve", lambda e: e.reciprocal(out=rl[:], in_=pl[:, :]), reads=[pl], writes=[rl])
                    o = ob[hh]
                    k.op("dve", lambda e, o=o: e.tensor_tensor(out=o[:], in0=po[:, :], in1=rl[:], op=ALU.mult),
                         reads=[po, rl], writes=[o])
                    jb = qt // 2
                    k.dma("sp", self.agF.src.t[jb * 256 + hh * 128:jb * 256 + (hh + 1) * 128, (qt % 2) * 512:(qt % 2 + 1) * 512],
                          o[:], reads=[o], writes=[self.agF.src], sem=o)
                if qt % 4 == 3:
                    self.agF.run_chunk(qt // 4)
        return self.end_phase("attn_%d" % j)

    def phase_attn_out(self, l, j, hsrc):
        k = self.k
        with k.scope() as st:
            oT = k.sb(st, "oT", [128, KC, TC], BF16)
            rows = self.agF.out.t[:, :]
            for kk in range(KC):
                k.gather(oT[:, kk, :], rows, self.tF[:, kk:kk + 1], reads=[self.tF, self.agF.out], writes=[oT], sem=oT)
            self.mix_and_tail(l, st, oT, self.inp["w_o"][j], hsrc, "ao")
        return self.end_phase("ao_%d" % l)

    def build(self):
        nc = self.nc
        self.declare()
        self.scratch()
        k = self.k
        with contextlib.ExitStack() as cst:
            self.consts(cst)
            self._run()
            if self.stopped or self.dumps:
                self._dump()
            k.barrier()
        return nc

    def _run(self):
        if self.phase_init():
            return
        for l in range(DEPTH):
            hsrc = self.inp["xT"][:, :] if l == 0 else self.hT_d.t[:, :]
            hdst = self.outT[:, :] if l == DEPTH - 1 else self.hT_d.t[:, :]
            if l < N_A:
                if self.phase_rg1(l, hsrc):
                    return
                if self.phase_rg2(l):
                    return
                if self.phase_rg3(l, hsrc):
                    return
            else:
                j = l - N_A
                if self.phase_q(j, hsrc, j == 0):
                    return
                if self.phase_attn(j):
                    return
                if self.phase_attn_out(l, j, hsrc):
                    return
            if self.phase_moe(l):
                return
            if self.phase_post(l, hdst):
                return

    def _dump(self):
        k = self.k
        allb = {}
        for nm in ("hT_d", "h1T_d", "gb_d", "dbg_d", "dbg2_d"):
            allb[nm] = getattr(self, nm)
        for grp in ("agH", "agF", "agG", "agKc", "agKp", "agQ", "agT"):
            a = getattr(self, grp)
            for b in (a.src, a.mid, a.out):
                allb[b.name] = b
        for b in self.ym + self.ysp:
            allb[b.name] = b
        for a in (self.agHt, self.agL):
            for b in (a.src, a.mid, a.out):
                allb[b.name] = b
        for nm in self.dumps:
            b = allb[nm]
            shape = list(b.t.shape)
            o = self.nc.dram_tensor("dump_" + nm, shape, b.t.dtype, kind="ExternalOutput")
            ob = Buf(o, "dump_" + nm)
            rows = shape[0]
            step = max(1, rows // 8)
            for r0 in range(0, rows, step):
                k.dma("sp", o[r0:r0 + step, :], b.t[r0:r0 + step, :], reads=[b], writes=[ob], sem=ob)


def _cols(v):
    v = np.asarray(v, np.float32)
    lead = v.shape[:-1]
    n = v.shape[-1] // 128
    return np.ascontiguousarray(np.swapaxes(v.reshape(*lead, n, 128), -1, -2))


def make_in_maps(inp, used=None):
    f = lambda a: np.ascontiguousarray(np.asarray(a, np.float32))
    swap = np.concatenate([np.arange(32, 64), np.arange(0, 32)])
    pp = np.arange(128)[:, None]
    cache = {}

    def get(name):
        if name not in cache:
            cache[name] = np.asarray(inp[name])
        return cache[name]

    def ropec():
        inv = (1.0 / (10000.0 ** (np.arange(0, ROPE, 2, dtype=np.float32) / ROPE))).astype(np.float32)
        r = np.zeros((64, 2), np.float32)
        r[:, 0] = np.concatenate([inv, inv])
        r[:32, 1] = -1.0
        r[32:, 1] = 1.0
        return r

    def masks():
        qq = np.arange(512)[None, :]
        return f(np.stack([(pp + 128 * jm <= qq).astype(np.float32) for jm in range(4)], axis=1))

    def w_uq():
        return get("mla_w_uq").reshape(2, QLORA, 16, 192)

    def w_ukv():
        return get("kv_w_ukv").reshape(KVLORA, 16, 256)

    common = {
        "ln_mix_g": lambda: _cols(get("ln_mix_g")), "ln_mix_b": lambda: _cols(get("ln_mix_b")),
        "ln_ffn_g": lambda: _cols(get("ln_ffn_g")), "ln_ffn_b": lambda: _cols(get("ln_ffn_b")),
        "w_in_g": lambda: f(get("rg_w_in")[:, :, :D]), "w_out": lambda: f(get("rg_w_out")),
        "router_w": lambda: f(get("moe_router_w")),
        "router_b": lambda: f(np.broadcast_to(get("moe_router_b")[:, None, :], (DEPTH, 128, NEXP))),
        "ple_proj": lambda: f(get("ple_w_proj")), "ple_gate": lambda: f(get("ple_w_gate")),
        "w_dq": lambda: f(get("mla_w_dq")), "q_norm": lambda: _cols(get("mla_q_norm")), "w_o": lambda: f(get("mla_w_o")),
        "w_dkv_c": lambda: f(get("kv_w_dkv")[:, :KVLORA]), "w_dkv_pe": lambda: f(get("kv_w_dkv")[:, KVLORA:]),
        "w_dkv_pesw": lambda: f(get("kv_w_dkv")[:, KVLORA:][:, swap]),
        "kv_norm": lambda: _cols(get("kv_norm")),
        "ropec": ropec, "masks": masks, "ident": lambda: np.eye(128, dtype=np.float32),
        "ltri": lambda: np.triu(np.ones((128, 128), np.float32), 1),
        "iota": lambda: f(np.broadcast_to(np.arange(CAP, dtype=np.float32)[None, :], (128, CAP))),
        "dumprow": lambda: dumprow(),
    }

    def idx_t1(c):
        kk = np.arange(KC)[None, :]
        return ((kk * 128 + pp) * 8 + c).astype(np.int32)

    def idx_tF(c):
        ch, pq = _ag_layout(8 * 256, TC, 2)
        t = np.zeros((128, KC), np.int32)
        for kk in range(KC):
            for q in range(128):
                f = kk * 128 + q
                t[q, kk] = _ag_rowoff(ch, pq, f // 256, c * 256 + f % 256)
        return t

    def idx_L(c):
        t = np.zeros((128, EPC * NCORE * 2), np.int32)
        p = np.arange(128)
        for el in range(EPC):
            for r in range(NCORE):
                for sb in range(2):
                    t[:, (el * NCORE + r) * 2 + sb] = r * (2 * 128 * NEXP) + (sb * 128 + p) * NEXP + (c * EPC + el)
        return t

    def vals4(c):
        ch, pq = _ag_layout(TC, D, 2)
        v = np.zeros((128, 8, NEXP, 4), np.float32)
        for tk in range(8):
            for q in range(128):
                v[q, tk, :, 0] = _ag_rowoff(ch, pq, c, tk * 128 + q)
                v[q, tk, :, 1] = _ysp_row(c, tk, q)
        v[:, :, :, 2] = 1.0
        return v

    def idx_y(c):
        return (c * TC + np.arange(8)[None, :] * 128 + pp).astype(np.int32)

    def dumprow():
        return (SEQ + np.arange(2)[None, :] * 128 + pp).astype(np.float32)

    def idx_g(c):
        ig = np.zeros((128, EPC * 16), np.int32)
        for el in range(EPC):
            for tt in range(16):
                r, half = tt // 2, tt % 2
                ig[:, el * 16 + tt] = (r * NEXP + c * EPC + el) * 2 + half
        return ig

    ts = lambda c: slice(c * TC, (c + 1) * TC)
    cs = lambda c: slice(c * 256, (c + 1) * 256)
    es = lambda c: slice(c * EPC, (c + 1) * EPC)
    percore = {
        "xT": lambda c: f(get("x")[0, ts(c)].T),
        "pT": lambda c: f(np.swapaxes(get("p")[:, 0, ts(c)], 1, 2)),
        "pos": lambda c: np.ascontiguousarray(get("positions")[:, ts(c)].astype(np.int32)),
        "w_in_r": lambda c: f(get("rg_w_in")[:, :, D + c * 256:D + (c + 1) * 256]),
        "conv_w": lambda c: f(np.transpose(get("rg_conv_w")[:, :, cs(c)].reshape(N_A, 4, 2, 128), (0, 3, 2, 1))),
        "conv_b": lambda c: _cols(get("rg_conv_b")[:, cs(c)]),
        "ga_w": lambda c: f(get("rg_gate_a_w")[:, c]), "gx_w": lambda c: f(get("rg_gate_x_w")[:, c]),
        "ga_b": lambda c: _cols(get("rg_gate_a_b")[:, c]), "gx_b": lambda c: _cols(get("rg_gate_x_b")[:, c]),
        "lam": lambda c: _cols(get("rg_lambda")[:, cs(c)]),
        "w1": lambda c: f(get("moe_w1")[:, es(c)]), "b1": lambda c: _cols(get("moe_b1")[:, es(c)]),
        "w2": lambda c: f(get("moe_w2")[:, es(c)]), "b2": lambda c: _cols(get("moe_b2")[:, es(c)]),
        "w_uq_n": lambda c: f(w_uq()[:, :, 2 * c:2 * c + 2, :128].reshape(2, QLORA, 256)),
        "w_uq_pe": lambda c: f(w_uq()[:, :, 2 * c:2 * c + 2, 128:].reshape(2, QLORA, 128)),
        "w_uq_pesw": lambda c: f(w_uq()[:, :, 2 * c:2 * c + 2, 128:][..., swap].reshape(2, QLORA, 128)),
        "w_ukv_k": lambda c: f(w_ukv()[:, 2 * c:2 * c + 2, :128].reshape(KVLORA, 256)),
        "w_ukv_v": lambda c: f(w_ukv()[:, 2 * c:2 * c + 2, 128:].reshape(KVLORA, 256)),
        "idx_t1": idx_t1, "idx_tF": idx_tF, "idx_g": idx_g, "idx_L": idx_L, "idx_y": idx_y, "vals4": vals4,
        "b2row": lambda c: f(get("moe_b2")[:, es(c)]),
    }
    names = list(common) + list(percore) if used is None else list(used)
    shared = {n: common[n]() for n in names if n in common}
    maps = []
    for c in range(NCORE):
        m = dict(shared)
        for n in names:
            if n in percore:
                m[n] = percore[n](c)
        maps.append(m)
    return maps


def build_program(stop_after=None, dumps=(), debug=False):
    nc = bass.Bass("TRN2", target_bir_lowering=False)
    prog = Prog(nc, stop_after=stop_after, dumps=dumps, debug=debug)
    with nc.allow_low_precision("bf16 matmul operands, fp32 accumulation (reference tolerance is bf16-level)"):
        prog.build()
    return nc, prog


def kernel(**inputs):
    nc, prog = build_program()
    maps = make_in_maps(inputs, used=sorted(prog.inp.keys()))
    res = run_bass_kernel_spmd(nc, maps, core_ids=list(range(NCORE)))
    out = np.concatenate([np.asarray(r["outT"], np.float32).T for r in res.results], axis=0)
    return np.ascontiguousarray(out[None]).astype(np.float32)
```
